# Optimizing a Trainium2 kernel written in Bass

```python
import math
import jax, jax.numpy as jnp
from jax import lax
import numpy as np

D_MODEL = 1024
BATCH = 4
SEQ = 4096
DEPTH = 4

HEAD_DIM = 64
MIX_WIDTH = D_MODEL
GROUP_WIDTH = MIX_WIDTH // 4
CONV_WIDTH = 3
POOL_WINDOWS = (2, 4, 8, 16)
POOL_GROUP = GROUP_WIDTH // len(POOL_WINDOWS)
DSA_HEADS = GROUP_WIDTH // HEAD_DIM
IDX_HEADS = 8
IDX_DIM = 64
DSA_TOPK_MAX = 256
MOBA_HEADS = GROUP_WIDTH // HEAD_DIM
MOBA_BLOCK = 256
MOBA_TOPB_MAX = 3
REL_BUCKETS = 32
REL_MAX_DIST = 128
N_ATTN_HEADS = DSA_HEADS + MOBA_HEADS
D_FF = -(-8 * D_MODEL // (3 * 256)) * 256
PLE_DIM = 256
Q_BLOCK = 128
MOBA_Q_BLOCK = 64
RMS_EPS = 1e-6
SPLIT_SIZES = (GROUP_WIDTH, GROUP_WIDTH, GROUP_WIDTH,
               GROUP_WIDTH,
               GROUP_WIDTH, GROUP_WIDTH, GROUP_WIDTH, IDX_HEADS * IDX_DIM, IDX_DIM, IDX_HEADS,
               GROUP_WIDTH, GROUP_WIDTH, GROUP_WIDTH)
IN_COLS = sum(SPLIT_SIZES)

kernel_name = "hybrid_conv_pool_dsa_moba_trunk"


def rmsnorm(x, g):
    x32 = x.astype(jnp.float32)
    y = x32 * lax.rsqrt(jnp.mean(x32 * x32, axis=-1, keepdims=True) + RMS_EPS)
    return (y * g.astype(jnp.float32)).astype(x.dtype)


def rel_bucket(dist):
    n = jnp.maximum(dist, 0)
    max_exact = REL_BUCKETS // 2
    nf = jnp.maximum(n, 1).astype(jnp.float32)
    large = max_exact + (jnp.log(nf / max_exact) / math.log(REL_MAX_DIST / max_exact)
                         * (REL_BUCKETS - max_exact)).astype(jnp.int32)
    large = jnp.minimum(large, REL_BUCKETS - 1)
    return jnp.where(n < max_exact, n, large)


def short_conv_mixer(a_in, gate_c, gate_b, conv_w):
    h = gate_c * a_in
    y = lax.conv_general_dilated(h, conv_w[:, None, :].astype(h.dtype), window_strides=(1,),
                                 padding=[(CONV_WIDTH - 1, 0)],
                                 dimension_numbers=("NWC", "WIO", "NWC"),
                                 feature_group_count=h.shape[-1])
    return gate_b * y


def pool_mixer(v, pool_w, pool_scale):
    b, s, _ = v.shape
    v32 = v.astype(jnp.float32)
    cs = jnp.concatenate([jnp.zeros((b, 1, GROUP_WIDTH), jnp.float32), lax.cumsum(v32, axis=1)], axis=1)
    t = jnp.arange(s)
    outs = []
    for g, w in enumerate(POOL_WINDOWS):
        sl = slice(g * POOL_GROUP, (g + 1) * POOL_GROUP)
        c = cs[:, :, sl]
        upper = c[:, 1:]
        lower = jnp.concatenate([jnp.zeros((b, w - 1, POOL_GROUP), jnp.float32), c[:, :s + 1 - w]], axis=1)
        cnt = jnp.minimum(t + 1, w).astype(jnp.float32)[None, :, None]
        outs.append((upper - lower) / cnt - v32[:, :, sl])
    d = jnp.stack(outs, axis=2)
    y = jnp.einsum("bsgc,gcd->bsgd", d, pool_w.astype(jnp.float32)).reshape(b, s, GROUP_WIDTH)
    return (y * pool_scale.astype(jnp.float32)).astype(v.dtype)


def dsa_mixer(q, k, v, iq, ik, iw, bias_tab):
    b, s, nh, hd = q.shape
    topk = min(DSA_TOPK_MAX, s // 4)
    idx_scale = (IDX_HEADS ** -0.5) * (IDX_DIM ** -0.5)
    ik32 = ik.astype(jnp.float32)
    bidx = jnp.arange(b)[:, None, None]
    spos = jnp.arange(s)

    def chunk(c):
        t0 = c * Q_BLOCK
        tq = t0 + jnp.arange(Q_BLOCK)
        qc = lax.dynamic_slice_in_dim(q, t0, Q_BLOCK, axis=1).astype(jnp.float32)
        iqc = lax.dynamic_slice_in_dim(iq, t0, Q_BLOCK, axis=1).astype(jnp.float32)
        iwc = lax.dynamic_slice_in_dim(iw, t0, Q_BLOCK, axis=1).astype(jnp.float32)
        sc = jax.nn.relu(jnp.einsum("bqhd,bsd->bqhs", iqc, ik32))
        score = jnp.einsum("bqh,bqhs->bqs", iwc, sc) * idx_scale
        score = jnp.where(spos[None, None, :] <= tq[None, :, None], score, -jnp.inf)
        _, sel = lax.top_k(score, topk)
        valid = sel <= tq[None, :, None]
        kg = k[bidx, sel].astype(jnp.float32)
        vg = v[bidx, sel].astype(jnp.float32)
        logits = jnp.einsum("bqhd,bqkhd->bhqk", qc, kg) * (hd ** -0.5)
        bias = jnp.moveaxis(bias_tab.astype(jnp.float32)[:, rel_bucket(tq[None, :, None] - sel)], 0, 1)
        logits = jnp.where(valid[:, None], logits + bias, -jnp.inf)
        probs = jax.nn.softmax(logits, axis=-1)
        return jnp.einsum("bhqk,bqkhd->bqhd", probs, vg)

    out = lax.map(chunk, jnp.arange(s // Q_BLOCK))
    return out.transpose(1, 0, 2, 3, 4).reshape(b, s, nh * hd).astype(q.dtype)


def moba_mixer(q, k, v, bias_tab):
    b, s, nh, hd = q.shape
    nb = -(-s // MOBA_BLOCK)
    pad = nb * MOBA_BLOCK - s
    kp = jnp.pad(k, ((0, 0), (0, pad), (0, 0), (0, 0)))
    vp = jnp.pad(v, ((0, 0), (0, pad), (0, 0), (0, 0)))
    kblk = kp.reshape(b, nb, MOBA_BLOCK, nh, hd)
    kmean = jnp.mean(kblk.astype(jnp.float32), axis=2)
    kblk_t = kblk.transpose(0, 3, 1, 2, 4)
    vblk_t = vp.reshape(b, nb, MOBA_BLOCK, nh, hd).transpose(0, 3, 1, 2, 4)
    topb = min(MOBA_TOPB_MAX, nb - 1)
    scale = hd ** -0.5
    tab = bias_tab.astype(jnp.float32)
    bi = jnp.arange(b)[:, None, None, None]
    hi = jnp.arange(nh)[None, :, None, None]
    blk_off = jnp.arange(MOBA_BLOCK)

    def chunk(c):
        t0 = c * MOBA_Q_BLOCK
        tq = t0 + jnp.arange(MOBA_Q_BLOCK)
        ob = t0 // MOBA_BLOCK
        qc = lax.dynamic_slice_in_dim(q, t0, MOBA_Q_BLOCK, axis=1).astype(jnp.float32)
        ko = lax.dynamic_slice_in_dim(kp, ob * MOBA_BLOCK, MOBA_BLOCK, axis=1).astype(jnp.float32)
        vo = lax.dynamic_slice_in_dim(vp, ob * MOBA_BLOCK, MOBA_BLOCK, axis=1).astype(jnp.float32)
        spos_o = ob * MOBA_BLOCK + blk_off
        lo = jnp.einsum("bqhd,bshd->bhqs", qc, ko) * scale + tab[:, rel_bucket(tq[:, None] - spos_o[None, :])][None]
        lo = jnp.where((spos_o[None, :] <= tq[:, None])[None, None], lo, -jnp.inf)
        if topb == 0:
            po = jax.nn.softmax(lo, axis=-1)
            return jnp.einsum("bhqs,bshd->bqhd", po, vo)
        gate = jnp.einsum("bqhd,bnhd->bhqn", qc, kmean)
        gate = jnp.where(jnp.arange(nb)[None, None, None, :] < ob, gate, -jnp.inf)
        _, sel = lax.top_k(gate, topb)
        valid = sel < ob
        kg = kblk_t[bi, hi, sel].astype(jnp.float32)
        vg = vblk_t[bi, hi, sel].astype(jnp.float32)
        lp = jnp.einsum("bqhd,bhqnsd->bhqns", qc, kg) * scale
        spos_p = sel[..., None] * MOBA_BLOCK + blk_off
        lp = lp + tab[hi[..., None], rel_bucket(tq[None, None, :, None, None] - spos_p)]
        lp = jnp.where(valid[..., None], lp, -jnp.inf)
        npast = topb * MOBA_BLOCK
        logits = jnp.concatenate([lp.reshape(b, nh, MOBA_Q_BLOCK, npast), lo], axis=-1)
        probs = jax.nn.softmax(logits, axis=-1)
        pp = probs[..., :npast].reshape(b, nh, MOBA_Q_BLOCK, topb, MOBA_BLOCK)
        po = probs[..., npast:]
        return (jnp.einsum("bhqns,bhqnsd->bqhd", pp, vg) + jnp.einsum("bhqs,bshd->bqhd", po, vo))

    out = lax.map(chunk, jnp.arange(s // MOBA_Q_BLOCK))
    return out.transpose(1, 0, 2, 3, 4).reshape(b, s, nh * hd).astype(q.dtype)


def swiglu(h, w_gate_up, w_down):
    gu = h @ w_gate_up
    gate, up = jnp.split(gu, 2, axis=-1)
    return (jax.nn.silu(gate) * up) @ w_down


def setup_inputs(seed: int = 0) -> dict:
    key = jax.random.key(seed)
    ks = jax.random.split(key, 20)
    f32 = jnp.float32

    def nrm(k, shape, scale):
        return jax.random.normal(k, shape, f32) * scale

    def gain(k, shape):
        return 1.0 + 0.05 * jax.random.normal(k, shape, f32)

    return {
        "x": nrm(ks[0], (BATCH, SEQ, D_MODEL), 1.0),
        "p": nrm(ks[1], (DEPTH, BATCH, SEQ, PLE_DIM), 1.0),
        "rel_bias": nrm(ks[2], (N_ATTN_HEADS, REL_BUCKETS), 0.5),
        "g_mix_pre": gain(ks[3], (DEPTH, D_MODEL)),
        "w_in": nrm(ks[4], (DEPTH, D_MODEL, IN_COLS), D_MODEL ** -0.5),
        "conv_w": nrm(ks[5], (DEPTH, CONV_WIDTH, GROUP_WIDTH), CONV_WIDTH ** -0.5),
        "pool_w": nrm(ks[6], (DEPTH, len(POOL_WINDOWS), POOL_GROUP, POOL_GROUP), POOL_GROUP ** -0.5),
        "pool_scale": gain(ks[7], (DEPTH, GROUP_WIDTH)),
        "w_out": nrm(ks[8], (DEPTH, MIX_WIDTH, D_MODEL), MIX_WIDTH ** -0.5),
        "g_mix_post": gain(ks[9], (DEPTH, D_MODEL)),
        "g_ffn_pre": gain(ks[10], (DEPTH, D_MODEL)),
        "w_gate_up": nrm(ks[11], (DEPTH, D_MODEL, 2 * D_FF), D_MODEL ** -0.5),
        "w_down": nrm(ks[12], (DEPTH, D_FF, D_MODEL), D_FF ** -0.5),
        "g_ffn_post": gain(ks[13], (DEPTH, D_MODEL)),
        "g_ple": gain(ks[14], (DEPTH, D_MODEL)),
        "w_ple_gate": nrm(ks[15], (DEPTH, D_MODEL, D_MODEL), D_MODEL ** -0.5),
        "w_ple_proj": nrm(ks[16], (DEPTH, PLE_DIM, D_MODEL), PLE_DIM ** -0.5),
    }


def reference(x, p, rel_bias, g_mix_pre, w_in, conv_w, pool_w, pool_scale, w_out, g_mix_post,
              g_ffn_pre, w_gate_up, w_down, g_ffn_post, g_ple, w_ple_gate, w_ple_proj):
    b, s, _ = x.shape
    split_at = tuple(int(o) for o in np.cumsum(SPLIT_SIZES)[:-1])

    def heads(t):
        return t.reshape(b, s, -1, HEAD_DIM)

    for i in range(DEPTH):
        h = rmsnorm(x, g_mix_pre[i])
        z = h @ w_in[i]
        (a_in, a_c, a_b, pv, cq, ck, cv, iq, ik, iw, dq, dk, dv) = jnp.split(z, split_at, axis=-1)
        ya = short_conv_mixer(a_in, a_c, a_b, conv_w[i])
        yb = pool_mixer(pv, pool_w[i], pool_scale[i])
        yc = dsa_mixer(heads(cq), heads(ck), heads(cv), iq.reshape(b, s, IDX_HEADS, IDX_DIM), ik, iw,
                       rel_bias[:DSA_HEADS])
        yd = moba_mixer(heads(dq), heads(dk), heads(dv), rel_bias[DSA_HEADS:])
        mix = jnp.concatenate([ya, yb, yc, yd], axis=-1) @ w_out[i]
        x = x + rmsnorm(mix, g_mix_post[i])
        f = swiglu(rmsnorm(x, g_ffn_pre[i]), w_gate_up[i], w_down[i])
        x = x + rmsnorm(f, g_ffn_post[i])
        gate = jax.nn.sigmoid(rmsnorm(x, g_ple[i]) @ w_ple_gate[i])
        x = x + gate * (p[i] @ w_ple_proj[i])
    return x
```

```python
import numpy as np
import concourse.bass as bass
import concourse.mybir as mybir
from concourse.bass_utils import run_bass_kernel_spmd

F32, BF16 = mybir.dt.float32, mybir.dt.bfloat16
ALU, AF, AX = mybir.AluOpType, mybir.ActivationFunctionType, mybir.AxisListType

S = 4096
D = 1024
NT = 32
NG = 8
DEPTH = 4
DFF = 2816
INC = 3144
NEG = -30000.0
KTOP = 256
NBIS = 16
ARENA_BYTES = 132 * 1024


class _Stop(Exception):
    pass


class Buf:
    __slots__ = ("name", "w", "r")

    def __init__(self, name):
        self.name = name
        self.w = None
        self.r = {}


class Ins:
    __slots__ = ("eng", "fn", "deps", "sig", "ms", "dma", "slot")


ENGS = ("pe", "act", "dve", "pool", "sp", "poolw")


KSEM = 14
DMAQ = ("sp", "pool", "poolw")


class Prog:
    def __init__(self):
        self.q = {e: [] for e in ENGS}
        self.extra = {e: set() for e in ENGS}
        self.lastdma = {}
        self.ndma = {e: 0 for e in ENGS}
        self.n = 0

    def emit(self, eng, fn, reads=(), writes=(), dma=False):
        ins = Ins()
        ins.eng = eng
        ins.fn = fn
        ins.sig = False
        ins.ms = None
        ins.dma = dma
        ins.slot = None
        deps = set(self.extra[eng])
        self.extra[eng] = set()
        for b in reads:
            if b.w is not None:
                deps.add(b.w)
        for b in writes:
            if b.w is not None:
                deps.add(b.w)
            deps.update(b.r.values())
        if dma:
            i = self.ndma[eng]
            self.ndma[eng] = i + 1
            ins.slot = i % KSEM
            ins.ms = 16 * (i // KSEM + 1)
            prev = self.lastdma.get((eng, ins.slot))
            if prev is not None:
                deps.add(prev)
            self.lastdma[(eng, ins.slot)] = ins
        fd = []
        for d in deps:
            if d is ins:
                continue
            if (not d.dma) and d.eng == eng and eng == "pe":
                continue
            d.sig = True
            fd.append(d)
        ins.deps = fd
        for b in reads:
            b.r[(eng, dma, ins.slot)] = ins
        for b in writes:
            b.w = ins
            b.r = {}
        self.q[eng].append(ins)
        self.n += 1
        return ins

    def generate(self, nc, block, sems):
        for e in ENGS:
            c = 0
            for ins in self.q[e]:
                if (not ins.dma) and ins.sig:
                    c += 1
                    ins.ms = c
        final = {}
        for (e, slot), ins in self.lastdma.items():
            if e != "poolw":
                final[("d", e, slot)] = ins.ms

        def run(e, h, waited):
            for ins in self.q[e]:
                need = {}
                for d in ins.deps:
                    key = ("d", d.eng, d.slot) if d.dma else ("c", d.eng)
                    if need.get(key, 0) < d.ms:
                        need[key] = d.ms
                for key, v in need.items():
                    if waited.get(key, 0) < v:
                        h.wait_ge(sems[key], v)
                        waited[key] = v
                r = ins.fn(h)
                if ins.dma:
                    r.then_inc(sems[("d", e, ins.slot)], 16)
                elif ins.sig:
                    r.then_inc(sems[("c", e)], 1)
            if e == "sp":
                for key, v in final.items():
                    if waited.get(key, 0) < v:
                        h.wait_ge(sems[key], v)

        @block.tensor
        def _(h):
            run("pe", h, {})

        @block.scalar
        def _(h):
            run("act", h, {})

        @block.vector
        def _(h):
            run("dve", h, {})

        @block.gpsimd
        def _(h):
            w = {}
            run("poolw", h, w)
            run("pool", h, w)

        @block.sync
        def _(h):
            run("sp", h, {})


def build(nlayers=DEPTH, stop=None, debug=False, nbis=NBIS):
    nc = bass.Bass("TRN2", target_bir_lowering=False)
    P = Prog()
    skind = "ExternalOutput" if debug else "Internal"

    def din(name, shape, dt=F32):
        return nc.dram_tensor(name, list(shape), dt, kind="ExternalInput").ap()

    def dscr(name, shape, dt):
        return nc.dram_tensor(name, list(shape), dt, kind=skind).ap()

    x_in = din("x", [S, D])
    p_in = din("p", [nlayers, S, 256])
    w_in_d = din("w_in", [nlayers, D, INC])
    w_out_d = din("w_out", [nlayers, D, D])
    w_gu_d = din("w_gate_up", [nlayers, D, 2 * DFF])
    w_dn_d = din("w_down", [nlayers, DFF, D])
    w_pg_d = din("w_ple_gate", [nlayers, D, D])
    w_pp_d = din("w_ple_proj", [nlayers, 256, D])
    gpack_d = din("gpack", [DEPTH + 1, 5, D])
    convw_d = din("convw_t", [DEPTH, 256, 3])
    poolw_d = din("pool_w", [DEPTH, 4, 64, 64])
    pscale_d = din("pscale_t", [DEPTH, 128, 2])
    relT_d = din("relT", [128, 16, 128])
    relb_d = din("relb", [128, 256])
    consts_d = din("consts", [128, 512])
    y_out = nc.dram_tensor("y", [S, D], F32, kind="ExternalOutput").ap()

    wb = {}
    for l in range(nlayers):
        wb[l] = dict(
            w_in=nc.dram_tensor(f"wb_in{l}", [D, INC], BF16, kind="Internal").ap(),
            w_out=nc.dram_tensor(f"wb_out{l}", [D, D], BF16, kind="Internal").ap(),
            w_gu=nc.dram_tensor(f"wb_gu{l}", [D, 2 * DFF], BF16, kind="Internal").ap(),
            w_dn=nc.dram_tensor(f"wb_dn{l}", [DFF, D], BF16, kind="Internal").ap(),
            w_pg=nc.dram_tensor(f"wb_pg{l}", [D, D], BF16, kind="Internal").ap(),
            w_pp=nc.dram_tensor(f"wb_pp{l}", [256, D], BF16, kind="Internal").ap(),
        )
    wsrc = dict(w_in=w_in_d, w_out=w_out_d, w_gu=w_gu_d, w_dn=w_dn_d, w_pg=w_pg_d, w_pp=w_pp_d)
    wbufs = {}

    x_cur = dscr("x_cur", [S, D], F32)
    aT = dscr("aT", [1024, S], F32)
    cqT = dscr("cqT", [256, S], BF16)
    dqT = dscr("dqT", [256, S], BF16)
    iqT = dscr("iqT", [512, S], BF16)
    ckT = dscr("ckT", [256, S], BF16)
    dkT = dscr("dkT", [256, S], BF16)
    ikT = dscr("ikT", [64, S], BF16)
    cvd = dscr("cv", [S, 256], BF16)
    dvd = dscr("dv", [S, 256], BF16)
    iwd = dscr("iw", [S, 8], F32)
    mixT = dscr("mixT", [1024, S], BF16)
    B_z = Buf("zscratch")
    B_mix = Buf("mixT")
    B_xcur = [Buf(f"xcur{g}") for g in range(NG)]

    import contextlib
    es = contextlib.ExitStack()
    with es:
        def sb(name, shape, dt):
            return es.enter_context(nc.sbuf_tensor(name, list(shape), dt))

        arena = sb("arena", [128, ARENA_BYTES // 2], BF16)
        consts = sb("consts_sb", [128, 512], F32)
        ident = sb("ident_bf", [128, 128], BF16)
        I4 = sb("I4", [128, 512], BF16)
        ones64 = sb("ones64", [128, 64], BF16)
        Tb = sb("Tb", [128, 8, 2, 128], BF16)
        relb = sb("relb_sb", [128, 256], F32)
        pwblk = sb("pwblk", [128, 2, 128], BF16)
        cw = sb("cw", [128, 2, 3], F32)
        pscale = sb("pscale", [128, 2], F32)
        small = sb("small", [128, 256], F32)
        bar_s = sb("bar_s", [128, 8], F32)
        WBt = [sb(f"WB{i}", [128, 8, 512], BF16) for i in range(4)]
        B_WB = [Buf(f"WB{i}") for i in range(4)]
        ps = [es.enter_context(nc.psum_tensor(f"ps{i}", [128, 512], F32)) for i in range(8)]
        B_ps = [Buf(f"ps{i}") for i in range(8)]
        B_const = Buf("const")
        B_layerc = Buf("layerconst")
        B_small = Buf("small")
        B_bar = {e: Buf("bar" + e) for e in ENGS}
        B_sm = {}
        B_msbs = [Buf(f"msb{t}") for t in range(4)]

        sem_names = [("c", "pe"), ("c", "act"), ("c", "dve"), ("c", "pool")]
        sem_names += [("d", q_, i_) for q_ in DMAQ for i_ in range(KSEM)]
        sems = {k: es.enter_context(nc.semaphore("s_" + "_".join(str(x_) for x_ in k))) for k in sem_names}
        block = es.enter_context(nc.Block())

        tri_f = consts[:, 128:256]
        trineg = consts[:, 256:384]
        c1 = consts[:, 384:400]
        c2 = consts[:, 400:416]
        invc = consts[:, 416:448].rearrange("p (c t) -> p c t", c=2, t=16)

        class Arena:
            def __init__(self):
                self.off = 0

            def reset(self):
                self.off = 0

            def get(self, name, shape, dt):
                nel = 1
                for s_ in shape[1:]:
                    nel *= s_
                nbytes = nel * (4 if dt == F32 else 2)
                nbytes = (nbytes + 63) // 64 * 64
                assert self.off + nbytes <= ARENA_BYTES, (name, self.off, nbytes)
                ap = arena[:, self.off // 2:(self.off + nbytes) // 2]
                if dt == F32:
                    ap = ap.bitcast(F32)
                ap = ap[0:shape[0], 0:nel]
                if len(shape) == 3:
                    ap = ap.rearrange("p (a b) -> p a b", a=shape[1], b=shape[2])
                elif len(shape) == 4:
                    ap = ap.rearrange("p (a b c) -> p a b c", a=shape[1], b=shape[2], c=shape[3])
                self.off += nbytes
                return ap, Buf(name)

        AR = Arena()

        def MM(out, lhsT, rhs, st, sp_, R, W):
            P.emit("pe", lambda h: h.matmul(out, lhsT=lhsT, rhs=rhs, start=st, stop=sp_), R, W)

        def TRP(out, in_, R, W):
            P.emit("pe", lambda h: h.transpose(out, in_, ident[:]), R, W)

        def ACT(out, in_, func, R, W, scale=None, bias=None, accum=None):
            kw = {}
            if scale is not None:
                kw["scale"] = scale
            if bias is not None:
                kw["bias"] = bias
            if accum is not None:
                kw["accum_out"] = accum
            P.emit("act", lambda h: h.activation(out=out, in_=in_, func=func, **kw), R, W)

        def TS(eng, out, in0, s1, s2, op0, op1, R, W, accum=None):
            kw = {}
            if op1 is not None:
                kw["op1"] = op1
            if accum is not None:
                kw["accum_out"] = accum
            P.emit(eng, lambda h: h.tensor_scalar(out=out, in0=in0, scalar1=s1, scalar2=s2, op0=op0, **kw), R, W)

        def TT(eng, out, in0, in1, op, R, W):
            P.emit(eng, lambda h: h.tensor_tensor(out=out, in0=in0, in1=in1, op=op), R, W)

        def STT(out, in0, scalar, in1, op0, op1, R, W):
            P.emit("dve", lambda h: h.scalar_tensor_tensor(out=out, in0=in0, scalar=scalar, in1=in1, op0=op0, op1=op1), R, W)

        def CP(eng, out, in_, R, W):
            P.emit(eng, lambda h: h.tensor_copy(out, in_), R, W)

        def MSET(eng, ap, val, W):
            P.emit(eng, lambda h: h.memset(ap, val), (), W)

        def RED(out, in_, op, R, W):
            P.emit("dve", lambda h: h.tensor_reduce(out=out, in_=in_, axis=AX.X, op=op), R, W)

        def DMA(q, out, in_, R, W):
            return P.emit(q, lambda h: h.dma_start(out=out, in_=in_), R, W, dma=True)

        def barrier():
            t = []
            t.append(P.emit("pe", lambda h: h.matmul(ps[5][0:1, 0:1], lhsT=ones64[0:1, 0:1], rhs=ones64[0:1, 0:1], start=True, stop=True),
                            (B_const,), (B_ps[5], B_bar["pe"])))
            t.append(P.emit("act", lambda h: h.activation(out=bar_s[0:1, 0:1], in_=bar_s[0:1, 4:5], func=AF.Copy), (B_const,), (B_bar["act"],)))
            t.append(P.emit("dve", lambda h: h.tensor_copy(bar_s[0:1, 1:2], bar_s[0:1, 5:6]), (B_const,), (B_bar["dve"],)))
            t.append(P.emit("pool", lambda h: h.tensor_copy(bar_s[0:1, 2:3], bar_s[0:1, 6:7]), (B_const,), (B_bar["pool"],)))
            for i in t:
                i.sig = True
            allb = set(t) | set(v for k, v in P.lastdma.items() if k[0] != "poolw")
            for e in ENGS:
                if e != "poolw":
                    P.extra[e] |= allb

        DMA("sp", consts[:], consts_d, (), (B_const,))
        DMA("sp", relb[:], relb_d, (), (B_const,))
        DMA("pool", ident[:], consts_d[:, 0:128], (), (B_const,))
        for i in range(4):
            DMA("pool", I4[:, i * 128:(i + 1) * 128], consts_d[:, 0:128], (), (B_const,))
        MSET("pool", ones64[:], 1.0, (B_const,))
        MSET("pool", pwblk[:], 0.0, (B_const,))
        MSET("pool", bar_s[:], 0.0, (B_const,))
        AR.reset()
        relT_sb, B_relT = AR.get("relT", [128, 16, 128], F32)
        DMA("sp", relT_sb, relT_d, (), (B_relT,))
        for h_ in range(8):
            b31 = relb[:, h_ * 32 + 31:h_ * 32 + 32]
            STT(Tb[:, h_, 0, :], relT_sb[:, h_ * 2, :], b31, tri_f, ALU.subtract, ALU.add, (B_relT, B_const), (B_const,))
            TS("dve", Tb[:, h_, 1, :], relT_sb[:, h_ * 2 + 1, :], b31, None, ALU.subtract, None, (B_relT, B_const), (B_const,))

        barrier()
        for l in range(nlayers):
            for nm in ("w_in", "w_out", "w_gu", "w_dn", "w_pg", "w_pp"):
                src = wsrc[nm][l]
                dst = wb[l][nm]
                rows = src.shape[0]
                step = 256
                bl = []
                for r0 in range(0, rows, step):
                    r1 = min(rows, r0 + step)
                    b = Buf(f"wb{l}{nm}{r0}")
                    DMA("poolw", dst[r0:r1, :], src[r0:r1, :], (), (b,))
                    bl.append(b)
                wbufs[(l, nm)] = bl

        class WStream:
            def __init__(self):
                self.plan = []
                self.issued = 0
                self.taken = 0

            def add(self, l, nm, view_fn, shape):
                self.plan.append((l, nm, view_fn, shape))

            def _issue(self):
                i = self.issued
                if i >= len(self.plan):
                    return
                l, nm, view_fn, shape = self.plan[i]
                slot = i % 4
                dst = WBt[slot][:]
                for dstv, srcv in view_fn(dst, wb[l][nm]):
                    DMA("sp", dstv, srcv, tuple(wbufs[(l, nm)]), (B_WB[slot],))
                self.issued += 1

            def start(self):
                while self.issued < min(3, len(self.plan)):
                    self._issue()

            def get(self):
                i = self.taken
                assert i < self.issued, "weight stream underflow"
                slot = i % 4
                self.taken += 1
                self._issue()
                return WBt[slot], B_WB[slot]

        WS = WStream()

        def wv_cols(k0, nk, c0, c1):
            def f(dst, w):
                return [(dst[:, 0:nk, 0:c1 - c0],
                         w[k0 * 128:(k0 + nk) * 128, c0:c1].rearrange("(k p) c -> p k c", p=128))]
            return f

        def wv_gu(gb):
            def f(dst, w):
                w3 = w.rearrange("(k p) c -> p k c", p=128)
                return [(dst[:, :, 0:256], w3[:, :, gb * 256:(gb + 1) * 256]),
                        (dst[:, :, 256:512], w3[:, :, DFF + gb * 256:DFF + (gb + 1) * 256])]
            return f

        IN_BLOCKS = [(0, 512), (512, 1024), (1024, 1536), (1536, 2048), (2048, 2376), (2376, 2888), (2888, 3144)]

        def plan_A(l):
            for (c0, c1) in IN_BLOCKS:
                WS.add(l, "w_in", wv_cols(0, 8, c0, c1), None)

        def plan_dense(l, with_A):
            for c in range(2):
                WS.add(l, "w_out", wv_cols(0, 8, c * 512, (c + 1) * 512), None)
            for gb in range(11):
                WS.add(l, "w_gu", wv_gu(gb), None)
            for c in range(2):
                for (k0, nk) in ((0, 8), (8, 8), (16, 6)):
                    WS.add(l, "w_dn", wv_cols(k0, nk, c * 512, (c + 1) * 512), None)
            for c in range(2):
                WS.add(l, "w_pg", wv_cols(0, 8, c * 512, (c + 1) * 512), None)
            if with_A:
                plan_A(l + 1)

        for g in range(NG):
            plan_A(0)
        if stop != "A0":
            for l in range(nlayers):
                if stop is not None and stop.startswith("B1a0"):
                    break
                for g in range(NG):
                    plan_dense(l, l + 1 < nlayers)
                if stop is not None and stop.startswith("dense0"):
                    break
        WS.start()

        class Rot:
            def __init__(self, idx):
                self.idx = idx
                self.i = 0

            def get(self):
                k = self.idx[self.i % len(self.idx)]
                self.i += 1
                return ps[k], B_ps[k]

        def load_gains(pidx, gB, B_g):
            DMA("sp", gB.rearrange("p a b -> p (a b)"),
                gpack_d[pidx:pidx + 1].rearrange("o a b -> o (a b)").partition_broadcast(128), (), (B_g,))

        def rstd_from(ssq_ap, out_ap, n_inv, R, W):
            ACT(out_ap, ssq_ap, AF.Sqrt, R, W, scale=n_inv, bias=1e-6)
            P.emit("dve", lambda h: h.reciprocal(out=out_ap, in_=out_ap), W, W)

        def norm_T(xg, B_xg, gB, B_g, gslot, hT, B_hT, hb, B_hb, junk, B_junk, rotT, sc0):
            for t in range(4):
                ssq = small[:, sc0 + t:sc0 + t + 1]
                rs = small[:, sc0 + 4 + t:sc0 + 5 + t]
                B_s_ = B_sm.setdefault(("n", t), Buf(f"smn{t}"))
                ACT(junk[:, 0:1024], xg[:, t, :], AF.Square, (B_xg,), (B_junk, B_s_), accum=ssq)
                rstd_from(ssq, rs, 1.0 / D, (B_s_,), (B_s_,))
                hbt = hb[t % 2]
                STT(hbt, xg[:, t, :], rs, gB[:, gslot, :], ALU.mult, ALU.mult, (B_xg, B_s_, B_g), (B_hb[t % 2],))
                pt, B_pt = rotT.get()
                ptb = pt[:].bitcast(BF16)
                for k in range(8):
                    TRP(ptb[:, k * 128:(k + 1) * 128], hbt[:, k * 128:(k + 1) * 128], (B_hb[t % 2], B_const), (B_pt,))
                ACT(hT[:, :, t * 128:(t + 1) * 128], ptb.rearrange("p (k q) -> p k q", k=8, q=128), AF.Copy, (B_pt,), (B_hT,))

        def tm_postnorm(lhs_fn, B_lhs, nkc_list, gB, B_g, gslot, xg, B_xg, msb, B_msb, junk, B_junk, rotM, sc0):
            for c in range(2):
                accs = [rotM.get() for _ in range(4)]
                nblk = len(nkc_list)
                kbase = 0
                for bi, nk in enumerate(nkc_list):
                    wt, B_w = WS.get()
                    for t in range(4):
                        pa, B_pa = accs[t]
                        for k in range(nk):
                            MM(pa[:, :], lhs_fn(kbase + k, t), wt[:, k, :], (bi == 0 and k == 0), (bi == nblk - 1 and k == nk - 1),
                               (B_lhs, B_w), (B_pa,))
                    kbase += nk
                if stop == "dense0_1a":
                    raise _Stop()
                for t in range(4):
                    pa, B_pa = accs[t]
                    B_s_ = B_sm.setdefault(("p", t), Buf(f"smp{t}"))
                    CP("dve", msb[:, t, c * 512:(c + 1) * 512], pa[:, :], (B_pa,), (B_msbs[t],))
                    ACT(junk[:, 0:512], msb[:, t, c * 512:(c + 1) * 512], AF.Square, (B_msbs[t],), (B_junk, B_s_),
                        accum=small[:, sc0 + t * 2 + c:sc0 + t * 2 + c + 1])
                if stop == "dense0_1b":
                    raise _Stop()
            if stop == "dense0_1c":
                raise _Stop()
            for t in range(4):
                B_s_ = B_sm[("p", t)]
                tot = small[:, sc0 + 8 + t:sc0 + 9 + t]
                TT("dve", tot, small[:, sc0 + t * 2:sc0 + t * 2 + 1], small[:, sc0 + t * 2 + 1:sc0 + t * 2 + 2], ALU.add, (B_s_,), (B_s_,))
                rs = small[:, sc0 + 12 + t:sc0 + 13 + t]
                rstd_from(tot, rs, 1.0 / D, (B_s_,), (B_s_,))
                STT(msb[:, t, :], msb[:, t, :], rs, gB[:, gslot, :], ALU.mult, ALU.mult, (B_msbs[t], B_s_, B_g), (B_msbs[t],))
                TT("pool", xg[:, t, :], xg[:, t, :], msb[:, t, :], ALU.add, (B_xg, B_msbs[t]), (B_xg,))

        evac_flip = [0]

        def evac(out, in_, R, W, scale=None):
            evac_flip[0] ^= 1
            if evac_flip[0]:
                ACT(out, in_, AF.Copy, R, W, scale=scale)
            else:
                if scale is None:
                    CP("dve", out, in_, R, W)
                else:
                    TS("dve", out, in_, scale, None, ALU.mult, None, R, W)

        def phase_A(l, g, hT, B_hT, stf, B_stf, stb, B_stb, rotM):
            tok0 = g * 512
            cnt = [0]

            def fm(wt, B_w, cl0, cl1, dst_ap, scale=None, f32=False):
                M = cl1 - cl0
                pa, B_pa = rotM.get()
                for k in range(8):
                    MM(pa[0:M, :], wt[:, k, cl0:cl1], hT[:, k, :], k == 0, k == 7, (B_w, B_hT), (B_pa,))
                i = cnt[0] % 3
                cnt[0] += 1
                if f32:
                    st_, B_st = stf[i], B_stf[i]
                else:
                    st_, B_st = stb[i], B_stb[i]
                evac(st_[0:M, :], pa[0:M, :], (B_pa,), (B_st,), scale=scale)
                DMA("sp", dst_ap, st_[0:M, :], (B_st,), (B_z,))

            def tmm(wt, B_w, cl0, cl1, dst_fn, f32=False):
                ncol = cl1 - cl0
                for t in range(4):
                    pa, B_pa = rotM.get()
                    for k in range(8):
                        MM(pa[:, 0:ncol], hT[:, k, t * 128:(t + 1) * 128], wt[:, k, cl0:cl1], k == 0, k == 7, (B_w, B_hT), (B_pa,))
                    i = cnt[0] % 3
                    cnt[0] += 1
                    if f32:
                        st_, B_st = stf[i], B_stf[i]
                    else:
                        st_, B_st = stb[i], B_stb[i]
                    evac(st_[:, 0:ncol], pa[:, 0:ncol], (B_pa,), (B_st,))
                    DMA("sp", dst_fn(tok0 + t * 128), st_[:, 0:ncol], (B_st,), (B_z,))

            cols = slice(tok0, tok0 + 512)
            for bi in range(2):
                wt, B_w = WS.get()
                for j in range(4):
                    r0 = bi * 512 + j * 128
                    fm(wt, B_w, j * 128, (j + 1) * 128, aT[r0:r0 + 128, cols], f32=True)
            wt, B_w = WS.get()
            fm(wt, B_w, 0, 128, cqT[0:128, cols], scale=0.125)
            fm(wt, B_w, 128, 256, cqT[128:256, cols], scale=0.125)
            fm(wt, B_w, 256, 384, ckT[0:128, cols])
            fm(wt, B_w, 384, 512, ckT[128:256, cols])
            wt, B_w = WS.get()
            tmm(wt, B_w, 0, 256, lambda r: cvd[r:r + 128, :])
            fm(wt, B_w, 256, 384, iqT[0:128, cols])
            fm(wt, B_w, 384, 512, iqT[128:256, cols])
            wt, B_w = WS.get()
            fm(wt, B_w, 0, 128, iqT[256:384, cols])
            fm(wt, B_w, 128, 256, iqT[384:512, cols])
            fm(wt, B_w, 256, 320, ikT[0:64, cols])
            tmm(wt, B_w, 320, 328, lambda r: iwd[r:r + 128, :], f32=True)
            wt, B_w = WS.get()
            fm(wt, B_w, 0, 128, dqT[0:128, cols], scale=0.125)
            fm(wt, B_w, 128, 256, dqT[128:256, cols], scale=0.125)
            fm(wt, B_w, 256, 384, dkT[0:128, cols])
            fm(wt, B_w, 384, 512, dkT[128:256, cols])
            wt, B_w = WS.get()
            tmm(wt, B_w, 0, 256, lambda r: dvd[r:r + 128, :])

        def dense_arena():
            AR.reset()
            A = {}
            A["xg"] = AR.get("xg", [128, 4, 1024], F32)
            A["gB"] = AR.get("gB", [128, 5, 1024], F32)
            A["hb0"] = AR.get("hb0", [128, 1024], BF16)
            A["hb1"] = AR.get("hb1", [128, 1024], BF16)
            A["hT0"] = AR.get("hT0", [128, 8, 512], BF16)
            A["hT1"] = AR.get("hT1", [128, 8, 512], BF16)
            A["actT"] = AR.get("actT", [128, 22, 512], BF16)
            A["mixg"] = AR.get("mixg", [128, 8, 512], BF16)
            A["msb"] = AR.get("msb", [128, 4, 1024], F32)
            A["junk"] = AR.get("junk", [128, 1024], BF16)
            A["pg"] = AR.get("pg", [128, 4, 256], BF16)
            A["pT"] = AR.get("pT", [128, 2, 512], BF16)
            A["wpp"] = AR.get("wpp", [128, 2, 2, 512], BF16)
            A["sg0"] = AR.get("sg0", [128, 512], F32)
            A["sg1"] = AR.get("sg1", [128, 512], F32)
            for i in range(3):
                A[f"stf{i}"] = AR.get(f"stf{i}", [128, 512], F32)
                A[f"stb{i}"] = AR.get(f"stb{i}", [128, 512], BF16)
            return A

        def run_A0():
            A = dense_arena()
            xg, B_xg = A["xg"]
            gB, B_g = A["gB"]
            load_gains(0, gB, B_g)
            rotT = Rot([0, 1])
            rotM = Rot([2, 3, 4, 5, 6, 7])
            hTs = [A["hT0"], A["hT1"]]
            hb = [A["hb0"][0], A["hb1"][0]]
            B_hb = [A["hb0"][1], A["hb1"][1]]
            junk, B_junk = A["junk"]
            stf = [A[f"stf{i}"][0] for i in range(3)]
            B_stf = [A[f"stf{i}"][1] for i in range(3)]
            stb = [A[f"stb{i}"][0] for i in range(3)]
            B_stb = [A[f"stb{i}"][1] for i in range(3)]
            for g in range(NG):
                DMA("sp", xg, x_in[g * 512:(g + 1) * 512, :].rearrange("(t p) d -> p t d", p=128), (), (B_xg,))
                hT, B_hT = hTs[g % 2]
                norm_T(xg, B_xg, gB, B_g, 4, hT, B_hT, hb, B_hb, junk, B_junk, rotT, 0)
                phase_A(0, g, hT, B_hT, stf, B_stf, stb, B_stb, rotM)

        def run_dense(l):
            last = (l == DEPTH - 1)
            A = dense_arena()
            xg, B_xg = A["xg"]
            gB, B_g = A["gB"]
            load_gains(l + 1, gB, B_g)
            rotT = Rot([0, 1])
            rotM = Rot([2, 3, 4, 5, 6, 7])
            hTs = [A["hT0"], A["hT1"]]
            hb = [A["hb0"][0], A["hb1"][0]]
            B_hb = [A["hb0"][1], A["hb1"][1]]
            junk, B_junk = A["junk"]
            actT, B_actT = A["actT"]
            mixg, B_mixg = A["mixg"]
            msb, B_msb = A["msb"]
            pg, B_pg = A["pg"]
            pT, B_pT = A["pT"]
            sg = [A["sg0"][0], A["sg1"][0]]
            B_sg = [A["sg0"][1], A["sg1"][1]]
            stf = [A[f"stf{i}"][0] for i in range(3)]
            B_stf = [A[f"stf{i}"][1] for i in range(3)]
            stb = [A[f"stb{i}"][0] for i in range(3)]
            B_stb = [A[f"stb{i}"][1] for i in range(3)]
            xsrc = x_in if l == 0 else x_cur
            xdst = y_out if last else x_cur
            for g in range(NG):
                rows = slice(g * 512, (g + 1) * 512)
                rd = () if l == 0 else (B_xcur[g],)
                DMA("sp", xg, xsrc[rows, :].rearrange("(t p) d -> p t d", p=128), rd, (B_xg,))
                if stop == "dense0_0a":
                    return
                DMA("sp", mixg, mixT[:, rows].rearrange("(k p) s -> p k s", p=128), (B_mix,), (B_mixg,))
                if stop == "dense0_0b":
                    return
                DMA("pool", pg, p_in[l, rows, :].rearrange("(t p) c -> p t c", p=128), (), (B_pg,))
                if stop == "dense0_0":
                    return
                tm_postnorm(lambda k, t: mixg[:, k, t * 128:(t + 1) * 128], B_mixg, [8], gB, B_g, 0, xg, B_xg, msb, B_msb,
                            junk, B_junk, rotM, 16)
                if stop == 'dense0_1':
                    return
                hT, B_hT = hTs[0]
                norm_T(xg, B_xg, gB, B_g, 1, hT, B_hT, hb, B_hb, junk, B_junk, rotT, 0)
                if stop == 'dense0_2':
                    return
                for gb in range(11):
                    wt, B_w = WS.get()
                    for sc in range(2):
                        pg_, B_pg_ = rotM.get()
                        pu_, B_pu_ = rotM.get()
                        for k in range(8):
                            MM(pg_[:, :], wt[:, k, sc * 128:(sc + 1) * 128], hT[:, k, :], k == 0, k == 7, (B_w, B_hT), (B_pg_,))
                        for k in range(8):
                            MM(pu_[:, :], wt[:, k, 256 + sc * 128:256 + (sc + 1) * 128], hT[:, k, :], k == 0, k == 7, (B_w, B_hT), (B_pu_,))
                        ci = gb * 2 + sc
                        s_, B_s = sg[ci % 2], B_sg[ci % 2]
                        ACT(s_, pg_[:, :], AF.Silu, (B_pg_,), (B_s,))
                        TT("dve", actT[:, ci, :], s_, pu_[:, :], ALU.mult, (B_s, B_pu_), (B_actT,))
                if stop == 'dense0_3':
                    return
                tm_postnorm(lambda k, t: actT[:, k, t * 128:(t + 1) * 128], B_actT, [8, 8, 6], gB, B_g, 2, xg, B_xg, msb, B_msb,
                            junk, B_junk, rotM, 16)
                if stop == 'dense0_4':
                    return
                hT, B_hT = hTs[1]
                norm_T(xg, B_xg, gB, B_g, 3, hT, B_hT, hb, B_hb, junk, B_junk, rotT, 0)
                for t in range(4):
                    pt, B_pt = rotT.get()
                    ptb = pt[:].bitcast(BF16)
                    for k in range(2):
                        TRP(ptb[:, k * 128:(k + 1) * 128], pg[:, t, k * 128:(k + 1) * 128], (B_pg, B_const), (B_pt,))
                    CP("dve", pT[:, :, t * 128:(t + 1) * 128], ptb[:, 0:256].rearrange("p (k q) -> p k q", k=2, q=128), (B_pt,), (B_pT,))
                wpp, B_wpp = A["wpp"]
                for u_ in range(2):
                    DMA("sp", wpp[:, :, u_, :], wb[l]["w_pp"].rearrange("(k p) c -> p k c", p=128)[:, :, u_ * 512:(u_ + 1) * 512],
                        tuple(wbufs[(l, "w_pp")]), (B_wpp,))
                for c in range(2):
                    wt, B_w = WS.get()
                    for t in range(4):
                        pa, B_pa = rotM.get()
                        pb, B_pb = rotM.get()
                        for k in range(8):
                            MM(pa[:, :], hT[:, k, t * 128:(t + 1) * 128], wt[:, k, :], k == 0, k == 7, (B_hT, B_w), (B_pa,))
                        for k in range(2):
                            MM(pb[:, :], pT[:, k, t * 128:(t + 1) * 128], wpp[:, k, c, :], k == 0, k == 1, (B_pT, B_wpp), (B_pb,))
                        i = (c * 4 + t) % 2
                        ACT(sg[i], pa[:, :], AF.Sigmoid, (B_pa,), (B_sg[i],))
                        TT("dve", sg[i], sg[i], pb[:, :], ALU.mult, (B_sg[i], B_pb), (B_sg[i],))
                        TT("pool", xg[:, t, c * 512:(c + 1) * 512], xg[:, t, c * 512:(c + 1) * 512], sg[i], ALU.add, (B_xg, B_sg[i]), (B_xg,))
                if stop == 'dense0_5':
                    return
                DMA("sp", xdst[rows, :].rearrange("(t p) d -> p t d", p=128), xg, (B_xg,), () if last else (B_xcur[g],))
                if l + 1 < nlayers:
                    hT, B_hT = hTs[0]
                    norm_T(xg, B_xg, gB, B_g, 4, hT, B_hT, hb, B_hb, junk, B_junk, rotT, 0)
                    phase_A(l + 1, g, hT, B_hT, stf, B_stf, stb, B_stb, rotM)

        def run_B1a(l):
            AR.reset()
            ckS, B_ck = AR.get("ckS", [128, 2, S], BF16)
            cvS, B_cv = AR.get("cvS", [128, 32, 256], BF16)
            ikS, B_ik = AR.get("ikS", [128, S], BF16)
            dkS, B_dk = AR.get("dkS", [128, 2, S], BF16)
            dvS, B_dv = AR.get("dvS", [128, 32, 256], BF16)
            scores, B_sc = AR.get("scores", [128, S], F32)
            maskb, B_mk = AR.get("maskb", [128, S], BF16)
            kmf, B_kmf = AR.get("kmf", [128, 32], F32)
            kmT, B_km = AR.get("kmT", [128, 2, 16], BF16)
            mark = AR.off
            DMA("sp", ckS, ckT.rearrange("(hh p) s -> p hh s", p=128), (B_z,), (B_ck,))
            DMA("sp", dkS, dkT.rearrange("(hh p) s -> p hh s", p=128), (B_z,), (B_dk,))
            DMA("sp", ikS[0:64, :], ikT, (B_z,), (B_ik,))
            DMA("sp", ikS[64:128, :], ikT, (B_z,), (B_ik,))
            for n0 in range(0, 32, 4):
                DMA("sp", cvS[:, n0:n0 + 4, :], cvd[n0 * 128:(n0 + 4) * 128, :].rearrange("(n p) c -> p n c", p=128), (B_z,), (B_cv,))
                DMA("sp", dvS[:, n0:n0 + 4, :], dvd[n0 * 128:(n0 + 4) * 128, :].rearrange("(n p) c -> p n c", p=128), (B_z,), (B_dv,))
            DMA("sp", cw[:], convw_d[l].rearrange("(c p) j -> p c j", p=128), (), (B_layerc,))
            DMA("sp", pscale[:], pscale_d[l], (), (B_layerc,))
            for c in range(2):
                for gl in range(2):
                    DMA("pool", pwblk[gl * 64:(gl + 1) * 64, c, gl * 64:(gl + 1) * 64], poolw_d[l, 2 * c + gl], (), (B_layerc,))
            RED(kmf, dkS.rearrange("p hh (n s) -> p (hh n) s", n=16, s=256), ALU.add, (B_dk,), (B_kmf,))
            CP("dve", kmT.rearrange("p a b -> p (a b)"), kmf, (B_kmf,), (B_km,))

            if stop == "B1a0_ld":
                return
            CH = 256
            W_ = CH + 16
            cp = {}
            for nm in ("ain", "ac", "pv", "hh", "yy", "T1", "T2", "T3", "T4"):
                cp[nm] = AR.get(nm, [128, 2, W_], F32)
            cp["ab"] = AR.get("ab", [128, 2, CH], F32)
            cp["d"] = AR.get("d", [128, 2, CH], BF16)
            cp["ya"] = AR.get("ya", [128, 2, CH], BF16)
            cp["yb"] = AR.get("yb", [128, 2, CH], BF16)
            cp["t16"] = AR.get("t16", [128, 2, 16], F32)
            ain, B_ain = cp["ain"]
            ac, B_ac = cp["ac"]
            pv, B_pv = cp["pv"]
            hh, B_hh = cp["hh"]
            yy, B_yy = cp["yy"]
            T1, B_T1 = cp["T1"]
            T2, B_T2 = cp["T2"]
            T3, B_T3 = cp["T3"]
            T4, B_T4 = cp["T4"]
            ab, B_ab = cp["ab"]
            dd, B_dd = cp["d"]
            ya, B_ya = cp["ya"]
            yb, B_yb = cp["yb"]
            t16, B_t16 = cp["t16"]
            pcv, B_pcv = ps[6], B_ps[6]
            for ci in range(S // CH):
                c0 = ci * CH

                def ld(dst, r0, halo):
                    if halo:
                        if ci == 0:
                            return [("m", dst[:, :, 0:16]), ("d", dst[:, :, 16:W_], aT[r0:r0 + 256, 0:CH])]
                        return [("d", dst[:, :, :], aT[r0:r0 + 256, c0 - 16:c0 + CH])]
                    return [("d", dst[:, :, :], aT[r0:r0 + 256, c0:c0 + CH])]

                for (dst, B_d, r0, halo) in ((ain, B_ain, 0, True), (ac, B_ac, 256, True), (ab, B_ab, 512, False), (pv, B_pv, 768, True)):
                    for op in ld(dst, r0, halo):
                        if op[0] == "m":
                            MSET("pool", op[1], 0.0, (B_d,))
                        else:
                            DMA("sp", op[1], op[2].rearrange("(c p) t -> p c t", p=128), (B_z,), (B_d,))
                TT("pool", hh, ain, ac, ALU.mult, (B_ain, B_ac), (B_hh,))
                for c in range(2):
                    TS("dve", yy[:, c, 0:CH], hh[:, c, 16:W_], cw[:, c, 2:3], None, ALU.mult, None, (B_hh, B_layerc), (B_yy,))
                    STT(yy[:, c, 0:CH], hh[:, c, 15:W_ - 1], cw[:, c, 1:2], yy[:, c, 0:CH], ALU.mult, ALU.add, (B_hh, B_layerc, B_yy), (B_yy,))
                    STT(yy[:, c, 0:CH], hh[:, c, 14:W_ - 2], cw[:, c, 0:1], yy[:, c, 0:CH], ALU.mult, ALU.add, (B_hh, B_layerc, B_yy), (B_yy,))
                TT("pool", ya, yy[:, :, 0:CH], ab, ALU.mult, (B_yy, B_ab), (B_ya,))
                DMA("sp", mixT[0:256, c0:c0 + CH].rearrange("(c p) t -> p c t", p=128), ya, (B_ya,), (B_mix,))
                TT("pool", T1[:, :, 1:W_], pv[:, :, 1:W_], pv[:, :, 0:W_ - 1], ALU.add, (B_pv,), (B_T1,))
                TT("pool", T2[:, :, 3:W_], T1[:, :, 3:W_], T1[:, :, 1:W_ - 2], ALU.add, (B_T1,), (B_T2,))
                TT("pool", T3[:, 1, 7:W_], T2[:, 1, 7:W_], T2[:, 1, 3:W_ - 4], ALU.add, (B_T2,), (B_T3,))
                TT("pool", T4[64:128, 1, 15:W_], T3[64:128, 1, 15:W_], T3[64:128, 1, 7:W_ - 8], ALU.add, (B_T3,), (B_T4,))
                grp = ((T1, B_T1, 0, 64, 0, 0.5), (T2, B_T2, 64, 128, 0, 0.25), (T3, B_T3, 0, 64, 1, 0.125), (T4, B_T4, 64, 128, 1, 0.0625))
                for (Tg, B_Tg, p0, p1, c, iw_) in grp:
                    STT(dd[p0:p1, c, :], Tg[p0:p1, c, 16:W_], iw_, pv[p0:p1, c, 16:W_], ALU.mult, ALU.subtract, (B_Tg, B_pv), (B_dd,))
                    if ci == 0:
                        TT("dve", t16[p0:p1, c, :], Tg[p0:p1, c, 16:32], invc[p0:p1, c, :], ALU.mult, (B_Tg, B_const), (B_t16,))
                        TT("dve", dd[p0:p1, c, 0:16], t16[p0:p1, c, :], pv[p0:p1, c, 16:32], ALU.subtract, (B_t16, B_pv), (B_dd,))
                for c in range(2):
                    MM(pcv[:, c * CH:(c + 1) * CH], pwblk[:, c, :], dd[:, c, :], c == 0, c == 1, (B_layerc, B_dd, B_const), (B_pcv,))
                for c in range(2):
                    ACT(yb[:, c, :], pcv[:, c * CH:(c + 1) * CH], AF.Copy, (B_pcv, B_layerc), (B_yb,), scale=pscale[:, c:c + 1])
                DMA("sp", mixT[256:512, c0:c0 + CH].rearrange("(c p) t -> p c t", p=128), yb, (B_yb,), (B_mix,))

            if stop == "B1a0_cp":
                return
            barrier()
            AR.off = mark
            iqj = [AR.get(f"iqj{i}", [128, 8, 128], BF16) for i in range(2)]
            cqj = [AR.get(f"cqj{i}", [128, 4, 128], BF16) for i in range(2)]
            dqj = [AR.get(f"dqj{i}", [128, 4, 128], BF16) for i in range(2)]
            for lst in (iqj, cqj, dqj):
                for (ap_, b_) in lst:
                    MSET("pool", ap_, 0.0, (b_,))
            iwj = [AR.get(f"iwj{i}", [128, 8], F32) for i in range(2)]
            rsb = [AR.get(f"rsb{i}", [128, 512], F32) for i in range(2)]
            PTs = [AR.get(f"PT{i}", [128, 512], BF16) for i in range(3)]
            rden, B_rden = AR.get("rden", [64, 512], F32)
            outb = [AR.get(f"outb{i}", [64, 4, 128], BF16) for i in range(2)]
            st_, B_st = AR.get("tk", [128, 64], F32)
            Gt, B_G = AR.get("G", [128, 4, 16], F32)
            top8, B_t8 = AR.get("top8", [128, 4, 8], F32)
            mbias, B_mb = AR.get("mbias", [128, 4, 16], BF16)
            ptc = [0]

            def attn_tile(kS, B_k, vS, B_v, qj, B_q, stl, head0, j, O, B_O, Dn, B_Dn, Lrot, first, lastt, extra_ops):
                L, B_L = Lrot.get()
                ops = []
                for h_ in range(4):
                    ops.append((h_ * 128, (h_ + 1) * 128, kS[:, h_ // 2, stl * 128:(stl + 1) * 128], qj[:, h_, :], (B_k, B_q)))
                ops.extend(extra_ops(stl))
                for dlt in (0, 1):
                    if stl == j - dlt:
                        for h_ in range(4):
                            ops.append((h_ * 128, (h_ + 1) * 128, Tb[:, head0 + h_, dlt, :], ident[:], (B_const,)))
                firstw = {}
                lastw = {}
                for oi, op in enumerate(ops):
                    for r_ in range(op[0] // 128, op[1] // 128):
                        firstw.setdefault(r_, oi)
                        lastw[r_] = oi
                for oi, op in enumerate(ops):
                    MM(L[:, op[0]:op[1]], op[2], op[3], oi == 0, oi == len(ops) - 1, op[4], (B_L,))
                PT, B_PT = PTs[ptc[0] % 3]
                ptc[0] += 1
                ACT(PT, L[:, :], AF.Exp, (B_L,), (B_PT,))
                for h_ in range(4):
                    MM(O[0:64, h_ * 128:(h_ + 1) * 128], vS[:, stl, h_ * 64:(h_ + 1) * 64], PT[:, h_ * 128:(h_ + 1) * 128],
                       first and h_ == 0, lastt and h_ == 3, (B_v, B_PT), (B_O,))
                MM(Dn[0:64, :], ones64[:, :], PT, first, lastt, (B_const, B_PT), (B_Dn,))

            def finish(O, B_O, Dn, B_Dn, ob, B_ob, row0, j):
                P.emit("dve", lambda h: h.reciprocal(out=rden, in_=Dn[0:64, :]), (B_Dn,), (B_rden,))
                TT("dve", ob.rearrange("p a b -> p (a b)"), O[0:64, :], rden, ALU.mult, (B_O, B_rden), (B_ob,))
                DMA("sp", mixT[row0:row0 + 256, j * 128:(j + 1) * 128].rearrange("(h d) q -> d h q", d=64), ob, (B_ob,), (B_mix,))

            Lrot = Rot([0, 1])
            Srot = Rot([4, 5])
            for j in range(NT):
                if stop is not None and stop.startswith("B1a0_j") and j > int(stop[6:7]):
                    return
                jj = j % 2
                qcols = slice(j * 128, (j + 1) * 128)
                iq_, B_iq = iqj[jj]
                cq_, B_cq = cqj[jj]
                dq_, B_dq = dqj[jj]
                iw_, B_iw = iwj[jj]
                for e_ in range(2):
                    pr_ = slice(e_ * 64, (e_ + 1) * 64)
                    for (dst_, Bd_, src_) in ((iq_, B_iq, iqT), (cq_, B_cq, cqT), (dq_, B_dq, dqT)):
                        DMA("sp", dst_.rearrange("p (hh e) q -> p hh e q", e=2)[pr_, :, e_, :],
                            src_[:, qcols].rearrange("(hh e d) q -> e d hh q", e=2, d=64)[e_], (B_z,), (Bd_,))
                DMA("sp", iw_, iwd[qcols, :], (B_z,), (B_iw,))
                ns = j + 1
                nv = ns * 128
                n512 = (ns + 3) // 4
                ri = 0
                for sc_ in range(n512):
                    wdt = min(512, nv - sc_ * 512)
                    for h_ in range(8):
                        pS, B_pS = Srot.get()
                        MM(pS[:, 0:wdt], iq_[:, h_, :], ikS[:, sc_ * 512:sc_ * 512 + wdt], True, True, (B_iq, B_ik), (B_pS,))
                        r_, B_r = rsb[ri % 2]
                        ri += 1
                        ACT(r_[:, 0:wdt], pS[:, 0:wdt], AF.Relu, (B_pS,), (B_r,))
                        dst = scores[:, sc_ * 512:sc_ * 512 + wdt]
                        if h_ == 0:
                            TS("dve", dst, r_[:, 0:wdt], iw_[:, 0:1], None, ALU.mult, None, (B_r, B_iw), (B_sc,))
                        else:
                            STT(dst, r_[:, 0:wdt], iw_[:, h_:h_ + 1], dst, ALU.mult, ALU.add, (B_r, B_iw, B_sc), (B_sc,))
                if stop == "B1a0_j0i":
                    return
                sv = scores[:, 0:nv]
                RED(st_[:, 0:1], sv, ALU.min, (B_sc,), (B_st,))
                RED(st_[:, 1:2], sv, ALU.max, (B_sc,), (B_st,))
                TT("dve", scores[:, j * 128:(j + 1) * 128], scores[:, j * 128:(j + 1) * 128], trineg, ALU.add, (B_sc, B_const), (B_sc,))
                TS("dve", st_[:, 2:3], st_[:, 1:2], st_[:, 0:1], 0.02, ALU.subtract, ALU.add, (B_st,), (B_st,))
                STT(st_[:, 3:4], st_[:, 2:3], 0.5, st_[:, 0:1], ALU.mult, ALU.add, (B_st,), (B_st,))
                TS("dve", st_[:, 3:4], st_[:, 3:4], -0.01, None, ALU.add, None, (B_st,), (B_st,))
                TS("dve", st_[:, 16:32], c1, st_[:, 2:3], None, ALU.mult, None, (B_st, B_const), (B_st,))
                TS("dve", st_[:, 32:48], c2, st_[:, 2:3], None, ALU.mult, None, (B_st, B_const), (B_st,))
                for it in range(nbis):
                    TS("dve", maskb[:, 0:nv], sv, st_[:, 3:4], 0.0, ALU.is_ge, ALU.add, (B_sc, B_st), (B_mk, B_st), accum=st_[:, 4:5])
                    STT(st_[:, 5:6], st_[:, 4:5], KTOP - 0.5, st_[:, 32 + it:33 + it], ALU.is_ge, ALU.mult, (B_st,), (B_st,))
                    STT(st_[:, 3:4], st_[:, 3:4], st_[:, 16 + it:17 + it], st_[:, 5:6], ALU.subtract, ALU.add, (B_st,), (B_st,))
                TS("dve", maskb[:, 0:nv], sv, st_[:, 3:4], NEG, ALU.is_lt, ALU.mult, (B_sc, B_st), (B_mk,))

                if stop == "B1a0_j0t":
                    return
                def dsa_mask(stl):
                    return [(0, 512, maskb[:, stl * 128:(stl + 1) * 128], I4[:, :], (B_mk, B_const))]

                O, B_O = ps[2], B_ps[2]
                Dn, B_Dn = ps[3], B_ps[3]
                for stl in range(ns):
                    attn_tile(ckS, B_ck, cvS, B_cv, cq_, B_cq, stl, 0, j, O, B_O, Dn, B_Dn, Lrot, stl == 0, stl == ns - 1, dsa_mask)
                ob, B_ob = outb[0]
                finish(O, B_O, Dn, B_Dn, ob, B_ob, 512, j)

                if stop == "B1a0_j0a":
                    return
                obk = j // 2
                if obk > 0:
                    pG, B_pG = Srot.get()
                    for h_ in range(4):
                        MM(pG[:, h_ * 16:(h_ + 1) * 16], dq_[:, h_, :], kmT[:, h_ // 2, :], h_ == 0, h_ == 3, (B_dq, B_km), (B_pG,))
                    MSET("dve", Gt[:], -1e9, (B_G,))
                    CP("dve", Gt[:, :, 0:obk], pG[:, 0:64].rearrange("p (h n) -> p h n", h=4, n=16)[:, :, 0:obk], (B_pG,), (B_G,))
                    for h_ in range(4):
                        P.emit("dve", (lambda hh_: (lambda h: h.max(out=top8[:, hh_, :], in_=Gt[:, hh_, :])))(h_), (B_G,), (B_t8,))
                        TS("dve", top8[:, h_, 2:3], top8[:, h_, 2:3], -1e8, None, ALU.max, None, (B_t8,), (B_t8,))
                        TS("dve", mbias[:, h_, :], Gt[:, h_, :], top8[:, h_, 2:3], NEG, ALU.is_lt, ALU.mult, (B_G, B_t8), (B_mb,))

                def moba_mask(stl, obk=obk):
                    n = stl // 2
                    r_ = []
                    if n < obk:
                        for h_ in range(4):
                            r_.append((h_ * 128, (h_ + 1) * 128, mbias[:, h_, n:n + 1].to_broadcast([128, 128]), ident[:], (B_mb, B_const)))
                    return r_

                O, B_O = ps[6], B_ps[6]
                Dn, B_Dn = ps[7], B_ps[7]
                for stl in range(ns):
                    attn_tile(dkS, B_dk, dvS, B_dv, dq_, B_dq, stl, 4, j, O, B_O, Dn, B_Dn, Lrot, stl == 0, stl == ns - 1, moba_mask)
                ob, B_ob = outb[1]
                finish(O, B_O, Dn, B_Dn, ob, B_ob, 768, j)

        run_A0()
        barrier()
        if stop != "A0":
            for l in range(nlayers):
                run_B1a(l)
                barrier()
                if stop is not None and stop.startswith("B1a0"):
                    break
                try:
                    run_dense(l)
                except _Stop:
                    pass
                barrier()
                if stop is not None and stop.startswith("dense0"):
                    break
        if stop is not None:
            AR.reset()
            t_, B_t = AR.get("dbg", [128, 64], F32)
            MSET("dve", t_, 0.0, (B_t,))
            DMA("sp", y_out[0:128, 0:64], t_, (B_t,), ())
        P.generate(nc, block, sems)
    return nc, P


def _rel_bucket(n):
    n = np.maximum(n, 0)
    nf = np.maximum(n, 1).astype(np.float32)
    large = 16 + (np.log(nf / np.float32(16)) / np.float32(np.log(128 / 16)) * np.float32(16)).astype(np.int32)
    large = np.minimum(large, 31)
    return np.where(n < 16, n, large)


def host_inputs(inputs, nlayers=DEPTH):
    f = lambda a: np.ascontiguousarray(np.asarray(a, dtype=np.float32))
    rel_bias = f(inputs["rel_bias"])
    q = np.arange(128)[:, None]
    s = np.arange(128)[None, :]
    relT = np.zeros((128, 16, 128), np.float32)
    for h in range(8):
        for d in range(2):
            relT[:, h * 2 + d, :] = rel_bias[h][_rel_bucket(d * 128 + q - s)]
    relb = np.ascontiguousarray(np.broadcast_to(rel_bias.reshape(1, 256), (128, 256)))
    consts = np.zeros((128, 512), np.float32)
    consts[:, 0:128] = np.eye(128)
    consts[:, 128:256] = np.where(s > q, NEG, 0.0)
    consts[:, 256:384] = np.where(s > q, -1e9, 0.0)
    K = NBIS
    c1 = np.array([2.0 ** -(i + 2) for i in range(K - 1)] + [2.0 ** -K])
    c2 = np.array([2.0 ** -(i + 1) for i in range(K - 1)] + [2.0 ** -K])
    consts[:, 384:384 + K] = c1
    consts[:, 400:400 + K] = c2
    wins = {(0, 0): 2, (1, 0): 4, (0, 1): 8, (1, 1): 16}
    for ph in range(2):
        for c in range(2):
            w = wins[(ph, c)]
            for t in range(16):
                consts[ph * 64:(ph + 1) * 64, 416 + c * 16 + t] = 1.0 / min(t + 1, w)
    g = lambda k: f(inputs[k])
    gpack = np.zeros((DEPTH + 1, 5, D), np.float32)
    for l in range(DEPTH):
        gpack[l + 1, 0] = g("g_mix_post")[l]
        gpack[l + 1, 1] = g("g_ffn_pre")[l]
        gpack[l + 1, 2] = g("g_ffn_post")[l]
        gpack[l + 1, 3] = g("g_ple")[l]
        gpack[l, 4] = g("g_mix_pre")[l]
    shared = dict(
        w_in=g("w_in")[:nlayers], w_out=g("w_out")[:nlayers], w_gate_up=g("w_gate_up")[:nlayers], w_down=g("w_down")[:nlayers],
        w_ple_gate=g("w_ple_gate")[:nlayers], w_ple_proj=g("w_ple_proj")[:nlayers], gpack=gpack,
        convw_t=np.ascontiguousarray(g("conv_w").transpose(0, 2, 1)),
        pool_w=g("pool_w"),
        pscale_t=np.ascontiguousarray(g("pool_scale").reshape(DEPTH, 2, 128).transpose(0, 2, 1)),
        relT=relT, relb=relb, consts=consts,
    )
    x = g("x")
    p = g("p")
    maps = []
    for c in range(8):
        b = c % 4
        m = dict(shared)
        m["x"] = np.ascontiguousarray(x[b])
        m["p"] = np.ascontiguousarray(p[:nlayers, b])
        maps.append(m)
    return maps


_CACHE = {}


def kernel(**inputs):
    if "nc" not in _CACHE:
        _CACHE["nc"] = build()[0]
    nc = _CACHE["nc"]
    maps = host_inputs(inputs)
    res = run_bass_kernel_spmd(nc, maps, core_ids=list(range(8)))
    out = np.stack([np.asarray(res.results[b]["y"], dtype=np.float32) for b in range(4)], axis=0)
    return out
```

```python
import numpy as np
import concourse.bass as bass
import concourse.mybir as mybir
from concourse.bass_utils import run_bass_kernel_spmd

F32, BF16 = mybir.dt.float32, mybir.dt.bfloat16
ALU, AF, AX = mybir.AluOpType, mybir.ActivationFunctionType, mybir.AxisListType

S = 4096
D = 1024
NT = 32
NG = 8
DEPTH = 4
DFF = 2816
INC = 3144
NEG = -30000.0
KTOP = 256
NBIS = 12
ARENA_BYTES = 132 * 1024


class _Stop(Exception):
    pass


class Buf:
    __slots__ = ("name", "w", "r")

    def __init__(self, name):
        self.name = name
        self.w = None
        self.r = {}


class Ins:
    __slots__ = ("eng", "fn", "deps", "sig", "ms", "dma", "slot")


ENGS = ("pe", "act", "dve", "pool", "sp", "poolw")


KSEM = 14
DMAQ = ("sp", "pool", "poolw")


class Prog:
    def __init__(self):
        self.q = {e: [] for e in ENGS}
        self.extra = {e: set() for e in ENGS}
        self.lastdma = {}
        self.ndma = {e: 0 for e in ENGS}
        self.n = 0

    def emit(self, eng, fn, reads=(), writes=(), dma=False):
        ins = Ins()
        ins.eng = eng
        ins.fn = fn
        ins.sig = False
        ins.ms = None
        ins.dma = dma
        ins.slot = None
        deps = set(self.extra[eng])
        self.extra[eng] = set()
        for b in reads:
            if b.w is not None:
                deps.add(b.w)
        for b in writes:
            if b.w is not None:
                deps.add(b.w)
            deps.update(b.r.values())
        if dma:
            i = self.ndma[eng]
            self.ndma[eng] = i + 1
            ins.slot = i % KSEM
            ins.ms = 16 * (i // KSEM + 1)
            prev = self.lastdma.get((eng, ins.slot))
            if prev is not None:
                deps.add(prev)
            self.lastdma[(eng, ins.slot)] = ins
        fd = []
        for d in deps:
            if d is ins:
                continue
            if (not d.dma) and d.eng == eng and eng == "pe":
                continue
            d.sig = True
            fd.append(d)
        ins.deps = fd
        for b in reads:
            b.r[(eng, dma, ins.slot)] = ins
        for b in writes:
            b.w = ins
            b.r = {}
        self.q[eng].append(ins)
        self.n += 1
        return ins

    def generate(self, nc, block, sems):
        for e in ENGS:
            c = 0
            for ins in self.q[e]:
                if (not ins.dma) and ins.sig:
                    c += 1
                    ins.ms = c
        final = {}
        for (e, slot), ins in self.lastdma.items():
            if e != "poolw":
                final[("d", e, slot)] = ins.ms

        def run(e, h, waited):
            for ins in self.q[e]:
                need = {}
                for d in ins.deps:
                    key = ("d", d.eng, d.slot) if d.dma else ("c", d.eng)
                    if need.get(key, 0) < d.ms:
                        need[key] = d.ms
                for key, v in need.items():
                    if waited.get(key, 0) < v:
                        h.wait_ge(sems[key], v)
                        waited[key] = v
                r = ins.fn(h)
                if ins.dma:
                    r.then_inc(sems[("d", e, ins.slot)], 16)
                elif ins.sig:
                    r.then_inc(sems[("c", e)], 1)
            if e == "sp":
                for key, v in final.items():
                    if waited.get(key, 0) < v:
                        h.wait_ge(sems[key], v)

        @block.tensor
        def _(h):
            run("pe", h, {})

        @block.scalar
        def _(h):
            run("act", h, {})

        @block.vector
        def _(h):
            run("dve", h, {})

        @block.gpsimd
        def _(h):
            w = {}
            run("poolw", h, w)
            run("pool", h, w)

        @block.sync
        def _(h):
            run("sp", h, {})


def build(nlayers=DEPTH, stop=None, debug=False, nbis=NBIS):
    nc = bass.Bass("TRN2", target_bir_lowering=False)
    P = Prog()
    skind = "ExternalOutput" if debug else "Internal"

    def din(name, shape, dt=F32):
        return nc.dram_tensor(name, list(shape), dt, kind="ExternalInput").ap()

    def dscr(name, shape, dt):
        return nc.dram_tensor(name, list(shape), dt, kind=skind).ap()

    x_in = din("x", [S, D])
    p_in = din("p", [nlayers, S, 256])
    w_in_d = din("w_in", [nlayers, D, INC])
    w_out_d = din("w_out", [nlayers, D, D])
    w_gu_d = din("w_gate_up", [nlayers, D, 2 * DFF])
    w_dn_d = din("w_down", [nlayers, DFF, D])
    w_pg_d = din("w_ple_gate", [nlayers, D, D])
    w_pp_d = din("w_ple_proj", [nlayers, 256, D])
    gpack_d = din("gpack", [DEPTH + 1, 5, D])
    convw_d = din("convw_t", [DEPTH, 256, 3])
    poolw_d = din("pool_w", [DEPTH, 4, 64, 64])
    pscale_d = din("pscale_t", [DEPTH, 128, 2])
    relT_d = din("relT", [128, 16, 128])
    relb_d = din("relb", [128, 256])
    consts_d = din("consts", [128, 512])
    y_out = nc.dram_tensor("y", [S, D], F32, kind="ExternalOutput").ap()

    wb = {}
    for l in range(nlayers):
        wb[l] = dict(
            w_in=nc.dram_tensor(f"wb_in{l}", [D, INC], BF16, kind="Internal").ap(),
            w_out=nc.dram_tensor(f"wb_out{l}", [D, D], BF16, kind="Internal").ap(),
            w_gu=nc.dram_tensor(f"wb_gu{l}", [D, 2 * DFF], BF16, kind="Internal").ap(),
            w_dn=nc.dram_tensor(f"wb_dn{l}", [DFF, D], BF16, kind="Internal").ap(),
            w_pg=nc.dram_tensor(f"wb_pg{l}", [D, D], BF16, kind="Internal").ap(),
            w_pp=nc.dram_tensor(f"wb_pp{l}", [256, D], BF16, kind="Internal").ap(),
        )
    wsrc = dict(w_in=w_in_d, w_out=w_out_d, w_gu=w_gu_d, w_dn=w_dn_d, w_pg=w_pg_d, w_pp=w_pp_d)
    wbufs = {}

    x_cur = dscr("x_cur", [S, D], F32)
    aT = dscr("aT", [1024, S], F32)
    cqT = dscr("cqT", [256, S], BF16)
    dqT = dscr("dqT", [256, S], BF16)
    iqT = dscr("iqT", [512, S], BF16)
    ckT = dscr("ckT", [256, S], BF16)
    dkT = dscr("dkT", [256, S], BF16)
    ikT = dscr("ikT", [64, S], BF16)
    cvd = dscr("cv", [S, 256], BF16)
    dvd = dscr("dv", [S, 256], BF16)
    iwd = dscr("iw", [S, 8], F32)
    mixT = dscr("mixT", [1024, S], BF16)
    B_z = Buf("zscratch")
    B_mix = Buf("mixT")
    B_xcur = [Buf(f"xcur{g}") for g in range(NG)]

    import contextlib
    es = contextlib.ExitStack()
    with es:
        def sb(name, shape, dt):
            return es.enter_context(nc.sbuf_tensor(name, list(shape), dt))

        arena = sb("arena", [128, ARENA_BYTES // 2], BF16)
        consts = sb("consts_sb", [128, 512], F32)
        ident = sb("ident_bf", [128, 128], BF16)
        I4 = sb("I4", [128, 512], BF16)
        ones64 = sb("ones64", [128, 64], BF16)
        Tb = sb("Tb", [128, 8, 2, 128], BF16)
        relb = sb("relb_sb", [128, 256], F32)
        pwblk = sb("pwblk", [128, 2, 128], BF16)
        cw = sb("cw", [128, 2, 3], F32)
        pscale = sb("pscale", [128, 2], F32)
        small = sb("small", [128, 256], F32)
        bar_s = sb("bar_s", [128, 8], F32)
        WBt = [sb(f"WB{i}", [128, 8, 512], BF16) for i in range(4)]
        B_WB = [Buf(f"WB{i}") for i in range(4)]
        ps = [es.enter_context(nc.psum_tensor(f"ps{i}", [128, 512], F32)) for i in range(8)]
        B_ps = [Buf(f"ps{i}") for i in range(8)]
        B_const = Buf("const")
        B_layerc = Buf("layerconst")
        B_small = Buf("small")
        B_bar = {e: Buf("bar" + e) for e in ENGS}
        B_sm = {}
        B_msbs = [Buf(f"msb{t}") for t in range(4)]

        sem_names = [("c", "pe"), ("c", "act"), ("c", "dve"), ("c", "pool")]
        sem_names += [("d", q_, i_) for q_ in DMAQ for i_ in range(KSEM)]
        sems = {k: es.enter_context(nc.semaphore("s_" + "_".join(str(x_) for x_ in k))) for k in sem_names}
        block = es.enter_context(nc.Block())

        tri_f = consts[:, 128:256]
        trineg = consts[:, 256:384]
        c1 = consts[:, 384:400]
        c2 = consts[:, 400:416]
        invc = consts[:, 416:448].rearrange("p (c t) -> p c t", c=2, t=16)

        class Arena:
            def __init__(self):
                self.off = 0

            def reset(self):
                self.off = 0

            def get(self, name, shape, dt):
                nel = 1
                for s_ in shape[1:]:
                    nel *= s_
                nbytes = nel * (4 if dt == F32 else 2)
                nbytes = (nbytes + 63) // 64 * 64
                assert self.off + nbytes <= ARENA_BYTES, (name, self.off, nbytes)
                ap = arena[:, self.off // 2:(self.off + nbytes) // 2]
                if dt == F32:
                    ap = ap.bitcast(F32)
                ap = ap[0:shape[0], 0:nel]
                if len(shape) == 3:
                    ap = ap.rearrange("p (a b) -> p a b", a=shape[1], b=shape[2])
                elif len(shape) == 4:
                    ap = ap.rearrange("p (a b c) -> p a b c", a=shape[1], b=shape[2], c=shape[3])
                self.off += nbytes
                return ap, Buf(name)

        AR = Arena()

        def MM(out, lhsT, rhs, st, sp_, R, W):
            P.emit("pe", lambda h: h.matmul(out, lhsT=lhsT, rhs=rhs, start=st, stop=sp_), R, W)

        def TRP(out, in_, R, W):
            P.emit("pe", lambda h: h.transpose(out, in_, ident[:]), R, W)

        def ACT(out, in_, func, R, W, scale=None, bias=None, accum=None):
            kw = {}
            if scale is not None:
                kw["scale"] = scale
            if bias is not None:
                kw["bias"] = bias
            if accum is not None:
                kw["accum_out"] = accum
            P.emit("act", lambda h: h.activation(out=out, in_=in_, func=func, **kw), R, W)

        def TS(eng, out, in0, s1, s2, op0, op1, R, W, accum=None):
            kw = {}
            if op1 is not None:
                kw["op1"] = op1
            if accum is not None:
                kw["accum_out"] = accum
            P.emit(eng, lambda h: h.tensor_scalar(out=out, in0=in0, scalar1=s1, scalar2=s2, op0=op0, **kw), R, W)

        def TT(eng, out, in0, in1, op, R, W):
            P.emit(eng, lambda h: h.tensor_tensor(out=out, in0=in0, in1=in1, op=op), R, W)

        def STT(out, in0, scalar, in1, op0, op1, R, W):
            P.emit("dve", lambda h: h.scalar_tensor_tensor(out=out, in0=in0, scalar=scalar, in1=in1, op0=op0, op1=op1), R, W)

        def CP(eng, out, in_, R, W):
            P.emit(eng, lambda h: h.tensor_copy(out, in_), R, W)

        def MSET(eng, ap, val, W):
            P.emit(eng, lambda h: h.memset(ap, val), (), W)

        def RED(out, in_, op, R, W):
            P.emit("dve", lambda h: h.tensor_reduce(out=out, in_=in_, axis=AX.X, op=op), R, W)

        def DMA(q, out, in_, R, W):
            return P.emit(q, lambda h: h.dma_start(out=out, in_=in_), R, W, dma=True)

        def barrier():
            t = []
            t.append(P.emit("pe", lambda h: h.matmul(ps[5][0:1, 0:1], lhsT=ones64[0:1, 0:1], rhs=ones64[0:1, 0:1], start=True, stop=True),
                            (B_const,), (B_ps[5], B_bar["pe"])))
            t.append(P.emit("act", lambda h: h.activation(out=bar_s[0:1, 0:1], in_=bar_s[0:1, 4:5], func=AF.Copy), (B_const,), (B_bar["act"],)))
            t.append(P.emit("dve", lambda h: h.tensor_copy(bar_s[0:1, 1:2], bar_s[0:1, 5:6]), (B_const,), (B_bar["dve"],)))
            t.append(P.emit("pool", lambda h: h.tensor_copy(bar_s[0:1, 2:3], bar_s[0:1, 6:7]), (B_const,), (B_bar["pool"],)))
            for i in t:
                i.sig = True
            allb = set(t) | set(v for k, v in P.lastdma.items() if k[0] != "poolw")
            for e in ENGS:
                if e != "poolw":
                    P.extra[e] |= allb

        DMA("sp", consts[:], consts_d, (), (B_const,))
        DMA("sp", relb[:], relb_d, (), (B_const,))
        DMA("pool", ident[:], consts_d[:, 0:128], (), (B_const,))
        for i in range(4):
            DMA("pool", I4[:, i * 128:(i + 1) * 128], consts_d[:, 0:128], (), (B_const,))
        MSET("pool", ones64[:], 1.0, (B_const,))
        MSET("pool", pwblk[:], 0.0, (B_const,))
        MSET("pool", bar_s[:], 0.0, (B_const,))
        AR.reset()
        relT_sb, B_relT = AR.get("relT", [128, 16, 128], F32)
        DMA("sp", relT_sb, relT_d, (), (B_relT,))
        for h_ in range(8):
            b31 = relb[:, h_ * 32 + 31:h_ * 32 + 32]
            STT(Tb[:, h_, 0, :], relT_sb[:, h_ * 2, :], b31, tri_f, ALU.subtract, ALU.add, (B_relT, B_const), (B_const,))
            TS("dve", Tb[:, h_, 1, :], relT_sb[:, h_ * 2 + 1, :], b31, None, ALU.subtract, None, (B_relT, B_const), (B_const,))

        barrier()
        for l in range(nlayers):
            for nm in ("w_in", "w_out", "w_gu", "w_dn", "w_pg", "w_pp"):
                src = wsrc[nm][l]
                dst = wb[l][nm]
                rows = src.shape[0]
                step = 256
                bl = []
                for r0 in range(0, rows, step):
                    r1 = min(rows, r0 + step)
                    b = Buf(f"wb{l}{nm}{r0}")
                    DMA("poolw", dst[r0:r1, :], src[r0:r1, :], (), (b,))
                    bl.append(b)
                wbufs[(l, nm)] = bl

        class WStream:
            def __init__(self):
                self.plan = []
                self.issued = 0
                self.taken = 0

            def add(self, l, nm, view_fn, shape):
                self.plan.append((l, nm, view_fn, shape))

            def _issue(self):
                i = self.issued
                if i >= len(self.plan):
                    return
                l, nm, view_fn, shape = self.plan[i]
                slot = i % 4
                dst = WBt[slot][:]
                for dstv, srcv in view_fn(dst, wb[l][nm]):
                    DMA("sp", dstv, srcv, tuple(wbufs[(l, nm)]), (B_WB[slot],))
                self.issued += 1

            def start(self):
                while self.issued < min(3, len(self.plan)):
                    self._issue()

            def get(self):
                i = self.taken
                assert i < self.issued, "weight stream underflow"
                slot = i % 4
                self.taken += 1
                self._issue()
                return WBt[slot], B_WB[slot]

        WS = WStream()

        def wv_cols(k0, nk, c0, c1):
            def f(dst, w):
                return [(dst[:, 0:nk, 0:c1 - c0],
                         w[k0 * 128:(k0 + nk) * 128, c0:c1].rearrange("(k p) c -> p k c", p=128))]
            return f

        def wv_gu(gb):
            def f(dst, w):
                w3 = w.rearrange("(k p) c -> p k c", p=128)
                return [(dst[:, :, 0:256], w3[:, :, gb * 256:(gb + 1) * 256]),
                        (dst[:, :, 256:512], w3[:, :, DFF + gb * 256:DFF + (gb + 1) * 256])]
            return f

        IN_BLOCKS = [(0, 512), (512, 1024), (1024, 1536), (1536, 2048), (2048, 2376), (2376, 2888), (2888, 3144)]

        def plan_A(l):
            for (c0, c1) in IN_BLOCKS:
                WS.add(l, "w_in", wv_cols(0, 8, c0, c1), None)

        def plan_dense(l, with_A):
            for c in range(2):
                WS.add(l, "w_out", wv_cols(0, 8, c * 512, (c + 1) * 512), None)
            for gb in range(11):
                WS.add(l, "w_gu", wv_gu(gb), None)
            for c in range(2):
                for (k0, nk) in ((0, 8), (8, 8), (16, 6)):
                    WS.add(l, "w_dn", wv_cols(k0, nk, c * 512, (c + 1) * 512), None)
            for c in range(2):
                WS.add(l, "w_pg", wv_cols(0, 8, c * 512, (c + 1) * 512), None)
            if with_A:
                plan_A(l + 1)

        for g in range(NG):
            plan_A(0)
        if stop != "A0":
            for l in range(nlayers):
                if stop is not None and stop.startswith("B1a0"):
                    break
                for g in range(NG):
                    plan_dense(l, l + 1 < nlayers)
                if stop is not None and stop.startswith("dense0"):
                    break
        WS.start()

        class Rot:
            def __init__(self, idx):
                self.idx = idx
                self.i = 0

            def get(self):
                k = self.idx[self.i % len(self.idx)]
                self.i += 1
                return ps[k], B_ps[k]

        def load_gains(pidx, gB, B_g):
            DMA("sp", gB.rearrange("p a b -> p (a b)"),
                gpack_d[pidx:pidx + 1].rearrange("o a b -> o (a b)").partition_broadcast(128), (), (B_g,))

        def rstd_from(ssq_ap, out_ap, n_inv, R, W):
            ACT(out_ap, ssq_ap, AF.Sqrt, R, W, scale=n_inv, bias=1e-6)
            P.emit("dve", lambda h: h.reciprocal(out=out_ap, in_=out_ap), W, W)

        def norm_T(xg, B_xg, gB, B_g, gslot, hT, B_hT, hb, B_hb, junk, B_junk, rotT, sc0):
            for t in range(4):
                ssq = small[:, sc0 + t:sc0 + t + 1]
                rs = small[:, sc0 + 4 + t:sc0 + 5 + t]
                B_s_ = B_sm.setdefault(("n", t), Buf(f"smn{t}"))
                ACT(junk[:, 0:1024], xg[:, t, :], AF.Square, (B_xg,), (B_junk, B_s_), accum=ssq)
                rstd_from(ssq, rs, 1.0 / D, (B_s_,), (B_s_,))
                hbt = hb[t % 2]
                STT(hbt, xg[:, t, :], rs, gB[:, gslot, :], ALU.mult, ALU.mult, (B_xg, B_s_, B_g), (B_hb[t % 2],))
                pt, B_pt = rotT.get()
                ptb = pt[:].bitcast(BF16)
                for k in range(8):
                    TRP(ptb[:, k * 128:(k + 1) * 128], hbt[:, k * 128:(k + 1) * 128], (B_hb[t % 2], B_const), (B_pt,))
                ACT(hT[:, :, t * 128:(t + 1) * 128], ptb.rearrange("p (k q) -> p k q", k=8, q=128), AF.Copy, (B_pt,), (B_hT,))

        def tm_postnorm(lhs_fn, B_lhs, nkc_list, gB, B_g, gslot, xg, B_xg, msb, B_msb, junk, B_junk, rotM, sc0):
            for c in range(2):
                accs = [rotM.get() for _ in range(4)]
                nblk = len(nkc_list)
                kbase = 0
                for bi, nk in enumerate(nkc_list):
                    wt, B_w = WS.get()
                    for t in range(4):
                        pa, B_pa = accs[t]
                        for k in range(nk):
                            MM(pa[:, :], lhs_fn(kbase + k, t), wt[:, k, :], (bi == 0 and k == 0), (bi == nblk - 1 and k == nk - 1),
                               (B_lhs, B_w), (B_pa,))
                    kbase += nk
                if stop == "dense0_1a":
                    raise _Stop()
                for t in range(4):
                    pa, B_pa = accs[t]
                    B_s_ = B_sm.setdefault(("p", t), Buf(f"smp{t}"))
                    CP("dve", msb[:, t, c * 512:(c + 1) * 512], pa[:, :], (B_pa,), (B_msbs[t],))
                    ACT(junk[:, 0:512], msb[:, t, c * 512:(c + 1) * 512], AF.Square, (B_msbs[t],), (B_junk, B_s_),
                        accum=small[:, sc0 + t * 2 + c:sc0 + t * 2 + c + 1])
                if stop == "dense0_1b":
                    raise _Stop()
            if stop == "dense0_1c":
                raise _Stop()
            for t in range(4):
                B_s_ = B_sm[("p", t)]
                tot = small[:, sc0 + 8 + t:sc0 + 9 + t]
                TT("dve", tot, small[:, sc0 + t * 2:sc0 + t * 2 + 1], small[:, sc0 + t * 2 + 1:sc0 + t * 2 + 2], ALU.add, (B_s_,), (B_s_,))
                rs = small[:, sc0 + 12 + t:sc0 + 13 + t]
                rstd_from(tot, rs, 1.0 / D, (B_s_,), (B_s_,))
                STT(msb[:, t, :], msb[:, t, :], rs, gB[:, gslot, :], ALU.mult, ALU.mult, (B_msbs[t], B_s_, B_g), (B_msbs[t],))
                TT("pool", xg[:, t, :], xg[:, t, :], msb[:, t, :], ALU.add, (B_xg, B_msbs[t]), (B_xg,))

        evac_flip = [0]

        def evac(out, in_, R, W, scale=None):
            evac_flip[0] ^= 1
            if evac_flip[0]:
                ACT(out, in_, AF.Copy, R, W, scale=scale)
            else:
                if scale is None:
                    CP("dve", out, in_, R, W)
                else:
                    TS("dve", out, in_, scale, None, ALU.mult, None, R, W)

        def phase_A(l, g, hT, B_hT, stf, B_stf, stb, B_stb, rotM):
            tok0 = g * 512
            cnt = [0]

            def fm(wt, B_w, cl0, cl1, dst_ap, scale=None, f32=False):
                M = cl1 - cl0
                pa, B_pa = rotM.get()
                for k in range(8):
                    MM(pa[0:M, :], wt[:, k, cl0:cl1], hT[:, k, :], k == 0, k == 7, (B_w, B_hT), (B_pa,))
                i = cnt[0] % 3
                cnt[0] += 1
                if f32:
                    st_, B_st = stf[i], B_stf[i]
                else:
                    st_, B_st = stb[i], B_stb[i]
                evac(st_[0:M, :], pa[0:M, :], (B_pa,), (B_st,), scale=scale)
                DMA("sp", dst_ap, st_[0:M, :], (B_st,), (B_z,))

            def tmm(wt, B_w, cl0, cl1, dst_fn, f32=False):
                ncol = cl1 - cl0
                for t in range(4):
                    pa, B_pa = rotM.get()
                    for k in range(8):
                        MM(pa[:, 0:ncol], hT[:, k, t * 128:(t + 1) * 128], wt[:, k, cl0:cl1], k == 0, k == 7, (B_w, B_hT), (B_pa,))
                    i = cnt[0] % 3
                    cnt[0] += 1
                    if f32:
                        st_, B_st = stf[i], B_stf[i]
                    else:
                        st_, B_st = stb[i], B_stb[i]
                    evac(st_[:, 0:ncol], pa[:, 0:ncol], (B_pa,), (B_st,))
                    DMA("sp", dst_fn(tok0 + t * 128), st_[:, 0:ncol], (B_st,), (B_z,))

            cols = slice(tok0, tok0 + 512)
            for bi in range(2):
                wt, B_w = WS.get()
                for j in range(4):
                    r0 = bi * 512 + j * 128
                    fm(wt, B_w, j * 128, (j + 1) * 128, aT[r0:r0 + 128, cols], f32=True)
            wt, B_w = WS.get()
            fm(wt, B_w, 0, 128, cqT[0:128, cols], scale=0.125)
            fm(wt, B_w, 128, 256, cqT[128:256, cols], scale=0.125)
            fm(wt, B_w, 256, 384, ckT[0:128, cols])
            fm(wt, B_w, 384, 512, ckT[128:256, cols])
            wt, B_w = WS.get()
            tmm(wt, B_w, 0, 256, lambda r: cvd[r:r + 128, :])
            fm(wt, B_w, 256, 384, iqT[0:128, cols])
            fm(wt, B_w, 384, 512, iqT[128:256, cols])
            wt, B_w = WS.get()
            fm(wt, B_w, 0, 128, iqT[256:384, cols])
            fm(wt, B_w, 128, 256, iqT[384:512, cols])
            fm(wt, B_w, 256, 320, ikT[0:64, cols])
            tmm(wt, B_w, 320, 328, lambda r: iwd[r:r + 128, :], f32=True)
            wt, B_w = WS.get()
            fm(wt, B_w, 0, 128, dqT[0:128, cols], scale=0.125)
            fm(wt, B_w, 128, 256, dqT[128:256, cols], scale=0.125)
            fm(wt, B_w, 256, 384, dkT[0:128, cols])
            fm(wt, B_w, 384, 512, dkT[128:256, cols])
            wt, B_w = WS.get()
            tmm(wt, B_w, 0, 256, lambda r: dvd[r:r + 128, :])

        def dense_arena():
            AR.reset()
            A = {}
            A["xg"] = AR.get("xg", [128, 4, 1024], F32)
            A["gB"] = AR.get("gB", [128, 5, 1024], F32)
            A["hb0"] = AR.get("hb0", [128, 1024], BF16)
            A["hb1"] = AR.get("hb1", [128, 1024], BF16)
            A["hT0"] = AR.get("hT0", [128, 8, 512], BF16)
            A["hT1"] = AR.get("hT1", [128, 8, 512], BF16)
            A["actT"] = AR.get("actT", [128, 22, 512], BF16)
            A["mixg"] = AR.get("mixg", [128, 8, 512], BF16)
            A["msb"] = AR.get("msb", [128, 4, 1024], F32)
            A["junk"] = AR.get("junk", [128, 1024], BF16)
            A["pg"] = AR.get("pg", [128, 4, 256], BF16)
            A["pT"] = AR.get("pT", [128, 2, 512], BF16)
            A["wpp"] = AR.get("wpp", [128, 2, 2, 512], BF16)
            A["sg0"] = AR.get("sg0", [128, 512], F32)
            A["sg1"] = AR.get("sg1", [128, 512], F32)
            for i in range(3):
                A[f"stf{i}"] = AR.get(f"stf{i}", [128, 512], F32)
                A[f"stb{i}"] = AR.get(f"stb{i}", [128, 512], BF16)
            return A

        def run_A0():
            A = dense_arena()
            xg, B_xg = A["xg"]
            gB, B_g = A["gB"]
            load_gains(0, gB, B_g)
            rotT = Rot([0, 1])
            rotM = Rot([2, 3, 4, 5, 6, 7])
            hTs = [A["hT0"], A["hT1"]]
            hb = [A["hb0"][0], A["hb1"][0]]
            B_hb = [A["hb0"][1], A["hb1"][1]]
            junk, B_junk = A["junk"]
            stf = [A[f"stf{i}"][0] for i in range(3)]
            B_stf = [A[f"stf{i}"][1] for i in range(3)]
            stb = [A[f"stb{i}"][0] for i in range(3)]
            B_stb = [A[f"stb{i}"][1] for i in range(3)]
            for g in range(NG):
                DMA("sp", xg, x_in[g * 512:(g + 1) * 512, :].rearrange("(t p) d -> p t d", p=128), (), (B_xg,))
                hT, B_hT = hTs[g % 2]
                norm_T(xg, B_xg, gB, B_g, 4, hT, B_hT, hb, B_hb, junk, B_junk, rotT, 0)
                phase_A(0, g, hT, B_hT, stf, B_stf, stb, B_stb, rotM)

        def run_dense(l):
            last = (l == DEPTH - 1)
            A = dense_arena()
            xg, B_xg = A["xg"]
            gB, B_g = A["gB"]
            load_gains(l + 1, gB, B_g)
            rotT = Rot([0, 1])
            rotM = Rot([2, 3, 4, 5, 6, 7])
            hTs = [A["hT0"], A["hT1"]]
            hb = [A["hb0"][0], A["hb1"][0]]
            B_hb = [A["hb0"][1], A["hb1"][1]]
            junk, B_junk = A["junk"]
            actT, B_actT = A["actT"]
            mixg, B_mixg = A["mixg"]
            msb, B_msb = A["msb"]
            pg, B_pg = A["pg"]
            pT, B_pT = A["pT"]
            sg = [A["sg0"][0], A["sg1"][0]]
            B_sg = [A["sg0"][1], A["sg1"][1]]
            stf = [A[f"stf{i}"][0] for i in range(3)]
            B_stf = [A[f"stf{i}"][1] for i in range(3)]
            stb = [A[f"stb{i}"][0] for i in range(3)]
            B_stb = [A[f"stb{i}"][1] for i in range(3)]
            xsrc = x_in if l == 0 else x_cur
            xdst = y_out if last else x_cur
            for g in range(NG):
                rows = slice(g * 512, (g + 1) * 512)
                rd = () if l == 0 else (B_xcur[g],)
                DMA("sp", xg, xsrc[rows, :].rearrange("(t p) d -> p t d", p=128), rd, (B_xg,))
                if stop == "dense0_0a":
                    return
                DMA("sp", mixg, mixT[:, rows].rearrange("(k p) s -> p k s", p=128), (B_mix,), (B_mixg,))
                if stop == "dense0_0b":
                    return
                DMA("pool", pg, p_in[l, rows, :].rearrange("(t p) c -> p t c", p=128), (), (B_pg,))
                if stop == "dense0_0":
                    return
                tm_postnorm(lambda k, t: mixg[:, k, t * 128:(t + 1) * 128], B_mixg, [8], gB, B_g, 0, xg, B_xg, msb, B_msb,
                            junk, B_junk, rotM, 16)
                if stop == 'dense0_1':
                    return
                hT, B_hT = hTs[0]
                norm_T(xg, B_xg, gB, B_g, 1, hT, B_hT, hb, B_hb, junk, B_junk, rotT, 0)
                if stop == 'dense0_2':
                    return
                for gb in range(11):
                    wt, B_w = WS.get()
                    for sc in range(2):
                        pg_, B_pg_ = rotM.get()
                        pu_, B_pu_ = rotM.get()
                        for k in range(8):
                            MM(pg_[:, :], wt[:, k, sc * 128:(sc + 1) * 128], hT[:, k, :], k == 0, k == 7, (B_w, B_hT), (B_pg_,))
                        for k in range(8):
                            MM(pu_[:, :], wt[:, k, 256 + sc * 128:256 + (sc + 1) * 128], hT[:, k, :], k == 0, k == 7, (B_w, B_hT), (B_pu_,))
                        ci = gb * 2 + sc
                        s_, B_s = sg[ci % 2], B_sg[ci % 2]
                        ACT(s_, pg_[:, :], AF.Silu, (B_pg_,), (B_s,))
                        TT("dve", actT[:, ci, :], s_, pu_[:, :], ALU.mult, (B_s, B_pu_), (B_actT,))
                if stop == 'dense0_3':
                    return
                tm_postnorm(lambda k, t: actT[:, k, t * 128:(t + 1) * 128], B_actT, [8, 8, 6], gB, B_g, 2, xg, B_xg, msb, B_msb,
                            junk, B_junk, rotM, 16)
                if stop == 'dense0_4':
                    return
                hT, B_hT = hTs[1]
                norm_T(xg, B_xg, gB, B_g, 3, hT, B_hT, hb, B_hb, junk, B_junk, rotT, 0)
                for t in range(4):
                    pt, B_pt = rotT.get()
                    ptb = pt[:].bitcast(BF16)
                    for k in range(2):
                        TRP(ptb[:, k * 128:(k + 1) * 128], pg[:, t, k * 128:(k + 1) * 128], (B_pg, B_const), (B_pt,))
                    CP("dve", pT[:, :, t * 128:(t + 1) * 128], ptb[:, 0:256].rearrange("p (k q) -> p k q", k=2, q=128), (B_pt,), (B_pT,))
                wpp, B_wpp = A["wpp"]
                for u_ in range(2):
                    DMA("sp", wpp[:, :, u_, :], wb[l]["w_pp"].rearrange("(k p) c -> p k c", p=128)[:, :, u_ * 512:(u_ + 1) * 512],
                        tuple(wbufs[(l, "w_pp")]), (B_wpp,))
                for c in range(2):
                    wt, B_w = WS.get()
                    for t in range(4):
                        pa, B_pa = rotM.get()
                        pb, B_pb = rotM.get()
                        for k in range(8):
                            MM(pa[:, :], hT[:, k, t * 128:(t + 1) * 128], wt[:, k, :], k == 0, k == 7, (B_hT, B_w), (B_pa,))
                        for k in range(2):
                            MM(pb[:, :], pT[:, k, t * 128:(t + 1) * 128], wpp[:, k, c, :], k == 0, k == 1, (B_pT, B_wpp), (B_pb,))
                        i = (c * 4 + t) % 2
                        ACT(sg[i], pa[:, :], AF.Sigmoid, (B_pa,), (B_sg[i],))
                        TT("dve", sg[i], sg[i], pb[:, :], ALU.mult, (B_sg[i], B_pb), (B_sg[i],))
                        TT("pool", xg[:, t, c * 512:(c + 1) * 512], xg[:, t, c * 512:(c + 1) * 512], sg[i], ALU.add, (B_xg, B_sg[i]), (B_xg,))
                if stop == 'dense0_5':
                    return
                DMA("sp", xdst[rows, :].rearrange("(t p) d -> p t d", p=128), xg, (B_xg,), () if last else (B_xcur[g],))
                if l + 1 < nlayers:
                    hT, B_hT = hTs[0]
                    norm_T(xg, B_xg, gB, B_g, 4, hT, B_hT, hb, B_hb, junk, B_junk, rotT, 0)
                    phase_A(l + 1, g, hT, B_hT, stf, B_stf, stb, B_stb, rotM)

        def run_B1a(l):
            AR.reset()
            ckS, B_ck = AR.get("ckS", [128, 2, S], BF16)
            cvS, B_cv = AR.get("cvS", [128, 32, 256], BF16)
            ikS, B_ik = AR.get("ikS", [128, S], BF16)
            dkS, B_dk = AR.get("dkS", [128, 2, S], BF16)
            dvS, B_dv = AR.get("dvS", [128, 32, 256], BF16)
            scores, B_sc = AR.get("scores", [128, S], F32)
            maskb, B_mk = AR.get("maskb", [128, S], BF16)
            kmf, B_kmf = AR.get("kmf", [128, 32], F32)
            kmT, B_km = AR.get("kmT", [128, 2, 16], BF16)
            mark = AR.off
            DMA("sp", ckS, ckT.rearrange("(hh p) s -> p hh s", p=128), (B_z,), (B_ck,))
            DMA("sp", dkS, dkT.rearrange("(hh p) s -> p hh s", p=128), (B_z,), (B_dk,))
            DMA("sp", ikS[0:64, :], ikT, (B_z,), (B_ik,))
            DMA("sp", ikS[64:128, :], ikT, (B_z,), (B_ik,))
            for n0 in range(0, 32, 4):
                DMA("sp", cvS[:, n0:n0 + 4, :], cvd[n0 * 128:(n0 + 4) * 128, :].rearrange("(n p) c -> p n c", p=128), (B_z,), (B_cv,))
                DMA("sp", dvS[:, n0:n0 + 4, :], dvd[n0 * 128:(n0 + 4) * 128, :].rearrange("(n p) c -> p n c", p=128), (B_z,), (B_dv,))
            DMA("sp", cw[:], convw_d[l].rearrange("(c p) j -> p c j", p=128), (), (B_layerc,))
            DMA("sp", pscale[:], pscale_d[l], (), (B_layerc,))
            for c in range(2):
                for gl in range(2):
                    DMA("pool", pwblk[gl * 64:(gl + 1) * 64, c, gl * 64:(gl + 1) * 64], poolw_d[l, 2 * c + gl], (), (B_layerc,))
            RED(kmf, dkS.rearrange("p hh (n s) -> p (hh n) s", n=16, s=256), ALU.add, (B_dk,), (B_kmf,))
            CP("dve", kmT.rearrange("p a b -> p (a b)"), kmf, (B_kmf,), (B_km,))

            if stop == "B1a0_ld":
                return
            CH = 256
            W_ = CH + 16
            cp = {}
            for nm in ("ain", "ac", "pv", "hh", "yy", "T1", "T2", "T3", "T4"):
                cp[nm] = AR.get(nm, [128, 2, W_], F32)
            cp["ab"] = AR.get("ab", [128, 2, CH], F32)
            cp["d"] = AR.get("d", [128, 2, CH], BF16)
            cp["ya"] = AR.get("ya", [128, 2, CH], BF16)
            cp["yb"] = AR.get("yb", [128, 2, CH], BF16)
            cp["t16"] = AR.get("t16", [128, 2, 16], F32)
            ain, B_ain = cp["ain"]
            ac, B_ac = cp["ac"]
            pv, B_pv = cp["pv"]
            hh, B_hh = cp["hh"]
            yy, B_yy = cp["yy"]
            T1, B_T1 = cp["T1"]
            T2, B_T2 = cp["T2"]
            T3, B_T3 = cp["T3"]
            T4, B_T4 = cp["T4"]
            ab, B_ab = cp["ab"]
            dd, B_dd = cp["d"]
            ya, B_ya = cp["ya"]
            yb, B_yb = cp["yb"]
            t16, B_t16 = cp["t16"]
            pcv, B_pcv = ps[6], B_ps[6]
            for ci in range(S // CH):
                c0 = ci * CH

                def ld(dst, r0, halo):
                    if halo:
                        if ci == 0:
                            return [("m", dst[:, :, 0:16]), ("d", dst[:, :, 16:W_], aT[r0:r0 + 256, 0:CH])]
                        return [("d", dst[:, :, :], aT[r0:r0 + 256, c0 - 16:c0 + CH])]
                    return [("d", dst[:, :, :], aT[r0:r0 + 256, c0:c0 + CH])]

                for (dst, B_d, r0, halo) in ((ain, B_ain, 0, True), (ac, B_ac, 256, True), (ab, B_ab, 512, False), (pv, B_pv, 768, True)):
                    for op in ld(dst, r0, halo):
                        if op[0] == "m":
                            MSET("pool", op[1], 0.0, (B_d,))
                        else:
                            DMA("sp", op[1], op[2].rearrange("(c p) t -> p c t", p=128), (B_z,), (B_d,))
                TT("pool", hh, ain, ac, ALU.mult, (B_ain, B_ac), (B_hh,))
                for c in range(2):
                    TS("dve", yy[:, c, 0:CH], hh[:, c, 16:W_], cw[:, c, 2:3], None, ALU.mult, None, (B_hh, B_layerc), (B_yy,))
                    STT(yy[:, c, 0:CH], hh[:, c, 15:W_ - 1], cw[:, c, 1:2], yy[:, c, 0:CH], ALU.mult, ALU.add, (B_hh, B_layerc, B_yy), (B_yy,))
                    STT(yy[:, c, 0:CH], hh[:, c, 14:W_ - 2], cw[:, c, 0:1], yy[:, c, 0:CH], ALU.mult, ALU.add, (B_hh, B_layerc, B_yy), (B_yy,))
                TT("pool", ya, yy[:, :, 0:CH], ab, ALU.mult, (B_yy, B_ab), (B_ya,))
                DMA("sp", mixT[0:256, c0:c0 + CH].rearrange("(c p) t -> p c t", p=128), ya, (B_ya,), (B_mix,))
                TT("pool", T1[:, :, 1:W_], pv[:, :, 1:W_], pv[:, :, 0:W_ - 1], ALU.add, (B_pv,), (B_T1,))
                TT("pool", T2[:, :, 3:W_], T1[:, :, 3:W_], T1[:, :, 1:W_ - 2], ALU.add, (B_T1,), (B_T2,))
                TT("pool", T3[:, 1, 7:W_], T2[:, 1, 7:W_], T2[:, 1, 3:W_ - 4], ALU.add, (B_T2,), (B_T3,))
                TT("pool", T4[64:128, 1, 15:W_], T3[64:128, 1, 15:W_], T3[64:128, 1, 7:W_ - 8], ALU.add, (B_T3,), (B_T4,))
                grp = ((T1, B_T1, 0, 64, 0, 0.5), (T2, B_T2, 64, 128, 0, 0.25), (T3, B_T3, 0, 64, 1, 0.125), (T4, B_T4, 64, 128, 1, 0.0625))
                for (Tg, B_Tg, p0, p1, c, iw_) in grp:
                    STT(dd[p0:p1, c, :], Tg[p0:p1, c, 16:W_], iw_, pv[p0:p1, c, 16:W_], ALU.mult, ALU.subtract, (B_Tg, B_pv), (B_dd,))
                    if ci == 0:
                        TT("dve", t16[p0:p1, c, :], Tg[p0:p1, c, 16:32], invc[p0:p1, c, :], ALU.mult, (B_Tg, B_const), (B_t16,))
                        TT("dve", dd[p0:p1, c, 0:16], t16[p0:p1, c, :], pv[p0:p1, c, 16:32], ALU.subtract, (B_t16, B_pv), (B_dd,))
                for c in range(2):
                    MM(pcv[:, c * CH:(c + 1) * CH], pwblk[:, c, :], dd[:, c, :], c == 0, c == 1, (B_layerc, B_dd, B_const), (B_pcv,))
                for c in range(2):
                    ACT(yb[:, c, :], pcv[:, c * CH:(c + 1) * CH], AF.Copy, (B_pcv, B_layerc), (B_yb,), scale=pscale[:, c:c + 1])
                DMA("sp", mixT[256:512, c0:c0 + CH].rearrange("(c p) t -> p c t", p=128), yb, (B_yb,), (B_mix,))

            if stop == "B1a0_cp":
                return
            barrier()
            AR.off = mark
            iqj = [AR.get(f"iqj{i}", [128, 8, 128], BF16) for i in range(2)]
            cqj = [AR.get(f"cqj{i}", [128, 4, 128], BF16) for i in range(2)]
            dqj = [AR.get(f"dqj{i}", [128, 4, 128], BF16) for i in range(2)]
            for lst in (iqj, cqj, dqj):
                for (ap_, b_) in lst:
                    MSET("pool", ap_, 0.0, (b_,))
            iwj = [AR.get(f"iwj{i}", [128, 8], F32) for i in range(2)]
            rsb = [AR.get(f"rsb{i}", [128, 512], F32) for i in range(2)]
            PTs = [AR.get(f"PT{i}", [128, 512], BF16) for i in range(3)]
            rden, B_rden = AR.get("rden", [64, 512], F32)
            outb = [AR.get(f"outb{i}", [64, 4, 128], BF16) for i in range(2)]
            st_, B_st = AR.get("tk", [128, 64], F32)
            Gt, B_G = AR.get("G", [128, 4, 16], F32)
            top8, B_t8 = AR.get("top8", [128, 4, 8], F32)
            mbias, B_mb = AR.get("mbias", [128, 4, 16], BF16)
            ptc = [0]

            def attn_tile(kS, B_k, vS, B_v, qj, B_q, stl, head0, j, O, B_O, Dn, B_Dn, Lrot, first, lastt, extra_ops):
                L, B_L = Lrot.get()
                ops = []
                for h_ in range(4):
                    ops.append((h_ * 128, (h_ + 1) * 128, kS[:, h_ // 2, stl * 128:(stl + 1) * 128], qj[:, h_, :], (B_k, B_q)))
                ops.extend(extra_ops(stl))
                for dlt in (0, 1):
                    if stl == j - dlt:
                        for h_ in range(4):
                            ops.append((h_ * 128, (h_ + 1) * 128, Tb[:, head0 + h_, dlt, :], ident[:], (B_const,)))
                firstw = {}
                lastw = {}
                for oi, op in enumerate(ops):
                    for r_ in range(op[0] // 128, op[1] // 128):
                        firstw.setdefault(r_, oi)
                        lastw[r_] = oi
                for oi, op in enumerate(ops):
                    MM(L[:, op[0]:op[1]], op[2], op[3], oi == 0, oi == len(ops) - 1, op[4], (B_L,))
                PT, B_PT = PTs[ptc[0] % 3]
                ptc[0] += 1
                ACT(PT, L[:, :], AF.Exp, (B_L,), (B_PT,))
                for h_ in range(4):
                    MM(O[0:64, h_ * 128:(h_ + 1) * 128], vS[:, stl, h_ * 64:(h_ + 1) * 64], PT[:, h_ * 128:(h_ + 1) * 128],
                       first and h_ == 0, lastt and h_ == 3, (B_v, B_PT), (B_O,))
                MM(Dn[0:64, :], ones64[:, :], PT, first, lastt, (B_const, B_PT), (B_Dn,))

            def finish(O, B_O, Dn, B_Dn, ob, B_ob, row0, j):
                P.emit("dve", lambda h: h.reciprocal(out=rden, in_=Dn[0:64, :]), (B_Dn,), (B_rden,))
                TT("dve", ob.rearrange("p a b -> p (a b)"), O[0:64, :], rden, ALU.mult, (B_O, B_rden), (B_ob,))
                DMA("sp", mixT[row0:row0 + 256, j * 128:(j + 1) * 128].rearrange("(h d) q -> d h q", d=64), ob, (B_ob,), (B_mix,))

            Lrot = Rot([0, 1])
            Srot = Rot([4, 5])
            for j in range(NT):
                if stop is not None and stop.startswith("B1a0_j") and j > int(stop[6:7]):
                    return
                jj = j % 2
                qcols = slice(j * 128, (j + 1) * 128)
                iq_, B_iq = iqj[jj]
                cq_, B_cq = cqj[jj]
                dq_, B_dq = dqj[jj]
                iw_, B_iw = iwj[jj]
                for e_ in range(2):
                    pr_ = slice(e_ * 64, (e_ + 1) * 64)
                    for (dst_, Bd_, src_) in ((iq_, B_iq, iqT), (cq_, B_cq, cqT), (dq_, B_dq, dqT)):
                        DMA("sp", dst_.rearrange("p (hh e) q -> p hh e q", e=2)[pr_, :, e_, :],
                            src_[:, qcols].rearrange("(hh e d) q -> e d hh q", e=2, d=64)[e_], (B_z,), (Bd_,))
                DMA("sp", iw_, iwd[qcols, :], (B_z,), (B_iw,))
                ns = j + 1
                nv = ns * 128
                n512 = (ns + 3) // 4
                ri = 0
                for sc_ in range(n512):
                    wdt = min(512, nv - sc_ * 512)
                    for h_ in range(8):
                        pS, B_pS = Srot.get()
                        MM(pS[:, 0:wdt], iq_[:, h_, :], ikS[:, sc_ * 512:sc_ * 512 + wdt], True, True, (B_iq, B_ik), (B_pS,))
                        r_, B_r = rsb[ri % 2]
                        ri += 1
                        ACT(r_[:, 0:wdt], pS[:, 0:wdt], AF.Relu, (B_pS,), (B_r,))
                        dst = scores[:, sc_ * 512:sc_ * 512 + wdt]
                        if h_ == 0:
                            TS("dve", dst, r_[:, 0:wdt], iw_[:, 0:1], None, ALU.mult, None, (B_r, B_iw), (B_sc,))
                        else:
                            STT(dst, r_[:, 0:wdt], iw_[:, h_:h_ + 1], dst, ALU.mult, ALU.add, (B_r, B_iw, B_sc), (B_sc,))
                if stop == "B1a0_j0a":
                    return
                obk = j // 2
                if obk > 0:
                    pG, B_pG = Srot.get()
                    for h_ in range(4):
                        MM(pG[:, h_ * 16:(h_ + 1) * 16], dq_[:, h_, :], kmT[:, h_ // 2, :], h_ == 0, h_ == 3, (B_dq, B_km), (B_pG,))
                    MSET("dve", Gt[:], -1e9, (B_G,))
                    CP("dve", Gt[:, :, 0:obk], pG[:, 0:64].rearrange("p (h n) -> p h n", h=4, n=16)[:, :, 0:obk], (B_pG,), (B_G,))
                    for h_ in range(4):
                        P.emit("dve", (lambda hh_: (lambda h: h.max(out=top8[:, hh_, :], in_=Gt[:, hh_, :])))(h_), (B_G,), (B_t8,))
                        TS("dve", top8[:, h_, 2:3], top8[:, h_, 2:3], -1e8, None, ALU.max, None, (B_t8,), (B_t8,))
                        TS("dve", mbias[:, h_, :], Gt[:, h_, :], top8[:, h_, 2:3], NEG, ALU.is_lt, ALU.mult, (B_G, B_t8), (B_mb,))

                def moba_mask(stl, obk=obk):
                    n = stl // 2
                    r_ = []
                    if n < obk:
                        for h_ in range(4):
                            r_.append((h_ * 128, (h_ + 1) * 128, mbias[:, h_, n:n + 1].to_broadcast([128, 128]), ident[:], (B_mb, B_const)))
                    return r_

                Om, B_Om = ps[6], B_ps[6]
                Dm, B_Dm = ps[7], B_ps[7]
                for stl in range(ns):
                    attn_tile(dkS, B_dk, dvS, B_dv, dq_, B_dq, stl, 4, j, Om, B_Om, Dm, B_Dm, Lrot, stl == 0, stl == ns - 1, moba_mask)

                if stop == "B1a0_j0i":
                    return
                sv = scores[:, 0:nv]
                RED(st_[:, 0:1], sv, ALU.min, (B_sc,), (B_st,))
                RED(st_[:, 1:2], sv, ALU.max, (B_sc,), (B_st,))
                TT("dve", scores[:, j * 128:(j + 1) * 128], scores[:, j * 128:(j + 1) * 128], trineg, ALU.add, (B_sc, B_const), (B_sc,))
                TS("dve", st_[:, 2:3], st_[:, 1:2], st_[:, 0:1], 0.02, ALU.subtract, ALU.add, (B_st,), (B_st,))
                STT(st_[:, 3:4], st_[:, 2:3], 0.5, st_[:, 0:1], ALU.mult, ALU.add, (B_st,), (B_st,))
                TS("dve", st_[:, 3:4], st_[:, 3:4], -0.01, None, ALU.add, None, (B_st,), (B_st,))
                TS("dve", st_[:, 16:32], c1, st_[:, 2:3], None, ALU.mult, None, (B_st, B_const), (B_st,))
                TS("dve", st_[:, 32:48], c2, st_[:, 2:3], None, ALU.mult, None, (B_st, B_const), (B_st,))
                for it in range(nbis):
                    TS("dve", maskb[:, 0:nv], sv, st_[:, 3:4], 0.0, ALU.is_ge, ALU.add, (B_sc, B_st), (B_mk, B_st), accum=st_[:, 4:5])
                    STT(st_[:, 5:6], st_[:, 4:5], KTOP - 0.5, st_[:, 32 + it:33 + it], ALU.is_ge, ALU.mult, (B_st,), (B_st,))
                    STT(st_[:, 3:4], st_[:, 3:4], st_[:, 16 + it:17 + it], st_[:, 5:6], ALU.subtract, ALU.add, (B_st,), (B_st,))
                TS("dve", maskb[:, 0:nv], sv, st_[:, 3:4], NEG, ALU.is_lt, ALU.mult, (B_sc, B_st), (B_mk,))

                ob, B_ob = outb[1]
                finish(Om, B_Om, Dm, B_Dm, ob, B_ob, 768, j)
                if stop == "B1a0_j0t":
                    return
                def dsa_mask(stl):
                    return [(0, 512, maskb[:, stl * 128:(stl + 1) * 128], I4[:, :], (B_mk, B_const))]

                O, B_O = ps[2], B_ps[2]
                Dn, B_Dn = ps[3], B_ps[3]
                for stl in range(ns):
                    attn_tile(ckS, B_ck, cvS, B_cv, cq_, B_cq, stl, 0, j, O, B_O, Dn, B_Dn, Lrot, stl == 0, stl == ns - 1, dsa_mask)
                ob, B_ob = outb[0]
                finish(O, B_O, Dn, B_Dn, ob, B_ob, 512, j)


        run_A0()
        barrier()
        if stop != "A0":
            for l in range(nlayers):
                run_B1a(l)
                barrier()
                if stop is not None and stop.startswith("B1a0"):
                    break
                try:
                    run_dense(l)
                except _Stop:
                    pass
                barrier()
                if stop is not None and stop.startswith("dense0"):
                    break
        if stop is not None:
            AR.reset()
            t_, B_t = AR.get("dbg", [128, 64], F32)
            MSET("dve", t_, 0.0, (B_t,))
            DMA("sp", y_out[0:128, 0:64], t_, (B_t,), ())
        P.generate(nc, block, sems)
    return nc, P


def _rel_bucket(n):
    n = np.maximum(n, 0)
    nf = np.maximum(n, 1).astype(np.float32)
    large = 16 + (np.log(nf / np.float32(16)) / np.float32(np.log(128 / 16)) * np.float32(16)).astype(np.int32)
    large = np.minimum(large, 31)
    return np.where(n < 16, n, large)


def host_inputs(inputs, nlayers=DEPTH):
    f = lambda a: np.ascontiguousarray(np.asarray(a, dtype=np.float32))
    rel_bias = f(inputs["rel_bias"])
    q = np.arange(128)[:, None]
    s = np.arange(128)[None, :]
    relT = np.zeros((128, 16, 128), np.float32)
    for h in range(8):
        for d in range(2):
            relT[:, h * 2 + d, :] = rel_bias[h][_rel_bucket(d * 128 + q - s)]
    relb = np.ascontiguousarray(np.broadcast_to(rel_bias.reshape(1, 256), (128, 256)))
    consts = np.zeros((128, 512), np.float32)
    consts[:, 0:128] = np.eye(128)
    consts[:, 128:256] = np.where(s > q, NEG, 0.0)
    consts[:, 256:384] = np.where(s > q, -1e9, 0.0)
    K = NBIS
    c1 = np.array([2.0 ** -(i + 2) for i in range(K - 1)] + [2.0 ** -K])
    c2 = np.array([2.0 ** -(i + 1) for i in range(K - 1)] + [2.0 ** -K])
    consts[:, 384:384 + K] = c1
    consts[:, 400:400 + K] = c2
    wins = {(0, 0): 2, (1, 0): 4, (0, 1): 8, (1, 1): 16}
    for ph in range(2):
        for c in range(2):
            w = wins[(ph, c)]
            for t in range(16):
                consts[ph * 64:(ph + 1) * 64, 416 + c * 16 + t] = 1.0 / min(t + 1, w)
    g = lambda k: f(inputs[k])
    gpack = np.zeros((DEPTH + 1, 5, D), np.float32)
    for l in range(DEPTH):
        gpack[l + 1, 0] = g("g_mix_post")[l]
        gpack[l + 1, 1] = g("g_ffn_pre")[l]
        gpack[l + 1, 2] = g("g_ffn_post")[l]
        gpack[l + 1, 3] = g("g_ple")[l]
        gpack[l, 4] = g("g_mix_pre")[l]
    shared = dict(
        w_in=g("w_in")[:nlayers], w_out=g("w_out")[:nlayers], w_gate_up=g("w_gate_up")[:nlayers], w_down=g("w_down")[:nlayers],
        w_ple_gate=g("w_ple_gate")[:nlayers], w_ple_proj=g("w_ple_proj")[:nlayers], gpack=gpack,
        convw_t=np.ascontiguousarray(g("conv_w").transpose(0, 2, 1)),
        pool_w=g("pool_w"),
        pscale_t=np.ascontiguousarray(g("pool_scale").reshape(DEPTH, 2, 128).transpose(0, 2, 1)),
        relT=relT, relb=relb, consts=consts,
    )
    x = g("x")
    p = g("p")
    maps = []
    for c in range(8):
        b = c % 4
        m = dict(shared)
        m["x"] = np.ascontiguousarray(x[b])
        m["p"] = np.ascontiguousarray(p[:nlayers, b])
        maps.append(m)
    return maps


_CACHE = {}


def kernel(**inputs):
    if "nc" not in _CACHE:
        _CACHE["nc"] = build()[0]
    nc = _CACHE["nc"]
    maps = host_inputs(inputs)
    res = run_bass_kernel_spmd(nc, maps, core_ids=list(range(8)))
    out = np.stack([np.asarray(res.results[b]["y"], dtype=np.float32) for b in range(4)], axis=0)
    return out
```

```python
import numpy as np
import concourse.bass as bass
import concourse.mybir as mybir
from concourse.bass_utils import run_bass_kernel_spmd

F32, BF16 = mybir.dt.float32, mybir.dt.bfloat16
ALU, AF, AX = mybir.AluOpType, mybir.ActivationFunctionType, mybir.AxisListType

S = 4096
D = 1024
NT = 32
NG = 8
DEPTH = 4
DFF = 2816
INC = 3144
NEG = -30000.0
KTOP = 256
NBIS = 12
ARENA_BYTES = 132 * 1024


class _Stop(Exception):
    pass


class Buf:
    __slots__ = ("name", "w", "r")

    def __init__(self, name):
        self.name = name
        self.w = None
        self.r = {}


class Ins:
    __slots__ = ("eng", "fn", "deps", "sig", "ms", "dma", "slot")


ENGS = ("pe", "act", "dve", "pool", "sp", "poolw")


KSEM = 14
DMAQ = ("sp", "pool", "poolw")


class Prog:
    def __init__(self):
        self.q = {e: [] for e in ENGS}
        self.extra = {e: set() for e in ENGS}
        self.lastdma = {}
        self.ndma = {e: 0 for e in ENGS}
        self.n = 0

    def emit(self, eng, fn, reads=(), writes=(), dma=False):
        ins = Ins()
        ins.eng = eng
        ins.fn = fn
        ins.sig = False
        ins.ms = None
        ins.dma = dma
        ins.slot = None
        deps = set(self.extra[eng])
        self.extra[eng] = set()
        for b in reads:
            if b.w is not None:
                deps.add(b.w)
        for b in writes:
            if b.w is not None:
                deps.add(b.w)
            deps.update(b.r.values())
        if dma:
            i = self.ndma[eng]
            self.ndma[eng] = i + 1
            ins.slot = i % KSEM
            ins.ms = 16 * (i // KSEM + 1)
            prev = self.lastdma.get((eng, ins.slot))
            if prev is not None:
                deps.add(prev)
            self.lastdma[(eng, ins.slot)] = ins
        fd = []
        for d in deps:
            if d is ins:
                continue
            if (not d.dma) and d.eng == eng and eng == "pe":
                continue
            d.sig = True
            fd.append(d)
        ins.deps = fd
        for b in reads:
            b.r[(eng, dma, ins.slot)] = ins
        for b in writes:
            b.w = ins
            b.r = {}
        self.q[eng].append(ins)
        self.n += 1
        return ins

    def generate(self, nc, block, sems):
        for e in ENGS:
            c = 0
            for ins in self.q[e]:
                if (not ins.dma) and ins.sig:
                    c += 1
                    ins.ms = c
        final = {}
        for (e, slot), ins in self.lastdma.items():
            if e != "poolw":
                final[("d", e, slot)] = ins.ms

        def run(e, h, waited):
            for ins in self.q[e]:
                need = {}
                for d in ins.deps:
                    key = ("d", d.eng, d.slot) if d.dma else ("c", d.eng)
                    if need.get(key, 0) < d.ms:
                        need[key] = d.ms
                for key, v in need.items():
                    if waited.get(key, 0) < v:
                        h.wait_ge(sems[key], v)
                        waited[key] = v
                r = ins.fn(h)
                if ins.dma:
                    r.then_inc(sems[("d", e, ins.slot)], 16)
                elif ins.sig:
                    r.then_inc(sems[("c", e)], 1)
            if e == "sp":
                for key, v in final.items():
                    if waited.get(key, 0) < v:
                        h.wait_ge(sems[key], v)

        @block.tensor
        def _(h):
            run("pe", h, {})

        @block.scalar
        def _(h):
            run("act", h, {})

        @block.vector
        def _(h):
            run("dve", h, {})

        @block.gpsimd
        def _(h):
            w = {}
            run("poolw", h, w)
            run("pool", h, w)

        @block.sync
        def _(h):
            run("sp", h, {})


def build(nlayers=DEPTH, stop=None, debug=False, nbis=NBIS):
    nc = bass.Bass("TRN2", target_bir_lowering=False)
    P = Prog()
    skind = "ExternalOutput" if debug else "Internal"

    def din(name, shape, dt=F32):
        return nc.dram_tensor(name, list(shape), dt, kind="ExternalInput").ap()

    def dscr(name, shape, dt):
        return nc.dram_tensor(name, list(shape), dt, kind=skind).ap()

    x_in = din("x", [S, D])
    p_in = din("p", [nlayers, S, 256])
    w_in_d = din("w_in", [nlayers, D, INC])
    w_out_d = din("w_out", [nlayers, D, D])
    w_gu_d = din("w_gate_up", [nlayers, D, 2 * DFF])
    w_dn_d = din("w_down", [nlayers, DFF, D])
    w_pg_d = din("w_ple_gate", [nlayers, D, D])
    w_pp_d = din("w_ple_proj", [nlayers, 256, D])
    gpack_d = din("gpack", [DEPTH + 1, 5, D])
    convw_d = din("convw_t", [DEPTH, 256, 3])
    poolw_d = din("pool_w", [DEPTH, 4, 64, 64])
    pscale_d = din("pscale_t", [DEPTH, 128, 2])
    relT_d = din("relT", [128, 16, 128])
    relb_d = din("relb", [128, 256])
    consts_d = din("consts", [128, 512])
    y_out = nc.dram_tensor("y", [S, D], F32, kind="ExternalOutput").ap()

    wb = {}
    for l in range(nlayers):
        wb[l] = dict(
            w_in=nc.dram_tensor(f"wb_in{l}", [D, INC], BF16, kind="Internal").ap(),
            w_out=nc.dram_tensor(f"wb_out{l}", [D, D], BF16, kind="Internal").ap(),
            w_gu=nc.dram_tensor(f"wb_gu{l}", [D, 2 * DFF], BF16, kind="Internal").ap(),
            w_dn=nc.dram_tensor(f"wb_dn{l}", [DFF, D], BF16, kind="Internal").ap(),
            w_pg=nc.dram_tensor(f"wb_pg{l}", [D, D], BF16, kind="Internal").ap(),
            w_pp=nc.dram_tensor(f"wb_pp{l}", [256, D], BF16, kind="Internal").ap(),
        )
    wsrc = dict(w_in=w_in_d, w_out=w_out_d, w_gu=w_gu_d, w_dn=w_dn_d, w_pg=w_pg_d, w_pp=w_pp_d)
    wbufs = {}

    x_cur = dscr("x_cur", [S, D], F32)
    aT = dscr("aT", [1024, S], F32)
    cqT = dscr("cqT", [256, S], BF16)
    dqT = dscr("dqT", [256, S], BF16)
    iqT = dscr("iqT", [512, S], BF16)
    ckT = dscr("ckT", [256, S], BF16)
    dkT = dscr("dkT", [256, S], BF16)
    ikT = dscr("ikT", [64, S], BF16)
    cvd = dscr("cv", [S, 256], BF16)
    dvd = dscr("dv", [S, 256], BF16)
    iwd = dscr("iw", [S, 8], F32)
    mixT = dscr("mixT", [1024, S], BF16)
    B_z = Buf("zscratch")
    B_mix = Buf("mixT")
    B_xcur = [Buf(f"xcur{g}") for g in range(NG)]

    import contextlib
    es = contextlib.ExitStack()
    with es:
        def sb(name, shape, dt):
            return es.enter_context(nc.sbuf_tensor(name, list(shape), dt))

        arena = sb("arena", [128, ARENA_BYTES // 2], BF16)
        consts = sb("consts_sb", [128, 512], F32)
        ident = sb("ident_bf", [128, 128], BF16)
        I4 = sb("I4", [128, 512], BF16)
        ones64 = sb("ones64", [128, 64], BF16)
        Tb = sb("Tb", [128, 8, 2, 128], BF16)
        relb = sb("relb_sb", [128, 256], F32)
        pwblk = sb("pwblk", [128, 2, 128], BF16)
        cw = sb("cw", [128, 2, 3], F32)
        pscale = sb("pscale", [128, 2], F32)
        small = sb("small", [128, 256], F32)
        bar_s = sb("bar_s", [128, 8], F32)
        WBt = [sb(f"WB{i}", [128, 8, 512], BF16) for i in range(4)]
        B_WB = [Buf(f"WB{i}") for i in range(4)]
        ps = [es.enter_context(nc.psum_tensor(f"ps{i}", [128, 512], F32)) for i in range(8)]
        B_ps = [Buf(f"ps{i}") for i in range(8)]
        B_const = Buf("const")
        B_layerc = Buf("layerconst")
        B_small = Buf("small")
        B_bar = {e: Buf("bar" + e) for e in ENGS}
        B_sm = {}
        B_msbs = [Buf(f"msb{t}") for t in range(4)]

        sem_names = [("c", "pe"), ("c", "act"), ("c", "dve"), ("c", "pool")]
        sem_names += [("d", q_, i_) for q_ in DMAQ for i_ in range(KSEM)]
        sems = {k: es.enter_context(nc.semaphore("s_" + "_".join(str(x_) for x_ in k))) for k in sem_names}
        block = es.enter_context(nc.Block())

        tri_f = consts[:, 128:256]
        trineg = consts[:, 256:384]
        c1 = consts[:, 384:400]
        c2 = consts[:, 400:416]
        invc = consts[:, 416:448].rearrange("p (c t) -> p c t", c=2, t=16)

        class Arena:
            def __init__(self):
                self.off = 0

            def reset(self):
                self.off = 0

            def get(self, name, shape, dt):
                nel = 1
                for s_ in shape[1:]:
                    nel *= s_
                nbytes = nel * (4 if dt == F32 else 2)
                nbytes = (nbytes + 63) // 64 * 64
                assert self.off + nbytes <= ARENA_BYTES, (name, self.off, nbytes)
                ap = arena[:, self.off // 2:(self.off + nbytes) // 2]
                if dt == F32:
                    ap = ap.bitcast(F32)
                ap = ap[0:shape[0], 0:nel]
                if len(shape) == 3:
                    ap = ap.rearrange("p (a b) -> p a b", a=shape[1], b=shape[2])
                elif len(shape) == 4:
                    ap = ap.rearrange("p (a b c) -> p a b c", a=shape[1], b=shape[2], c=shape[3])
                self.off += nbytes
                return ap, Buf(name)

        AR = Arena()

        def MM(out, lhsT, rhs, st, sp_, R, W):
            P.emit("pe", lambda h: h.matmul(out, lhsT=lhsT, rhs=rhs, start=st, stop=sp_), R, W)

        def TRP(out, in_, R, W):
            P.emit("pe", lambda h: h.transpose(out, in_, ident[:]), R, W)

        def ACT(out, in_, func, R, W, scale=None, bias=None, accum=None):
            kw = {}
            if scale is not None:
                kw["scale"] = scale
            if bias is not None:
                kw["bias"] = bias
            if accum is not None:
                kw["accum_out"] = accum
            P.emit("act", lambda h: h.activation(out=out, in_=in_, func=func, **kw), R, W)

        def TS(eng, out, in0, s1, s2, op0, op1, R, W, accum=None):
            kw = {}
            if op1 is not None:
                kw["op1"] = op1
            if accum is not None:
                kw["accum_out"] = accum
            P.emit(eng, lambda h: h.tensor_scalar(out=out, in0=in0, scalar1=s1, scalar2=s2, op0=op0, **kw), R, W)

        def TT(eng, out, in0, in1, op, R, W):
            P.emit(eng, lambda h: h.tensor_tensor(out=out, in0=in0, in1=in1, op=op), R, W)

        def STT(out, in0, scalar, in1, op0, op1, R, W):
            P.emit("dve", lambda h: h.scalar_tensor_tensor(out=out, in0=in0, scalar=scalar, in1=in1, op0=op0, op1=op1), R, W)

        def CP(eng, out, in_, R, W):
            P.emit(eng, lambda h: h.tensor_copy(out, in_), R, W)

        def MSET(eng, ap, val, W):
            P.emit(eng, lambda h: h.memset(ap, val), (), W)

        def RED(out, in_, op, R, W):
            P.emit("dve", lambda h: h.tensor_reduce(out=out, in_=in_, axis=AX.X, op=op), R, W)

        def DMA(q, out, in_, R, W):
            return P.emit(q, lambda h: h.dma_start(out=out, in_=in_), R, W, dma=True)

        def barrier():
            t = []
            t.append(P.emit("pe", lambda h: h.matmul(ps[5][0:1, 0:1], lhsT=ones64[0:1, 0:1], rhs=ones64[0:1, 0:1], start=True, stop=True),
                            (B_const,), (B_ps[5], B_bar["pe"])))
            t.append(P.emit("act", lambda h: h.activation(out=bar_s[0:1, 0:1], in_=bar_s[0:1, 4:5], func=AF.Copy), (B_const,), (B_bar["act"],)))
            t.append(P.emit("dve", lambda h: h.tensor_copy(bar_s[0:1, 1:2], bar_s[0:1, 5:6]), (B_const,), (B_bar["dve"],)))
            t.append(P.emit("pool", lambda h: h.tensor_copy(bar_s[0:1, 2:3], bar_s[0:1, 6:7]), (B_const,), (B_bar["pool"],)))
            for i in t:
                i.sig = True
            allb = set(t) | set(v for k, v in P.lastdma.items() if k[0] != "poolw")
            for e in ENGS:
                if e != "poolw":
                    P.extra[e] |= allb

        DMA("sp", consts[:], consts_d, (), (B_const,))
        DMA("sp", relb[:], relb_d, (), (B_const,))
        DMA("pool", ident[:], consts_d[:, 0:128], (), (B_const,))
        for i in range(4):
            DMA("pool", I4[:, i * 128:(i + 1) * 128], consts_d[:, 0:128], (), (B_const,))
        MSET("pool", ones64[:], 1.0, (B_const,))
        MSET("pool", pwblk[:], 0.0, (B_const,))
        MSET("pool", bar_s[:], 0.0, (B_const,))
        AR.reset()
        relT_sb, B_relT = AR.get("relT", [128, 16, 128], F32)
        DMA("sp", relT_sb, relT_d, (), (B_relT,))
        for h_ in range(8):
            b31 = relb[:, h_ * 32 + 31:h_ * 32 + 32]
            STT(Tb[:, h_, 0, :], relT_sb[:, h_ * 2, :], b31, tri_f, ALU.subtract, ALU.add, (B_relT, B_const), (B_const,))
            TS("dve", Tb[:, h_, 1, :], relT_sb[:, h_ * 2 + 1, :], b31, None, ALU.subtract, None, (B_relT, B_const), (B_const,))

        barrier()
        for l in range(nlayers):
            for nm in ("w_in", "w_out", "w_gu", "w_dn", "w_pg", "w_pp"):
                src = wsrc[nm][l]
                dst = wb[l][nm]
                rows = src.shape[0]
                step = 256
                bl = []
                for r0 in range(0, rows, step):
                    r1 = min(rows, r0 + step)
                    b = Buf(f"wb{l}{nm}{r0}")
                    DMA("poolw", dst[r0:r1, :], src[r0:r1, :], (), (b,))
                    bl.append(b)
                wbufs[(l, nm)] = bl

        class WStream:
            def __init__(self):
                self.plan = []
                self.issued = 0
                self.taken = 0

            def add(self, l, nm, view_fn, shape):
                self.plan.append((l, nm, view_fn, shape))

            def _issue(self):
                i = self.issued
                if i >= len(self.plan):
                    return
                l, nm, view_fn, shape = self.plan[i]
                slot = i % 4
                dst = WBt[slot][:]
                for dstv, srcv in view_fn(dst, wb[l][nm]):
                    DMA("sp", dstv, srcv, tuple(wbufs[(l, nm)]), (B_WB[slot],))
                self.issued += 1

            def start(self):
                while self.issued < min(3, len(self.plan)):
                    self._issue()

            def get(self):
                i = self.taken
                assert i < self.issued, "weight stream underflow"
                slot = i % 4
                self.taken += 1
                self._issue()
                return WBt[slot], B_WB[slot]

        WS = WStream()

        def wv_cols(k0, nk, c0, c1):
            def f(dst, w):
                return [(dst[:, 0:nk, 0:c1 - c0],
                         w[k0 * 128:(k0 + nk) * 128, c0:c1].rearrange("(k p) c -> p k c", p=128))]
            return f

        def wv_gu(gb):
            def f(dst, w):
                w3 = w.rearrange("(k p) c -> p k c", p=128)
                return [(dst[:, :, 0:256], w3[:, :, gb * 256:(gb + 1) * 256]),
                        (dst[:, :, 256:512], w3[:, :, DFF + gb * 256:DFF + (gb + 1) * 256])]
            return f

        IN_BLOCKS = [(0, 512), (512, 1024), (1024, 1536), (1536, 2048), (2048, 2376), (2376, 2888), (2888, 3144)]

        def plan_A(l):
            for (c0, c1) in IN_BLOCKS:
                WS.add(l, "w_in", wv_cols(0, 8, c0, c1), None)

        def plan_dense(l, with_A):
            for c in range(2):
                WS.add(l, "w_out", wv_cols(0, 8, c * 512, (c + 1) * 512), None)
            for gb in range(11):
                WS.add(l, "w_gu", wv_gu(gb), None)
            for c in range(2):
                for (k0, nk) in ((0, 8), (8, 8), (16, 6)):
                    WS.add(l, "w_dn", wv_cols(k0, nk, c * 512, (c + 1) * 512), None)
            for c in range(2):
                WS.add(l, "w_pg", wv_cols(0, 8, c * 512, (c + 1) * 512), None)
            if with_A:
                plan_A(l + 1)

        for g in range(NG):
            plan_A(0)
        if stop != "A0":
            for l in range(nlayers):
                if stop is not None and stop.startswith("B1a0"):
                    break
                for g in range(NG):
                    plan_dense(l, l + 1 < nlayers)
                if stop is not None and stop.startswith("dense0"):
                    break
        WS.start()

        class Rot:
            def __init__(self, idx):
                self.idx = idx
                self.i = 0

            def get(self):
                k = self.idx[self.i % len(self.idx)]
                self.i += 1
                return ps[k], B_ps[k]

        def load_gains(pidx, gB, B_g):
            DMA("sp", gB.rearrange("p a b -> p (a b)"),
                gpack_d[pidx:pidx + 1].rearrange("o a b -> o (a b)").partition_broadcast(128), (), (B_g,))

        def rstd_from(ssq_ap, out_ap, n_inv, R, W):
            ACT(out_ap, ssq_ap, AF.Sqrt, R, W, scale=n_inv, bias=1e-6)
            P.emit("dve", lambda h: h.reciprocal(out=out_ap, in_=out_ap), W, W)

        def norm_T(xg, B_xg, gB, B_g, gslot, hT, B_hT, hb, B_hb, junk, B_junk, rotT, sc0):
            for t in range(4):
                ssq = small[:, sc0 + t:sc0 + t + 1]
                rs = small[:, sc0 + 4 + t:sc0 + 5 + t]
                B_s_ = B_sm.setdefault(("n", t), Buf(f"smn{t}"))
                ACT(junk[:, 0:1024], xg[:, t, :], AF.Square, (B_xg,), (B_junk, B_s_), accum=ssq)
                rstd_from(ssq, rs, 1.0 / D, (B_s_,), (B_s_,))
                hbt = hb[t % 2]
                STT(hbt, xg[:, t, :], rs, gB[:, gslot, :], ALU.mult, ALU.mult, (B_xg, B_s_, B_g), (B_hb[t % 2],))
                pt, B_pt = rotT.get()
                ptb = pt[:].bitcast(BF16)
                for k in range(8):
                    TRP(ptb[:, k * 128:(k + 1) * 128], hbt[:, k * 128:(k + 1) * 128], (B_hb[t % 2], B_const), (B_pt,))
                ACT(hT[:, :, t * 128:(t + 1) * 128], ptb.rearrange("p (k q) -> p k q", k=8, q=128), AF.Copy, (B_pt,), (B_hT,))

        def tm_postnorm(lhs_fn, B_lhs, nkc_list, gB, B_g, gslot, xg, B_xg, msb, B_msb, junk, B_junk, rotM, sc0):
            for c in range(2):
                accs = [rotM.get() for _ in range(4)]
                nblk = len(nkc_list)
                kbase = 0
                for bi, nk in enumerate(nkc_list):
                    wt, B_w = WS.get()
                    for t in range(4):
                        pa, B_pa = accs[t]
                        for k in range(nk):
                            MM(pa[:, :], lhs_fn(kbase + k, t), wt[:, k, :], (bi == 0 and k == 0), (bi == nblk - 1 and k == nk - 1),
                               (B_lhs, B_w), (B_pa,))
                    kbase += nk
                if stop == "dense0_1a":
                    raise _Stop()
                for t in range(4):
                    pa, B_pa = accs[t]
                    B_s_ = B_sm.setdefault(("p", t), Buf(f"smp{t}"))
                    CP("dve", msb[:, t, c * 512:(c + 1) * 512], pa[:, :], (B_pa,), (B_msbs[t],))
                    ACT(junk[:, 0:512], msb[:, t, c * 512:(c + 1) * 512], AF.Square, (B_msbs[t],), (B_junk, B_s_),
                        accum=small[:, sc0 + t * 2 + c:sc0 + t * 2 + c + 1])
                if stop == "dense0_1b":
                    raise _Stop()
            if stop == "dense0_1c":
                raise _Stop()
            for t in range(4):
                B_s_ = B_sm[("p", t)]
                tot = small[:, sc0 + 8 + t:sc0 + 9 + t]
                TT("dve", tot, small[:, sc0 + t * 2:sc0 + t * 2 + 1], small[:, sc0 + t * 2 + 1:sc0 + t * 2 + 2], ALU.add, (B_s_,), (B_s_,))
                rs = small[:, sc0 + 12 + t:sc0 + 13 + t]
                rstd_from(tot, rs, 1.0 / D, (B_s_,), (B_s_,))
                STT(msb[:, t, :], msb[:, t, :], rs, gB[:, gslot, :], ALU.mult, ALU.mult, (B_msbs[t], B_s_, B_g), (B_msbs[t],))
                TT("pool", xg[:, t, :], xg[:, t, :], msb[:, t, :], ALU.add, (B_xg, B_msbs[t]), (B_xg,))

        evac_flip = [0]

        def evac(out, in_, R, W, scale=None):
            evac_flip[0] ^= 1
            if evac_flip[0]:
                ACT(out, in_, AF.Copy, R, W, scale=scale)
            else:
                if scale is None:
                    CP("dve", out, in_, R, W)
                else:
                    TS("dve", out, in_, scale, None, ALU.mult, None, R, W)

        def phase_A(l, g, hT, B_hT, stf, B_stf, stb, B_stb, rotM):
            tok0 = g * 512
            cnt = [0]

            def fm(wt, B_w, cl0, cl1, dst_ap, scale=None, f32=False):
                M = cl1 - cl0
                pa, B_pa = rotM.get()
                for k in range(8):
                    MM(pa[0:M, :], wt[:, k, cl0:cl1], hT[:, k, :], k == 0, k == 7, (B_w, B_hT), (B_pa,))
                i = cnt[0] % 3
                cnt[0] += 1
                if f32:
                    st_, B_st = stf[i], B_stf[i]
                else:
                    st_, B_st = stb[i], B_stb[i]
                evac(st_[0:M, :], pa[0:M, :], (B_pa,), (B_st,), scale=scale)
                DMA("sp", dst_ap, st_[0:M, :], (B_st,), (B_z,))

            def tmm(wt, B_w, cl0, cl1, dst_fn, f32=False):
                ncol = cl1 - cl0
                for t in range(4):
                    pa, B_pa = rotM.get()
                    for k in range(8):
                        MM(pa[:, 0:ncol], hT[:, k, t * 128:(t + 1) * 128], wt[:, k, cl0:cl1], k == 0, k == 7, (B_w, B_hT), (B_pa,))
                    i = cnt[0] % 3
                    cnt[0] += 1
                    if f32:
                        st_, B_st = stf[i], B_stf[i]
                    else:
                        st_, B_st = stb[i], B_stb[i]
                    evac(st_[:, 0:ncol], pa[:, 0:ncol], (B_pa,), (B_st,))
                    DMA("sp", dst_fn(tok0 + t * 128), st_[:, 0:ncol], (B_st,), (B_z,))

            cols = slice(tok0, tok0 + 512)
            for bi in range(2):
                wt, B_w = WS.get()
                for j in range(4):
                    r0 = bi * 512 + j * 128
                    fm(wt, B_w, j * 128, (j + 1) * 128, aT[r0:r0 + 128, cols], f32=True)
            wt, B_w = WS.get()
            fm(wt, B_w, 0, 128, cqT[0:128, cols], scale=0.125)
            fm(wt, B_w, 128, 256, cqT[128:256, cols], scale=0.125)
            fm(wt, B_w, 256, 384, ckT[0:128, cols])
            fm(wt, B_w, 384, 512, ckT[128:256, cols])
            wt, B_w = WS.get()
            tmm(wt, B_w, 0, 256, lambda r: cvd[r:r + 128, :])
            fm(wt, B_w, 256, 384, iqT[0:128, cols])
            fm(wt, B_w, 384, 512, iqT[128:256, cols])
            wt, B_w = WS.get()
            fm(wt, B_w, 0, 128, iqT[256:384, cols])
            fm(wt, B_w, 128, 256, iqT[384:512, cols])
            fm(wt, B_w, 256, 320, ikT[0:64, cols])
            tmm(wt, B_w, 320, 328, lambda r: iwd[r:r + 128, :], f32=True)
            wt, B_w = WS.get()
            fm(wt, B_w, 0, 128, dqT[0:128, cols], scale=0.125)
            fm(wt, B_w, 128, 256, dqT[128:256, cols], scale=0.125)
            fm(wt, B_w, 256, 384, dkT[0:128, cols])
            fm(wt, B_w, 384, 512, dkT[128:256, cols])
            wt, B_w = WS.get()
            tmm(wt, B_w, 0, 256, lambda r: dvd[r:r + 128, :])

        def dense_arena():
            AR.reset()
            A = {}
            A["xg"] = AR.get("xg", [128, 4, 1024], F32)
            A["gB"] = AR.get("gB", [128, 5, 1024], F32)
            A["hb0"] = AR.get("hb0", [128, 1024], BF16)
            A["hb1"] = AR.get("hb1", [128, 1024], BF16)
            A["hT0"] = AR.get("hT0", [128, 8, 512], BF16)
            A["hT1"] = AR.get("hT1", [128, 8, 512], BF16)
            A["actT"] = AR.get("actT", [128, 22, 512], BF16)
            A["mixg"] = AR.get("mixg", [128, 8, 512], BF16)
            A["msb"] = AR.get("msb", [128, 4, 1024], F32)
            A["junk"] = AR.get("junk", [128, 1024], BF16)
            A["pg"] = AR.get("pg", [128, 4, 256], BF16)
            A["pT"] = AR.get("pT", [128, 2, 512], BF16)
            A["wpp"] = AR.get("wpp", [128, 2, 2, 512], BF16)
            A["sg0"] = AR.get("sg0", [128, 512], F32)
            A["sg1"] = AR.get("sg1", [128, 512], F32)
            for i in range(3):
                A[f"stf{i}"] = AR.get(f"stf{i}", [128, 512], F32)
                A[f"stb{i}"] = AR.get(f"stb{i}", [128, 512], BF16)
            return A

        def run_A0():
            A = dense_arena()
            xg, B_xg = A["xg"]
            gB, B_g = A["gB"]
            load_gains(0, gB, B_g)
            rotT = Rot([0, 1])
            rotM = Rot([2, 3, 4, 5, 6, 7])
            hTs = [A["hT0"], A["hT1"]]
            hb = [A["hb0"][0], A["hb1"][0]]
            B_hb = [A["hb0"][1], A["hb1"][1]]
            junk, B_junk = A["junk"]
            stf = [A[f"stf{i}"][0] for i in range(3)]
            B_stf = [A[f"stf{i}"][1] for i in range(3)]
            stb = [A[f"stb{i}"][0] for i in range(3)]
            B_stb = [A[f"stb{i}"][1] for i in range(3)]
            for g in range(NG):
                DMA("sp", xg, x_in[g * 512:(g + 1) * 512, :].rearrange("(t p) d -> p t d", p=128), (), (B_xg,))
                hT, B_hT = hTs[g % 2]
                norm_T(xg, B_xg, gB, B_g, 4, hT, B_hT, hb, B_hb, junk, B_junk, rotT, 0)
                phase_A(0, g, hT, B_hT, stf, B_stf, stb, B_stb, rotM)

        def run_dense(l):
            last = (l == DEPTH - 1)
            A = dense_arena()
            xg, B_xg = A["xg"]
            gB, B_g = A["gB"]
            load_gains(l + 1, gB, B_g)
            rotT = Rot([0, 1])
            rotM = Rot([2, 3, 4, 5, 6, 7])
            hTs = [A["hT0"], A["hT1"]]
            hb = [A["hb0"][0], A["hb1"][0]]
            B_hb = [A["hb0"][1], A["hb1"][1]]
            junk, B_junk = A["junk"]
            actT, B_actT = A["actT"]
            mixg, B_mixg = A["mixg"]
            msb, B_msb = A["msb"]
            pg, B_pg = A["pg"]
            pT, B_pT = A["pT"]
            sg = [A["sg0"][0], A["sg1"][0]]
            B_sg = [A["sg0"][1], A["sg1"][1]]
            stf = [A[f"stf{i}"][0] for i in range(3)]
            B_stf = [A[f"stf{i}"][1] for i in range(3)]
            stb = [A[f"stb{i}"][0] for i in range(3)]
            B_stb = [A[f"stb{i}"][1] for i in range(3)]
            xsrc = x_in if l == 0 else x_cur
            xdst = y_out if last else x_cur
            for g in range(NG):
                rows = slice(g * 512, (g + 1) * 512)
                rd = () if l == 0 else (B_xcur[g],)
                DMA("sp", xg, xsrc[rows, :].rearrange("(t p) d -> p t d", p=128), rd, (B_xg,))
                if stop == "dense0_0a":
                    return
                DMA("sp", mixg, mixT[:, rows].rearrange("(k p) s -> p k s", p=128), (B_mix,), (B_mixg,))
                if stop == "dense0_0b":
                    return
                DMA("pool", pg, p_in[l, rows, :].rearrange("(t p) c -> p t c", p=128), (), (B_pg,))
                if stop == "dense0_0":
                    return
                tm_postnorm(lambda k, t: mixg[:, k, t * 128:(t + 1) * 128], B_mixg, [8], gB, B_g, 0, xg, B_xg, msb, B_msb,
                            junk, B_junk, rotM, 16)
                if stop == 'dense0_1':
                    return
                hT, B_hT = hTs[0]
                norm_T(xg, B_xg, gB, B_g, 1, hT, B_hT, hb, B_hb, junk, B_junk, rotT, 0)
                if stop == 'dense0_2':
                    return
                for gb in range(11):
                    wt, B_w = WS.get()
                    for sc in range(2):
                        pg_, B_pg_ = rotM.get()
                        pu_, B_pu_ = rotM.get()
                        for k in range(8):
                            MM(pg_[:, :], wt[:, k, sc * 128:(sc + 1) * 128], hT[:, k, :], k == 0, k == 7, (B_w, B_hT), (B_pg_,))
                        for k in range(8):
                            MM(pu_[:, :], wt[:, k, 256 + sc * 128:256 + (sc + 1) * 128], hT[:, k, :], k == 0, k == 7, (B_w, B_hT), (B_pu_,))
                        ci = gb * 2 + sc
                        s_, B_s = sg[ci % 2], B_sg[ci % 2]
                        ACT(s_, pg_[:, :], AF.Silu, (B_pg_,), (B_s,))
                        TT("dve", actT[:, ci, :], s_, pu_[:, :], ALU.mult, (B_s, B_pu_), (B_actT,))
                if stop == 'dense0_3':
                    return
                tm_postnorm(lambda k, t: actT[:, k, t * 128:(t + 1) * 128], B_actT, [8, 8, 6], gB, B_g, 2, xg, B_xg, msb, B_msb,
                            junk, B_junk, rotM, 16)
                if stop == 'dense0_4':
                    return
                hT, B_hT = hTs[1]
                norm_T(xg, B_xg, gB, B_g, 3, hT, B_hT, hb, B_hb, junk, B_junk, rotT, 0)
                for t in range(4):
                    pt, B_pt = rotT.get()
                    ptb = pt[:].bitcast(BF16)
                    for k in range(2):
                        TRP(ptb[:, k * 128:(k + 1) * 128], pg[:, t, k * 128:(k + 1) * 128], (B_pg, B_const), (B_pt,))
                    CP("dve", pT[:, :, t * 128:(t + 1) * 128], ptb[:, 0:256].rearrange("p (k q) -> p k q", k=2, q=128), (B_pt,), (B_pT,))
                wpp, B_wpp = A["wpp"]
                for u_ in range(2):
                    DMA("sp", wpp[:, :, u_, :], wb[l]["w_pp"].rearrange("(k p) c -> p k c", p=128)[:, :, u_ * 512:(u_ + 1) * 512],
                        tuple(wbufs[(l, "w_pp")]), (B_wpp,))
                for c in range(2):
                    wt, B_w = WS.get()
                    for t in range(4):
                        pa, B_pa = rotM.get()
                        pb, B_pb = rotM.get()
                        for k in range(8):
                            MM(pa[:, :], hT[:, k, t * 128:(t + 1) * 128], wt[:, k, :], k == 0, k == 7, (B_hT, B_w), (B_pa,))
                        for k in range(2):
                            MM(pb[:, :], pT[:, k, t * 128:(t + 1) * 128], wpp[:, k, c, :], k == 0, k == 1, (B_pT, B_wpp), (B_pb,))
                        i = (c * 4 + t) % 2
                        ACT(sg[i], pa[:, :], AF.Sigmoid, (B_pa,), (B_sg[i],))
                        TT("dve", sg[i], sg[i], pb[:, :], ALU.mult, (B_sg[i], B_pb), (B_sg[i],))
                        TT("pool", xg[:, t, c * 512:(c + 1) * 512], xg[:, t, c * 512:(c + 1) * 512], sg[i], ALU.add, (B_xg, B_sg[i]), (B_xg,))
                if stop == 'dense0_5':
                    return
                DMA("sp", xdst[rows, :].rearrange("(t p) d -> p t d", p=128), xg, (B_xg,), () if last else (B_xcur[g],))
                if l + 1 < nlayers:
                    hT, B_hT = hTs[0]
                    norm_T(xg, B_xg, gB, B_g, 4, hT, B_hT, hb, B_hb, junk, B_junk, rotT, 0)
                    phase_A(l + 1, g, hT, B_hT, stf, B_stf, stb, B_stb, rotM)

        def run_B1a(l):
            AR.reset()
            ckS, B_ck = AR.get("ckS", [128, 2, S], BF16)
            cvS, B_cv = AR.get("cvS", [128, 32, 256], BF16)
            ikS, B_ik = AR.get("ikS", [128, S], BF16)
            dkS, B_dk = AR.get("dkS", [128, 2, S], BF16)
            dvS, B_dv = AR.get("dvS", [128, 32, 256], BF16)
            scores, B_sc = AR.get("scores", [128, S], F32)
            maskb, B_mk = AR.get("maskb", [128, S], BF16)
            kmf, B_kmf = AR.get("kmf", [128, 32], F32)
            kmT, B_km = AR.get("kmT", [128, 2, 16], BF16)
            mark = AR.off
            DMA("sp", ckS, ckT.rearrange("(hh p) s -> p hh s", p=128), (B_z,), (B_ck,))
            DMA("sp", dkS, dkT.rearrange("(hh p) s -> p hh s", p=128), (B_z,), (B_dk,))
            DMA("sp", ikS[0:64, :], ikT, (B_z,), (B_ik,))
            DMA("sp", ikS[64:128, :], ikT, (B_z,), (B_ik,))
            for n0 in range(0, 32, 4):
                DMA("sp", cvS[:, n0:n0 + 4, :], cvd[n0 * 128:(n0 + 4) * 128, :].rearrange("(n p) c -> p n c", p=128), (B_z,), (B_cv,))
                DMA("sp", dvS[:, n0:n0 + 4, :], dvd[n0 * 128:(n0 + 4) * 128, :].rearrange("(n p) c -> p n c", p=128), (B_z,), (B_dv,))
            DMA("sp", cw[:], convw_d[l].rearrange("(c p) j -> p c j", p=128), (), (B_layerc,))
            DMA("sp", pscale[:], pscale_d[l], (), (B_layerc,))
            for c in range(2):
                for gl in range(2):
                    DMA("pool", pwblk[gl * 64:(gl + 1) * 64, c, gl * 64:(gl + 1) * 64], poolw_d[l, 2 * c + gl], (), (B_layerc,))
            RED(kmf, dkS.rearrange("p hh (n s) -> p (hh n) s", n=16, s=256), ALU.add, (B_dk,), (B_kmf,))
            CP("dve", kmT.rearrange("p a b -> p (a b)"), kmf, (B_kmf,), (B_km,))

            if stop == "B1a0_ld":
                return
            CH = 256
            W_ = CH + 16
            cp = {}
            for nm in ("ain", "ac", "pv", "hh", "yy", "T1", "T2", "T3", "T4"):
                cp[nm] = AR.get(nm, [128, 2, W_], F32)
            cp["ab"] = AR.get("ab", [128, 2, CH], F32)
            cp["d"] = AR.get("d", [128, 2, CH], BF16)
            cp["ya"] = AR.get("ya", [128, 2, CH], BF16)
            cp["yb"] = AR.get("yb", [128, 2, CH], BF16)
            cp["t16"] = AR.get("t16", [128, 2, 16], F32)
            ain, B_ain = cp["ain"]
            ac, B_ac = cp["ac"]
            pv, B_pv = cp["pv"]
            hh, B_hh = cp["hh"]
            yy, B_yy = cp["yy"]
            T1, B_T1 = cp["T1"]
            T2, B_T2 = cp["T2"]
            T3, B_T3 = cp["T3"]
            T4, B_T4 = cp["T4"]
            ab, B_ab = cp["ab"]
            dd, B_dd = cp["d"]
            ya, B_ya = cp["ya"]
            yb, B_yb = cp["yb"]
            t16, B_t16 = cp["t16"]
            pcv, B_pcv = ps[6], B_ps[6]
            for ci in range(S // CH):
                c0 = ci * CH

                def ld(dst, r0, halo):
                    if halo:
                        if ci == 0:
                            return [("m", dst[:, :, 0:16]), ("d", dst[:, :, 16:W_], aT[r0:r0 + 256, 0:CH])]
                        return [("d", dst[:, :, :], aT[r0:r0 + 256, c0 - 16:c0 + CH])]
                    return [("d", dst[:, :, :], aT[r0:r0 + 256, c0:c0 + CH])]

                for (dst, B_d, r0, halo) in ((ain, B_ain, 0, True), (ac, B_ac, 256, True), (ab, B_ab, 512, False), (pv, B_pv, 768, True)):
                    for op in ld(dst, r0, halo):
                        if op[0] == "m":
                            MSET("pool", op[1], 0.0, (B_d,))
                        else:
                            DMA("sp", op[1], op[2].rearrange("(c p) t -> p c t", p=128), (B_z,), (B_d,))
                TT("pool", hh, ain, ac, ALU.mult, (B_ain, B_ac), (B_hh,))
                for c in range(2):
                    TS("dve", yy[:, c, 0:CH], hh[:, c, 16:W_], cw[:, c, 2:3], None, ALU.mult, None, (B_hh, B_layerc), (B_yy,))
                    STT(yy[:, c, 0:CH], hh[:, c, 15:W_ - 1], cw[:, c, 1:2], yy[:, c, 0:CH], ALU.mult, ALU.add, (B_hh, B_layerc, B_yy), (B_yy,))
                    STT(yy[:, c, 0:CH], hh[:, c, 14:W_ - 2], cw[:, c, 0:1], yy[:, c, 0:CH], ALU.mult, ALU.add, (B_hh, B_layerc, B_yy), (B_yy,))
                TT("pool", ya, yy[:, :, 0:CH], ab, ALU.mult, (B_yy, B_ab), (B_ya,))
                DMA("sp", mixT[0:256, c0:c0 + CH].rearrange("(c p) t -> p c t", p=128), ya, (B_ya,), (B_mix,))
                TT("pool", T1[:, :, 1:W_], pv[:, :, 1:W_], pv[:, :, 0:W_ - 1], ALU.add, (B_pv,), (B_T1,))
                TT("pool", T2[:, :, 3:W_], T1[:, :, 3:W_], T1[:, :, 1:W_ - 2], ALU.add, (B_T1,), (B_T2,))
                TT("pool", T3[:, 1, 7:W_], T2[:, 1, 7:W_], T2[:, 1, 3:W_ - 4], ALU.add, (B_T2,), (B_T3,))
                TT("pool", T4[64:128, 1, 15:W_], T3[64:128, 1, 15:W_], T3[64:128, 1, 7:W_ - 8], ALU.add, (B_T3,), (B_T4,))
                grp = ((T1, B_T1, 0, 64, 0, 0.5), (T2, B_T2, 64, 128, 0, 0.25), (T3, B_T3, 0, 64, 1, 0.125), (T4, B_T4, 64, 128, 1, 0.0625))
                for (Tg, B_Tg, p0, p1, c, iw_) in grp:
                    STT(dd[p0:p1, c, :], Tg[p0:p1, c, 16:W_], iw_, pv[p0:p1, c, 16:W_], ALU.mult, ALU.subtract, (B_Tg, B_pv), (B_dd,))
                    if ci == 0:
                        TT("dve", t16[p0:p1, c, :], Tg[p0:p1, c, 16:32], invc[p0:p1, c, :], ALU.mult, (B_Tg, B_const), (B_t16,))
                        TT("dve", dd[p0:p1, c, 0:16], t16[p0:p1, c, :], pv[p0:p1, c, 16:32], ALU.subtract, (B_t16, B_pv), (B_dd,))
                for c in range(2):
                    MM(pcv[:, c * CH:(c + 1) * CH], pwblk[:, c, :], dd[:, c, :], c == 0, c == 1, (B_layerc, B_dd, B_const), (B_pcv,))
                for c in range(2):
                    ACT(yb[:, c, :], pcv[:, c * CH:(c + 1) * CH], AF.Copy, (B_pcv, B_layerc), (B_yb,), scale=pscale[:, c:c + 1])
                DMA("sp", mixT[256:512, c0:c0 + CH].rearrange("(c p) t -> p c t", p=128), yb, (B_yb,), (B_mix,))

            if stop == "B1a0_cp":
                return
            barrier()
            AR.off = mark
            iqj = [AR.get(f"iqj{i}", [128, 8, 128], BF16) for i in range(2)]
            cqj = [AR.get(f"cqj{i}", [128, 4, 128], BF16) for i in range(2)]
            dqj = [AR.get(f"dqj{i}", [128, 4, 128], BF16) for i in range(2)]
            for lst in (iqj, cqj, dqj):
                for (ap_, b_) in lst:
                    MSET("pool", ap_, 0.0, (b_,))
            iwj = [AR.get(f"iwj{i}", [128, 8], F32) for i in range(2)]
            rsb = [AR.get(f"rsb{i}", [128, 512], F32) for i in range(2)]
            PTs = [AR.get(f"PT{i}", [128, 512], BF16) for i in range(3)]
            rden, B_rden = AR.get("rden", [64, 512], F32)
            outb = [AR.get(f"outb{i}", [64, 4, 128], BF16) for i in range(2)]
            st_, B_st = AR.get("tk", [128, 64], F32)
            Gt, B_G = AR.get("G", [128, 4, 16], F32)
            top8, B_t8 = AR.get("top8", [128, 4, 8], F32)
            mbias, B_mb = AR.get("mbias", [128, 4, 16], BF16)
            ptc = [0]

            def attn_tile(kS, B_k, vS, B_v, qj, B_q, stl, head0, j, O, B_O, Dn, B_Dn, Lrot, first, lastt, extra_ops):
                L, B_L = Lrot.get()
                ops = []
                for h_ in range(4):
                    ops.append((h_ * 128, (h_ + 1) * 128, kS[:, h_ // 2, stl * 128:(stl + 1) * 128], qj[:, h_, :], (B_k, B_q)))
                ops.extend(extra_ops(stl))
                for dlt in (0, 1):
                    if stl == j - dlt:
                        for h_ in range(4):
                            ops.append((h_ * 128, (h_ + 1) * 128, Tb[:, head0 + h_, dlt, :], ident[:], (B_const,)))
                firstw = {}
                lastw = {}
                for oi, op in enumerate(ops):
                    for r_ in range(op[0] // 128, op[1] // 128):
                        firstw.setdefault(r_, oi)
                        lastw[r_] = oi
                for oi, op in enumerate(ops):
                    MM(L[:, op[0]:op[1]], op[2], op[3], oi == 0, oi == len(ops) - 1, op[4], (B_L,))
                PT, B_PT = PTs[ptc[0] % 3]
                ptc[0] += 1
                ACT(PT, L[:, :], AF.Exp, (B_L,), (B_PT,))
                for h_ in range(4):
                    MM(O[0:64, h_ * 128:(h_ + 1) * 128], vS[:, stl, h_ * 64:(h_ + 1) * 64], PT[:, h_ * 128:(h_ + 1) * 128],
                       first and h_ == 0, lastt and h_ == 3, (B_v, B_PT), (B_O,))
                MM(Dn[0:64, :], ones64[:, :], PT, first, lastt, (B_const, B_PT), (B_Dn,))

            def finish(O, B_O, Dn, B_Dn, ob, B_ob, row0, j):
                P.emit("dve", lambda h: h.reciprocal(out=rden, in_=Dn[0:64, :]), (B_Dn,), (B_rden,))
                TT("dve", ob.rearrange("p a b -> p (a b)"), O[0:64, :], rden, ALU.mult, (B_O, B_rden), (B_ob,))
                DMA("sp", mixT[row0:row0 + 256, j * 128:(j + 1) * 128].rearrange("(h d) q -> d h q", d=64), ob, (B_ob,), (B_mix,))

            Lrot = Rot([0, 1])
            Srot = Rot([4, 5])
            def q_loads(j):
                jj = j % 2
                qcols = slice(j * 128, (j + 1) * 128)
                iq_, B_iq = iqj[jj]
                cq_, B_cq = cqj[jj]
                dq_, B_dq = dqj[jj]
                iw_, B_iw = iwj[jj]
                for e_ in range(2):
                    pr_ = slice(e_ * 64, (e_ + 1) * 64)
                    for (dst_, Bd_, src_) in ((iq_, B_iq, iqT), (cq_, B_cq, cqT), (dq_, B_dq, dqT)):
                        DMA("sp", dst_.rearrange("p (hh e) q -> p hh e q", e=2)[pr_, :, e_, :],
                            src_[:, qcols].rearrange("(hh e d) q -> e d hh q", e=2, d=64)[e_], (B_z,), (Bd_,))
                DMA("sp", iw_, iwd[qcols, :], (B_z,), (B_iw,))

            ric = [0]

            def indexer_units(j):
                jj = j % 2
                iq_, B_iq = iqj[jj]
                iw_, B_iw = iwj[jj]
                nv = (j + 1) * 128
                n512 = (j + 4) // 4
                units = []
                for sc_ in range(n512):
                    wdt = min(512, nv - sc_ * 512)
                    for h_ in range(8):
                        def unit(sc_=sc_, h_=h_, wdt=wdt):
                            pS, B_pS = Srot.get()
                            MM(pS[:, 0:wdt], iq_[:, h_, :], ikS[:, sc_ * 512:sc_ * 512 + wdt], True, True, (B_iq, B_ik), (B_pS,))
                            r_, B_r = rsb[ric[0] % 2]
                            ric[0] += 1
                            ACT(r_[:, 0:wdt], pS[:, 0:wdt], AF.Relu, (B_pS,), (B_r,))
                            dst = scores[:, sc_ * 512:sc_ * 512 + wdt]
                            if h_ == 0:
                                TS("dve", dst, r_[:, 0:wdt], iw_[:, 0:1], None, ALU.mult, None, (B_r, B_iw), (B_sc,))
                            else:
                                STT(dst, r_[:, 0:wdt], iw_[:, h_:h_ + 1], dst, ALU.mult, ALU.add, (B_r, B_iw, B_sc), (B_sc,))
                        units.append(unit)
                return units

            q_loads(0)
            for u_ in indexer_units(0):
                u_()
            for j in range(NT):
                jj = j % 2
                cq_, B_cq = cqj[jj]
                dq_, B_dq = dqj[jj]
                ns = j + 1
                nv = ns * 128
                obk = j // 2
                if obk > 0:
                    pG, B_pG = Srot.get()
                    for h_ in range(4):
                        MM(pG[:, h_ * 16:(h_ + 1) * 16], dq_[:, h_, :], kmT[:, h_ // 2, :], h_ == 0, h_ == 3, (B_dq, B_km), (B_pG,))
                    MSET("dve", Gt[:], -1e9, (B_G,))
                    CP("dve", Gt[:, :, 0:obk], pG[:, 0:64].rearrange("p (h n) -> p h n", h=4, n=16)[:, :, 0:obk], (B_pG,), (B_G,))
                    for h_ in range(4):
                        P.emit("dve", (lambda hh_: (lambda h: h.max(out=top8[:, hh_, :], in_=Gt[:, hh_, :])))(h_), (B_G,), (B_t8,))
                        TS("dve", top8[:, h_, 2:3], top8[:, h_, 2:3], -1e8, None, ALU.max, None, (B_t8,), (B_t8,))
                        TS("dve", mbias[:, h_, :], Gt[:, h_, :], top8[:, h_, 2:3], NEG, ALU.is_lt, ALU.mult, (B_G, B_t8), (B_mb,))

                def moba_mask(stl, obk=obk):
                    n = stl // 2
                    r_ = []
                    if n < obk:
                        for h_ in range(4):
                            r_.append((h_ * 128, (h_ + 1) * 128, mbias[:, h_, n:n + 1].to_broadcast([128, 128]), ident[:], (B_mb, B_const)))
                    return r_

                Om, B_Om = ps[6], B_ps[6]
                Dm, B_Dm = ps[7], B_ps[7]
                for stl in range(ns):
                    attn_tile(dkS, B_dk, dvS, B_dv, dq_, B_dq, stl, 4, j, Om, B_Om, Dm, B_Dm, Lrot, stl == 0, stl == ns - 1, moba_mask)

                sv = scores[:, 0:nv]
                RED(st_[:, 0:1], sv, ALU.min, (B_sc,), (B_st,))
                RED(st_[:, 1:2], sv, ALU.max, (B_sc,), (B_st,))
                TT("dve", scores[:, j * 128:(j + 1) * 128], scores[:, j * 128:(j + 1) * 128], trineg, ALU.add, (B_sc, B_const), (B_sc,))
                TS("dve", st_[:, 2:3], st_[:, 1:2], st_[:, 0:1], 0.02, ALU.subtract, ALU.add, (B_st,), (B_st,))
                STT(st_[:, 3:4], st_[:, 2:3], 0.5, st_[:, 0:1], ALU.mult, ALU.add, (B_st,), (B_st,))
                TS("dve", st_[:, 3:4], st_[:, 3:4], -0.01, None, ALU.add, None, (B_st,), (B_st,))
                TS("dve", st_[:, 16:32], c1, st_[:, 2:3], None, ALU.mult, None, (B_st, B_const), (B_st,))
                TS("dve", st_[:, 32:48], c2, st_[:, 2:3], None, ALU.mult, None, (B_st, B_const), (B_st,))
                for it in range(nbis):
                    TS("dve", maskb[:, 0:nv], sv, st_[:, 3:4], 0.0, ALU.is_ge, ALU.add, (B_sc, B_st), (B_mk, B_st), accum=st_[:, 4:5])
                    STT(st_[:, 5:6], st_[:, 4:5], KTOP - 0.5, st_[:, 32 + it:33 + it], ALU.is_ge, ALU.mult, (B_st,), (B_st,))
                    STT(st_[:, 3:4], st_[:, 3:4], st_[:, 16 + it:17 + it], st_[:, 5:6], ALU.subtract, ALU.add, (B_st,), (B_st,))
                TS("dve", maskb[:, 0:nv], sv, st_[:, 3:4], NEG, ALU.is_lt, ALU.mult, (B_sc, B_st), (B_mk,))

                ob, B_ob = outb[1]
                finish(Om, B_Om, Dm, B_Dm, ob, B_ob, 768, j)

                units = []
                if j + 1 < NT:
                    q_loads(j + 1)
                    units = indexer_units(j + 1)

                def dsa_mask(stl):
                    return [(0, 512, maskb[:, stl * 128:(stl + 1) * 128], I4[:, :], (B_mk, B_const))]

                O, B_O = ps[2], B_ps[2]
                Dn, B_Dn = ps[3], B_ps[3]
                ui = 0
                for stl in range(ns):
                    attn_tile(ckS, B_ck, cvS, B_cv, cq_, B_cq, stl, 0, j, O, B_O, Dn, B_Dn, Lrot, stl == 0, stl == ns - 1, dsa_mask)
                    tgt = (len(units) * (stl + 1) + ns - 1) // ns
                    while ui < tgt:
                        units[ui]()
                        ui += 1
                while ui < len(units):
                    units[ui]()
                    ui += 1
                ob, B_ob = outb[0]
                finish(O, B_O, Dn, B_Dn, ob, B_ob, 512, j)


        run_A0()
        barrier()
        if stop != "A0":
            for l in range(nlayers):
                run_B1a(l)
                barrier()
                if stop is not None and stop.startswith("B1a0"):
                    break
                try:
                    run_dense(l)
                except _Stop:
                    pass
                barrier()
                if stop is not None and stop.startswith("dense0"):
                    break
        if stop is not None:
            AR.reset()
            t_, B_t = AR.get("dbg", [128, 64], F32)
            MSET("dve", t_, 0.0, (B_t,))
            DMA("sp", y_out[0:128, 0:64], t_, (B_t,), ())
        P.generate(nc, block, sems)
    return nc, P


def _rel_bucket(n):
    n = np.maximum(n, 0)
    nf = np.maximum(n, 1).astype(np.float32)
    large = 16 + (np.log(nf / np.float32(16)) / np.float32(np.log(128 / 16)) * np.float32(16)).astype(np.int32)
    large = np.minimum(large, 31)
    return np.where(n < 16, n, large)


def host_inputs(inputs, nlayers=DEPTH):
    f = lambda a: np.ascontiguousarray(np.asarray(a, dtype=np.float32))
    rel_bias = f(inputs["rel_bias"])
    q = np.arange(128)[:, None]
    s = np.arange(128)[None, :]
    relT = np.zeros((128, 16, 128), np.float32)
    for h in range(8):
        for d in range(2):
            relT[:, h * 2 + d, :] = rel_bias[h][_rel_bucket(d * 128 + q - s)]
    relb = np.ascontiguousarray(np.broadcast_to(rel_bias.reshape(1, 256), (128, 256)))
    consts = np.zeros((128, 512), np.float32)
    consts[:, 0:128] = np.eye(128)
    consts[:, 128:256] = np.where(s > q, NEG, 0.0)
    consts[:, 256:384] = np.where(s > q, -1e9, 0.0)
    K = NBIS
    c1 = np.array([2.0 ** -(i + 2) for i in range(K - 1)] + [2.0 ** -K])
    c2 = np.array([2.0 ** -(i + 1) for i in range(K - 1)] + [2.0 ** -K])
    consts[:, 384:384 + K] = c1
    consts[:, 400:400 + K] = c2
    wins = {(0, 0): 2, (1, 0): 4, (0, 1): 8, (1, 1): 16}
    for ph in range(2):
        for c in range(2):
            w = wins[(ph, c)]
            for t in range(16):
                consts[ph * 64:(ph + 1) * 64, 416 + c * 16 + t] = 1.0 / min(t + 1, w)
    g = lambda k: f(inputs[k])
    gpack = np.zeros((DEPTH + 1, 5, D), np.float32)
    for l in range(DEPTH):
        gpack[l + 1, 0] = g("g_mix_post")[l]
        gpack[l + 1, 1] = g("g_ffn_pre")[l]
        gpack[l + 1, 2] = g("g_ffn_post")[l]
        gpack[l + 1, 3] = g("g_ple")[l]
        gpack[l, 4] = g("g_mix_pre")[l]
    shared = dict(
        w_in=g("w_in")[:nlayers], w_out=g("w_out")[:nlayers], w_gate_up=g("w_gate_up")[:nlayers], w_down=g("w_down")[:nlayers],
        w_ple_gate=g("w_ple_gate")[:nlayers], w_ple_proj=g("w_ple_proj")[:nlayers], gpack=gpack,
        convw_t=np.ascontiguousarray(g("conv_w").transpose(0, 2, 1)),
        pool_w=g("pool_w"),
        pscale_t=np.ascontiguousarray(g("pool_scale").reshape(DEPTH, 2, 128).transpose(0, 2, 1)),
        relT=relT, relb=relb, consts=consts,
    )
    x = g("x")
    p = g("p")
    maps = []
    for c in range(8):
        b = c % 4
        m = dict(shared)
        m["x"] = np.ascontiguousarray(x[b])
        m["p"] = np.ascontiguousarray(p[:nlayers, b])
        maps.append(m)
    return maps


_CACHE = {}


def kernel(**inputs):
    if "nc" not in _CACHE:
        _CACHE["nc"] = build()[0]
    nc = _CACHE["nc"]
    maps = host_inputs(inputs)
    res = run_bass_kernel_spmd(nc, maps, core_ids=list(range(8)))
    out = np.stack([np.asarray(res.results[b]["y"], dtype=np.float32) for b in range(4)], axis=0)
    return out
```

```python
import numpy as np
import concourse.bass as bass
import concourse.mybir as mybir
from concourse.bass_utils import run_bass_kernel_spmd

F32, BF16 = mybir.dt.float32, mybir.dt.bfloat16
ALU, AF, AX = mybir.AluOpType, mybir.ActivationFunctionType, mybir.AxisListType

S = 4096
D = 1024
NT = 32
NG = 8
DEPTH = 4
DFF = 2816
INC = 3144
NEG = -30000.0
KTOP = 256
NBIS = 10
ARENA_BYTES = 132 * 1024


class _Stop(Exception):
    pass


class Buf:
    __slots__ = ("name", "w", "r")

    def __init__(self, name):
        self.name = name
        self.w = None
        self.r = {}


class Ins:
    __slots__ = ("eng", "fn", "deps", "sig", "ms", "dma", "slot")


ENGS = ("pe", "act", "dve", "pool", "sp", "poolw")


KSEM = 14
DMAQ = ("sp", "pool", "poolw")


class Prog:
    def __init__(self):
        self.q = {e: [] for e in ENGS}
        self.extra = {e: set() for e in ENGS}
        self.lastdma = {}
        self.ndma = {e: 0 for e in ENGS}
        self.n = 0

    def emit(self, eng, fn, reads=(), writes=(), dma=False):
        ins = Ins()
        ins.eng = eng
        ins.fn = fn
        ins.sig = False
        ins.ms = None
        ins.dma = dma
        ins.slot = None
        deps = set(self.extra[eng])
        self.extra[eng] = set()
        for b in reads:
            if b.w is not None:
                deps.add(b.w)
        for b in writes:
            if b.w is not None:
                deps.add(b.w)
            deps.update(b.r.values())
        if dma:
            i = self.ndma[eng]
            self.ndma[eng] = i + 1
            ins.slot = i % KSEM
            ins.ms = 16 * (i // KSEM + 1)
            prev = self.lastdma.get((eng, ins.slot))
            if prev is not None:
                deps.add(prev)
            self.lastdma[(eng, ins.slot)] = ins
        fd = []
        for d in deps:
            if d is ins:
                continue
            if (not d.dma) and d.eng == eng and eng == "pe":
                continue
            d.sig = True
            fd.append(d)
        ins.deps = fd
        for b in reads:
            b.r[(eng, dma, ins.slot)] = ins
        for b in writes:
            b.w = ins
            b.r = {}
        self.q[eng].append(ins)
        self.n += 1
        return ins

    def generate(self, nc, block, sems):
        for e in ENGS:
            c = 0
            for ins in self.q[e]:
                if (not ins.dma) and ins.sig:
                    c += 1
                    ins.ms = c
        final = {}
        for (e, slot), ins in self.lastdma.items():
            if e != "poolw":
                final[("d", e, slot)] = ins.ms

        def run(e, h, waited):
            for ins in self.q[e]:
                need = {}
                for d in ins.deps:
                    key = ("d", d.eng, d.slot) if d.dma else ("c", d.eng)
                    if need.get(key, 0) < d.ms:
                        need[key] = d.ms
                for key, v in need.items():
                    if waited.get(key, 0) < v:
                        h.wait_ge(sems[key], v)
                        waited[key] = v
                r = ins.fn(h)
                if ins.dma:
                    r.then_inc(sems[("d", e, ins.slot)], 16)
                elif ins.sig:
                    r.then_inc(sems[("c", e)], 1)
            if e == "sp":
                for key, v in final.items():
                    if waited.get(key, 0) < v:
                        h.wait_ge(sems[key], v)

        @block.tensor
        def _(h):
            run("pe", h, {})

        @block.scalar
        def _(h):
            run("act", h, {})

        @block.vector
        def _(h):
            run("dve", h, {})

        @block.gpsimd
        def _(h):
            w = {}
            run("poolw", h, w)
            run("pool", h, w)

        @block.sync
        def _(h):
            run("sp", h, {})


def build(nlayers=DEPTH, stop=None, debug=False, nbis=NBIS):
    nc = bass.Bass("TRN2", target_bir_lowering=False)
    P = Prog()
    skind = "ExternalOutput" if debug else "Internal"

    def din(name, shape, dt=F32):
        return nc.dram_tensor(name, list(shape), dt, kind="ExternalInput").ap()

    def dscr(name, shape, dt):
        return nc.dram_tensor(name, list(shape), dt, kind=skind).ap()

    x_in = din("x", [S, D])
    p_in = din("p", [nlayers, S, 256])
    w_in_d = din("w_in", [nlayers, D, INC])
    w_out_d = din("w_out", [nlayers, D, D])
    w_gu_d = din("w_gate_up", [nlayers, D, 2 * DFF])
    w_dn_d = din("w_down", [nlayers, DFF, D])
    w_pg_d = din("w_ple_gate", [nlayers, D, D])
    w_pp_d = din("w_ple_proj", [nlayers, 256, D])
    gpack_d = din("gpack", [DEPTH + 1, 5, D])
    convw_d = din("convw_t", [DEPTH, 256, 3])
    poolw_d = din("pool_w", [DEPTH, 4, 64, 64])
    pscale_d = din("pscale_t", [DEPTH, 128, 2])
    relT_d = din("relT", [128, 16, 128])
    relb_d = din("relb", [128, 256])
    consts_d = din("consts", [128, 512])
    y_out = nc.dram_tensor("y", [S, D], F32, kind="ExternalOutput").ap()

    wb = {}
    for l in range(nlayers):
        wb[l] = dict(
            w_in=nc.dram_tensor(f"wb_in{l}", [D, INC], BF16, kind="Internal").ap(),
            w_out=nc.dram_tensor(f"wb_out{l}", [D, D], BF16, kind="Internal").ap(),
            w_gu=nc.dram_tensor(f"wb_gu{l}", [D, 2 * DFF], BF16, kind="Internal").ap(),
            w_dn=nc.dram_tensor(f"wb_dn{l}", [DFF, D], BF16, kind="Internal").ap(),
            w_pg=nc.dram_tensor(f"wb_pg{l}", [D, D], BF16, kind="Internal").ap(),
            w_pp=nc.dram_tensor(f"wb_pp{l}", [256, D], BF16, kind="Internal").ap(),
        )
    wsrc = dict(w_in=w_in_d, w_out=w_out_d, w_gu=w_gu_d, w_dn=w_dn_d, w_pg=w_pg_d, w_pp=w_pp_d)
    wbufs = {}

    x_cur = dscr("x_cur", [S, D], F32)
    aT = dscr("aT", [1024, S], F32)
    cqT = dscr("cqT", [256, S], BF16)
    dqT = dscr("dqT", [256, S], BF16)
    iqT = dscr("iqT", [512, S], BF16)
    ckT = dscr("ckT", [256, S], BF16)
    dkT = dscr("dkT", [256, S], BF16)
    ikT = dscr("ikT", [64, S], BF16)
    cvd = dscr("cv", [S, 256], BF16)
    dvd = dscr("dv", [S, 256], BF16)
    iwd = dscr("iw", [S, 8], F32)
    mixT = dscr("mixT", [1024, S], BF16)
    B_z = Buf("zscratch")
    B_mix = Buf("mixT")
    B_xcur = [Buf(f"xcur{g}") for g in range(NG)]

    import contextlib
    es = contextlib.ExitStack()
    with es:
        def sb(name, shape, dt):
            return es.enter_context(nc.sbuf_tensor(name, list(shape), dt))

        arena = sb("arena", [128, ARENA_BYTES // 2], BF16)
        consts = sb("consts_sb", [128, 512], F32)
        ident = sb("ident_bf", [128, 128], BF16)
        I4 = sb("I4", [128, 512], BF16)
        ones64 = sb("ones64", [128, 64], BF16)
        Tb = sb("Tb", [128, 8, 2, 128], BF16)
        relb = sb("relb_sb", [128, 256], F32)
        pwblk = sb("pwblk", [128, 2, 128], BF16)
        cw = sb("cw", [128, 2, 3], F32)
        pscale = sb("pscale", [128, 2], F32)
        small = sb("small", [128, 256], F32)
        bar_s = sb("bar_s", [128, 8], F32)
        WBt = [sb(f"WB{i}", [128, 8, 512], BF16) for i in range(4)]
        B_WB = [Buf(f"WB{i}") for i in range(4)]
        ps = [es.enter_context(nc.psum_tensor(f"ps{i}", [128, 512], F32)) for i in range(8)]
        B_ps = [Buf(f"ps{i}") for i in range(8)]
        B_const = Buf("const")
        B_layerc = Buf("layerconst")
        B_small = Buf("small")
        B_bar = {e: Buf("bar" + e) for e in ENGS}
        B_sm = {}
        B_msbs = [Buf(f"msb{t}") for t in range(4)]

        sem_names = [("c", "pe"), ("c", "act"), ("c", "dve"), ("c", "pool")]
        sem_names += [("d", q_, i_) for q_ in DMAQ for i_ in range(KSEM)]
        sems = {k: es.enter_context(nc.semaphore("s_" + "_".join(str(x_) for x_ in k))) for k in sem_names}
        block = es.enter_context(nc.Block())

        tri_f = consts[:, 128:256]
        trineg = consts[:, 256:384]
        c1 = consts[:, 384:400]
        c2 = consts[:, 400:416]
        invc = consts[:, 416:448].rearrange("p (c t) -> p c t", c=2, t=16)

        class Arena:
            def __init__(self):
                self.off = 0

            def reset(self):
                self.off = 0

            def get(self, name, shape, dt):
                nel = 1
                for s_ in shape[1:]:
                    nel *= s_
                nbytes = nel * (4 if dt == F32 else 2)
                nbytes = (nbytes + 63) // 64 * 64
                assert self.off + nbytes <= ARENA_BYTES, (name, self.off, nbytes)
                ap = arena[:, self.off // 2:(self.off + nbytes) // 2]
                if dt == F32:
                    ap = ap.bitcast(F32)
                ap = ap[0:shape[0], 0:nel]
                if len(shape) == 3:
                    ap = ap.rearrange("p (a b) -> p a b", a=shape[1], b=shape[2])
                elif len(shape) == 4:
                    ap = ap.rearrange("p (a b c) -> p a b c", a=shape[1], b=shape[2], c=shape[3])
                self.off += nbytes
                return ap, Buf(name)

        AR = Arena()

        def MM(out, lhsT, rhs, st, sp_, R, W):
            P.emit("pe", lambda h: h.matmul(out, lhsT=lhsT, rhs=rhs, start=st, stop=sp_), R, W)

        def TRP(out, in_, R, W):
            P.emit("pe", lambda h: h.transpose(out, in_, ident[:]), R, W)

        def ACT(out, in_, func, R, W, scale=None, bias=None, accum=None):
            kw = {}
            if scale is not None:
                kw["scale"] = scale
            if bias is not None:
                kw["bias"] = bias
            if accum is not None:
                kw["accum_out"] = accum
            P.emit("act", lambda h: h.activation(out=out, in_=in_, func=func, **kw), R, W)

        def TS(eng, out, in0, s1, s2, op0, op1, R, W, accum=None):
            kw = {}
            if op1 is not None:
                kw["op1"] = op1
            if accum is not None:
                kw["accum_out"] = accum
            P.emit(eng, lambda h: h.tensor_scalar(out=out, in0=in0, scalar1=s1, scalar2=s2, op0=op0, **kw), R, W)

        def TT(eng, out, in0, in1, op, R, W):
            P.emit(eng, lambda h: h.tensor_tensor(out=out, in0=in0, in1=in1, op=op), R, W)

        def STT(out, in0, scalar, in1, op0, op1, R, W):
            P.emit("dve", lambda h: h.scalar_tensor_tensor(out=out, in0=in0, scalar=scalar, in1=in1, op0=op0, op1=op1), R, W)

        def CP(eng, out, in_, R, W):
            P.emit(eng, lambda h: h.tensor_copy(out, in_), R, W)

        def MSET(eng, ap, val, W):
            P.emit(eng, lambda h: h.memset(ap, val), (), W)

        def RED(out, in_, op, R, W):
            P.emit("dve", lambda h: h.tensor_reduce(out=out, in_=in_, axis=AX.X, op=op), R, W)

        def DMA(q, out, in_, R, W):
            return P.emit(q, lambda h: h.dma_start(out=out, in_=in_), R, W, dma=True)

        def barrier():
            t = []
            t.append(P.emit("pe", lambda h: h.matmul(ps[5][0:1, 0:1], lhsT=ones64[0:1, 0:1], rhs=ones64[0:1, 0:1], start=True, stop=True),
                            (B_const,), (B_ps[5], B_bar["pe"])))
            t.append(P.emit("act", lambda h: h.activation(out=bar_s[0:1, 0:1], in_=bar_s[0:1, 4:5], func=AF.Copy), (B_const,), (B_bar["act"],)))
            t.append(P.emit("dve", lambda h: h.tensor_copy(bar_s[0:1, 1:2], bar_s[0:1, 5:6]), (B_const,), (B_bar["dve"],)))
            t.append(P.emit("pool", lambda h: h.tensor_copy(bar_s[0:1, 2:3], bar_s[0:1, 6:7]), (B_const,), (B_bar["pool"],)))
            for i in t:
                i.sig = True
            allb = set(t) | set(v for k, v in P.lastdma.items() if k[0] != "poolw")
            for e in ENGS:
                if e != "poolw":
                    P.extra[e] |= allb

        DMA("sp", consts[:], consts_d, (), (B_const,))
        DMA("sp", relb[:], relb_d, (), (B_const,))
        DMA("pool", ident[:], consts_d[:, 0:128], (), (B_const,))
        for i in range(4):
            DMA("pool", I4[:, i * 128:(i + 1) * 128], consts_d[:, 0:128], (), (B_const,))
        MSET("pool", ones64[:], 1.0, (B_const,))
        MSET("pool", pwblk[:], 0.0, (B_const,))
        MSET("pool", bar_s[:], 0.0, (B_const,))
        AR.reset()
        relT_sb, B_relT = AR.get("relT", [128, 16, 128], F32)
        DMA("sp", relT_sb, relT_d, (), (B_relT,))
        for h_ in range(8):
            b31 = relb[:, h_ * 32 + 31:h_ * 32 + 32]
            STT(Tb[:, h_, 0, :], relT_sb[:, h_ * 2, :], b31, tri_f, ALU.subtract, ALU.add, (B_relT, B_const), (B_const,))
            TS("dve", Tb[:, h_, 1, :], relT_sb[:, h_ * 2 + 1, :], b31, None, ALU.subtract, None, (B_relT, B_const), (B_const,))

        barrier()
        for l in range(nlayers):
            for nm in ("w_in", "w_out", "w_gu", "w_dn", "w_pg", "w_pp"):
                src = wsrc[nm][l]
                dst = wb[l][nm]
                rows = src.shape[0]
                step = 256
                bl = []
                for r0 in range(0, rows, step):
                    r1 = min(rows, r0 + step)
                    b = Buf(f"wb{l}{nm}{r0}")
                    DMA("poolw", dst[r0:r1, :], src[r0:r1, :], (), (b,))
                    bl.append(b)
                wbufs[(l, nm)] = bl

        class WStream:
            def __init__(self):
                self.plan = []
                self.issued = 0
                self.taken = 0

            def add(self, l, nm, view_fn, shape):
                self.plan.append((l, nm, view_fn, shape))

            def _issue(self):
                i = self.issued
                if i >= len(self.plan):
                    return
                l, nm, view_fn, shape = self.plan[i]
                slot = i % 4
                dst = WBt[slot][:]
                for dstv, srcv in view_fn(dst, wb[l][nm]):
                    DMA("sp", dstv, srcv, tuple(wbufs[(l, nm)]), (B_WB[slot],))
                self.issued += 1

            def start(self):
                while self.issued < min(3, len(self.plan)):
                    self._issue()

            def get(self):
                i = self.taken
                assert i < self.issued, "weight stream underflow"
                slot = i % 4
                self.taken += 1
                self._issue()
                return WBt[slot], B_WB[slot]

        WS = WStream()

        def wv_cols(k0, nk, c0, c1):
            def f(dst, w):
                return [(dst[:, 0:nk, 0:c1 - c0],
                         w[k0 * 128:(k0 + nk) * 128, c0:c1].rearrange("(k p) c -> p k c", p=128))]
            return f

        def wv_gu(gb):
            def f(dst, w):
                w3 = w.rearrange("(k p) c -> p k c", p=128)
                return [(dst[:, :, 0:256], w3[:, :, gb * 256:(gb + 1) * 256]),
                        (dst[:, :, 256:512], w3[:, :, DFF + gb * 256:DFF + (gb + 1) * 256])]
            return f

        IN_BLOCKS = [(0, 512), (512, 1024), (1024, 1536), (1536, 2048), (2048, 2376), (2376, 2888), (2888, 3144)]

        def plan_A(l):
            for (c0, c1) in IN_BLOCKS:
                WS.add(l, "w_in", wv_cols(0, 8, c0, c1), None)

        def plan_dense(l, with_A):
            for c in range(2):
                WS.add(l, "w_out", wv_cols(0, 8, c * 512, (c + 1) * 512), None)
            for gb in range(11):
                WS.add(l, "w_gu", wv_gu(gb), None)
            for c in range(2):
                for (k0, nk) in ((0, 8), (8, 8), (16, 6)):
                    WS.add(l, "w_dn", wv_cols(k0, nk, c * 512, (c + 1) * 512), None)
            for c in range(2):
                WS.add(l, "w_pg", wv_cols(0, 8, c * 512, (c + 1) * 512), None)
            if with_A:
                plan_A(l + 1)

        for g in range(NG):
            plan_A(0)
        if stop != "A0":
            for l in range(nlayers):
                if stop is not None and stop.startswith("B1a0"):
                    break
                for g in range(NG):
                    plan_dense(l, l + 1 < nlayers)
                if stop is not None and stop.startswith("dense0"):
                    break
        WS.start()

        class Rot:
            def __init__(self, idx):
                self.idx = idx
                self.i = 0

            def get(self):
                k = self.idx[self.i % len(self.idx)]
                self.i += 1
                return ps[k], B_ps[k]

        def load_gains(pidx, gB, B_g):
            DMA("sp", gB.rearrange("p a b -> p (a b)"),
                gpack_d[pidx:pidx + 1].rearrange("o a b -> o (a b)").partition_broadcast(128), (), (B_g,))

        def rstd_from(ssq_ap, out_ap, n_inv, R, W):
            ACT(out_ap, ssq_ap, AF.Sqrt, R, W, scale=n_inv, bias=1e-6)
            P.emit("dve", lambda h: h.reciprocal(out=out_ap, in_=out_ap), W, W)

        def norm_T(xg, B_xg, gB, B_g, gslot, hT, B_hT, hb, B_hb, junk, B_junk, rotT, sc0):
            for t in range(4):
                ssq = small[:, sc0 + t:sc0 + t + 1]
                rs = small[:, sc0 + 4 + t:sc0 + 5 + t]
                B_s_ = B_sm.setdefault(("n", t), Buf(f"smn{t}"))
                ACT(junk[:, 0:1024], xg[:, t, :], AF.Square, (B_xg,), (B_junk, B_s_), accum=ssq)
                rstd_from(ssq, rs, 1.0 / D, (B_s_,), (B_s_,))
                hbt = hb[t % 2]
                STT(hbt, xg[:, t, :], rs, gB[:, gslot, :], ALU.mult, ALU.mult, (B_xg, B_s_, B_g), (B_hb[t % 2],))
                pt, B_pt = rotT.get()
                ptb = pt[:].bitcast(BF16)
                for k in range(8):
                    TRP(ptb[:, k * 128:(k + 1) * 128], hbt[:, k * 128:(k + 1) * 128], (B_hb[t % 2], B_const), (B_pt,))
                ACT(hT[:, :, t * 128:(t + 1) * 128], ptb.rearrange("p (k q) -> p k q", k=8, q=128), AF.Copy, (B_pt,), (B_hT,))

        def tm_postnorm(lhs_fn, B_lhs, nkc_list, gB, B_g, gslot, xg, B_xg, msb, B_msb, junk, B_junk, rotM, sc0):
            for c in range(2):
                accs = [rotM.get() for _ in range(4)]
                nblk = len(nkc_list)
                kbase = 0
                for bi, nk in enumerate(nkc_list):
                    wt, B_w = WS.get()
                    for t in range(4):
                        pa, B_pa = accs[t]
                        for k in range(nk):
                            MM(pa[:, :], lhs_fn(kbase + k, t), wt[:, k, :], (bi == 0 and k == 0), (bi == nblk - 1 and k == nk - 1),
                               (B_lhs, B_w), (B_pa,))
                    kbase += nk
                if stop == "dense0_1a":
                    raise _Stop()
                for t in range(4):
                    pa, B_pa = accs[t]
                    B_s_ = B_sm.setdefault(("p", t), Buf(f"smp{t}"))
                    CP("dve", msb[:, t, c * 512:(c + 1) * 512], pa[:, :], (B_pa,), (B_msbs[t],))
                    ACT(junk[:, 0:512], msb[:, t, c * 512:(c + 1) * 512], AF.Square, (B_msbs[t],), (B_junk, B_s_),
                        accum=small[:, sc0 + t * 2 + c:sc0 + t * 2 + c + 1])
                if stop == "dense0_1b":
                    raise _Stop()
            if stop == "dense0_1c":
                raise _Stop()
            for t in range(4):
                B_s_ = B_sm[("p", t)]
                tot = small[:, sc0 + 8 + t:sc0 + 9 + t]
                TT("dve", tot, small[:, sc0 + t * 2:sc0 + t * 2 + 1], small[:, sc0 + t * 2 + 1:sc0 + t * 2 + 2], ALU.add, (B_s_,), (B_s_,))
                rs = small[:, sc0 + 12 + t:sc0 + 13 + t]
                rstd_from(tot, rs, 1.0 / D, (B_s_,), (B_s_,))
                STT(msb[:, t, :], msb[:, t, :], rs, gB[:, gslot, :], ALU.mult, ALU.mult, (B_msbs[t], B_s_, B_g), (B_msbs[t],))
                TT("pool", xg[:, t, :], xg[:, t, :], msb[:, t, :], ALU.add, (B_xg, B_msbs[t]), (B_xg,))

        evac_flip = [0]

        def evac(out, in_, R, W, scale=None):
            evac_flip[0] ^= 1
            if evac_flip[0]:
                ACT(out, in_, AF.Copy, R, W, scale=scale)
            else:
                if scale is None:
                    CP("dve", out, in_, R, W)
                else:
                    TS("dve", out, in_, scale, None, ALU.mult, None, R, W)

        def phase_A(l, g, hT, B_hT, stf, B_stf, stb, B_stb, rotM):
            tok0 = g * 512
            cnt = [0]

            def fm(wt, B_w, cl0, cl1, dst_ap, scale=None, f32=False):
                M = cl1 - cl0
                pa, B_pa = rotM.get()
                for k in range(8):
                    MM(pa[0:M, :], wt[:, k, cl0:cl1], hT[:, k, :], k == 0, k == 7, (B_w, B_hT), (B_pa,))
                i = cnt[0] % 3
                cnt[0] += 1
                if f32:
                    st_, B_st = stf[i], B_stf[i]
                else:
                    st_, B_st = stb[i], B_stb[i]
                evac(st_[0:M, :], pa[0:M, :], (B_pa,), (B_st,), scale=scale)
                DMA("sp", dst_ap, st_[0:M, :], (B_st,), (B_z,))

            def tmm(wt, B_w, cl0, cl1, dst_fn, f32=False):
                ncol = cl1 - cl0
                for t in range(4):
                    pa, B_pa = rotM.get()
                    for k in range(8):
                        MM(pa[:, 0:ncol], hT[:, k, t * 128:(t + 1) * 128], wt[:, k, cl0:cl1], k == 0, k == 7, (B_w, B_hT), (B_pa,))
                    i = cnt[0] % 3
                    cnt[0] += 1
                    if f32:
                        st_, B_st = stf[i], B_stf[i]
                    else:
                        st_, B_st = stb[i], B_stb[i]
                    evac(st_[:, 0:ncol], pa[:, 0:ncol], (B_pa,), (B_st,))
                    DMA("sp", dst_fn(tok0 + t * 128), st_[:, 0:ncol], (B_st,), (B_z,))

            cols = slice(tok0, tok0 + 512)
            for bi in range(2):
                wt, B_w = WS.get()
                for j in range(4):
                    r0 = bi * 512 + j * 128
                    fm(wt, B_w, j * 128, (j + 1) * 128, aT[r0:r0 + 128, cols], f32=True)
            wt, B_w = WS.get()
            fm(wt, B_w, 0, 128, cqT[0:128, cols], scale=0.125)
            fm(wt, B_w, 128, 256, cqT[128:256, cols], scale=0.125)
            fm(wt, B_w, 256, 384, ckT[0:128, cols])
            fm(wt, B_w, 384, 512, ckT[128:256, cols])
            wt, B_w = WS.get()
            tmm(wt, B_w, 0, 256, lambda r: cvd[r:r + 128, :])
            fm(wt, B_w, 256, 384, iqT[0:128, cols])
            fm(wt, B_w, 384, 512, iqT[128:256, cols])
            wt, B_w = WS.get()
            fm(wt, B_w, 0, 128, iqT[256:384, cols])
            fm(wt, B_w, 128, 256, iqT[384:512, cols])
            fm(wt, B_w, 256, 320, ikT[0:64, cols])
            tmm(wt, B_w, 320, 328, lambda r: iwd[r:r + 128, :], f32=True)
            wt, B_w = WS.get()
            fm(wt, B_w, 0, 128, dqT[0:128, cols], scale=0.125)
            fm(wt, B_w, 128, 256, dqT[128:256, cols], scale=0.125)
            fm(wt, B_w, 256, 384, dkT[0:128, cols])
            fm(wt, B_w, 384, 512, dkT[128:256, cols])
            wt, B_w = WS.get()
            tmm(wt, B_w, 0, 256, lambda r: dvd[r:r + 128, :])

        def dense_arena():
            AR.reset()
            A = {}
            A["xg"] = AR.get("xg", [128, 4, 1024], F32)
            A["gB"] = AR.get("gB", [128, 5, 1024], F32)
            A["hb0"] = AR.get("hb0", [128, 1024], BF16)
            A["hb1"] = AR.get("hb1", [128, 1024], BF16)
            A["hT0"] = AR.get("hT0", [128, 8, 512], BF16)
            A["hT1"] = AR.get("hT1", [128, 8, 512], BF16)
            A["actT"] = AR.get("actT", [128, 22, 512], BF16)
            A["mixg"] = AR.get("mixg", [128, 8, 512], BF16)
            A["msb"] = AR.get("msb", [128, 4, 1024], F32)
            A["junk"] = AR.get("junk", [128, 1024], BF16)
            A["pg"] = AR.get("pg", [128, 4, 256], BF16)
            A["pT"] = AR.get("pT", [128, 2, 512], BF16)
            A["wpp"] = AR.get("wpp", [128, 2, 2, 512], BF16)
            A["sg0"] = AR.get("sg0", [128, 512], F32)
            A["sg1"] = AR.get("sg1", [128, 512], F32)
            for i in range(3):
                A[f"stf{i}"] = AR.get(f"stf{i}", [128, 512], F32)
                A[f"stb{i}"] = AR.get(f"stb{i}", [128, 512], BF16)
            return A

        def run_A0():
            A = dense_arena()
            xg, B_xg = A["xg"]
            gB, B_g = A["gB"]
            load_gains(0, gB, B_g)
            rotT = Rot([0, 1])
            rotM = Rot([2, 3, 4, 5, 6, 7])
            hTs = [A["hT0"], A["hT1"]]
            hb = [A["hb0"][0], A["hb1"][0]]
            B_hb = [A["hb0"][1], A["hb1"][1]]
            junk, B_junk = A["junk"]
            stf = [A[f"stf{i}"][0] for i in range(3)]
            B_stf = [A[f"stf{i}"][1] for i in range(3)]
            stb = [A[f"stb{i}"][0] for i in range(3)]
            B_stb = [A[f"stb{i}"][1] for i in range(3)]
            for g in range(NG):
                DMA("sp", xg, x_in[g * 512:(g + 1) * 512, :].rearrange("(t p) d -> p t d", p=128), (), (B_xg,))
                hT, B_hT = hTs[g % 2]
                norm_T(xg, B_xg, gB, B_g, 4, hT, B_hT, hb, B_hb, junk, B_junk, rotT, 0)
                phase_A(0, g, hT, B_hT, stf, B_stf, stb, B_stb, rotM)

        def run_dense(l):
            last = (l == DEPTH - 1)
            A = dense_arena()
            xg, B_xg = A["xg"]
            gB, B_g = A["gB"]
            load_gains(l + 1, gB, B_g)
            rotT = Rot([0, 1])
            rotM = Rot([2, 3, 4, 5, 6, 7])
            hTs = [A["hT0"], A["hT1"]]
            hb = [A["hb0"][0], A["hb1"][0]]
            B_hb = [A["hb0"][1], A["hb1"][1]]
            junk, B_junk = A["junk"]
            actT, B_actT = A["actT"]
            mixg, B_mixg = A["mixg"]
            msb, B_msb = A["msb"]
            pg, B_pg = A["pg"]
            pT, B_pT = A["pT"]
            sg = [A["sg0"][0], A["sg1"][0]]
            B_sg = [A["sg0"][1], A["sg1"][1]]
            stf = [A[f"stf{i}"][0] for i in range(3)]
            B_stf = [A[f"stf{i}"][1] for i in range(3)]
            stb = [A[f"stb{i}"][0] for i in range(3)]
            B_stb = [A[f"stb{i}"][1] for i in range(3)]
            xsrc = x_in if l == 0 else x_cur
            xdst = y_out if last else x_cur
            for g in range(NG):
                rows = slice(g * 512, (g + 1) * 512)
                rd = () if l == 0 else (B_xcur[g],)
                DMA("sp", xg, xsrc[rows, :].rearrange("(t p) d -> p t d", p=128), rd, (B_xg,))
                if stop == "dense0_0a":
                    return
                DMA("sp", mixg, mixT[:, rows].rearrange("(k p) s -> p k s", p=128), (B_mix,), (B_mixg,))
                if stop == "dense0_0b":
                    return
                DMA("pool", pg, p_in[l, rows, :].rearrange("(t p) c -> p t c", p=128), (), (B_pg,))
                if stop == "dense0_0":
                    return
                tm_postnorm(lambda k, t: mixg[:, k, t * 128:(t + 1) * 128], B_mixg, [8], gB, B_g, 0, xg, B_xg, msb, B_msb,
                            junk, B_junk, rotM, 16)
                if stop == 'dense0_1':
                    return
                hT, B_hT = hTs[0]
                norm_T(xg, B_xg, gB, B_g, 1, hT, B_hT, hb, B_hb, junk, B_junk, rotT, 0)
                if stop == 'dense0_2':
                    return
                for gb in range(11):
                    wt, B_w = WS.get()
                    for sc in range(2):
                        pg_, B_pg_ = rotM.get()
                        pu_, B_pu_ = rotM.get()
                        for k in range(8):
                            MM(pg_[:, :], wt[:, k, sc * 128:(sc + 1) * 128], hT[:, k, :], k == 0, k == 7, (B_w, B_hT), (B_pg_,))
                        for k in range(8):
                            MM(pu_[:, :], wt[:, k, 256 + sc * 128:256 + (sc + 1) * 128], hT[:, k, :], k == 0, k == 7, (B_w, B_hT), (B_pu_,))
                        ci = gb * 2 + sc
                        s_, B_s = sg[ci % 2], B_sg[ci % 2]
                        ACT(s_, pg_[:, :], AF.Silu, (B_pg_,), (B_s,))
                        TT("dve", actT[:, ci, :], s_, pu_[:, :], ALU.mult, (B_s, B_pu_), (B_actT,))
                if stop == 'dense0_3':
                    return
                tm_postnorm(lambda k, t: actT[:, k, t * 128:(t + 1) * 128], B_actT, [8, 8, 6], gB, B_g, 2, xg, B_xg, msb, B_msb,
                            junk, B_junk, rotM, 16)
                if stop == 'dense0_4':
                    return
                hT, B_hT = hTs[1]
                norm_T(xg, B_xg, gB, B_g, 3, hT, B_hT, hb, B_hb, junk, B_junk, rotT, 0)
                for t in range(4):
                    pt, B_pt = rotT.get()
                    ptb = pt[:].bitcast(BF16)
                    for k in range(2):
                        TRP(ptb[:, k * 128:(k + 1) * 128], pg[:, t, k * 128:(k + 1) * 128], (B_pg, B_const), (B_pt,))
                    CP("dve", pT[:, :, t * 128:(t + 1) * 128], ptb[:, 0:256].rearrange("p (k q) -> p k q", k=2, q=128), (B_pt,), (B_pT,))
                wpp, B_wpp = A["wpp"]
                for u_ in range(2):
                    DMA("sp", wpp[:, :, u_, :], wb[l]["w_pp"].rearrange("(k p) c -> p k c", p=128)[:, :, u_ * 512:(u_ + 1) * 512],
                        tuple(wbufs[(l, "w_pp")]), (B_wpp,))
                for c in range(2):
                    wt, B_w = WS.get()
                    for t in range(4):
                        pa, B_pa = rotM.get()
                        pb, B_pb = rotM.get()
                        for k in range(8):
                            MM(pa[:, :], hT[:, k, t * 128:(t + 1) * 128], wt[:, k, :], k == 0, k == 7, (B_hT, B_w), (B_pa,))
                        for k in range(2):
                            MM(pb[:, :], pT[:, k, t * 128:(t + 1) * 128], wpp[:, k, c, :], k == 0, k == 1, (B_pT, B_wpp), (B_pb,))
                        i = (c * 4 + t) % 2
                        ACT(sg[i], pa[:, :], AF.Sigmoid, (B_pa,), (B_sg[i],))
                        TT("dve", sg[i], sg[i], pb[:, :], ALU.mult, (B_sg[i], B_pb), (B_sg[i],))
                        TT("pool", xg[:, t, c * 512:(c + 1) * 512], xg[:, t, c * 512:(c + 1) * 512], sg[i], ALU.add, (B_xg, B_sg[i]), (B_xg,))
                if stop == 'dense0_5':
                    return
                DMA("sp", xdst[rows, :].rearrange("(t p) d -> p t d", p=128), xg, (B_xg,), () if last else (B_xcur[g],))
                if l + 1 < nlayers:
                    hT, B_hT = hTs[0]
                    norm_T(xg, B_xg, gB, B_g, 4, hT, B_hT, hb, B_hb, junk, B_junk, rotT, 0)
                    phase_A(l + 1, g, hT, B_hT, stf, B_stf, stb, B_stb, rotM)

        def run_B1a(l):
            AR.reset()
            ckS, B_ck = AR.get("ckS", [128, 2, S], BF16)
            cvS, B_cv = AR.get("cvS", [128, 32, 256], BF16)
            ikS, B_ik = AR.get("ikS", [128, S], BF16)
            dkS, B_dk = AR.get("dkS", [128, 2, S], BF16)
            dvS, B_dv = AR.get("dvS", [128, 32, 256], BF16)
            scores, B_sc = AR.get("scores", [128, S], F32)
            maskb, B_mk = AR.get("maskb", [128, S], BF16)
            kmf, B_kmf = AR.get("kmf", [128, 32], F32)
            kmT, B_km = AR.get("kmT", [128, 2, 16], BF16)
            mark = AR.off
            DMA("sp", ckS, ckT.rearrange("(hh p) s -> p hh s", p=128), (B_z,), (B_ck,))
            DMA("sp", dkS, dkT.rearrange("(hh p) s -> p hh s", p=128), (B_z,), (B_dk,))
            DMA("sp", ikS[0:64, :], ikT, (B_z,), (B_ik,))
            DMA("sp", ikS[64:128, :], ikT, (B_z,), (B_ik,))
            for n0 in range(0, 32, 4):
                DMA("sp", cvS[:, n0:n0 + 4, :], cvd[n0 * 128:(n0 + 4) * 128, :].rearrange("(n p) c -> p n c", p=128), (B_z,), (B_cv,))
                DMA("sp", dvS[:, n0:n0 + 4, :], dvd[n0 * 128:(n0 + 4) * 128, :].rearrange("(n p) c -> p n c", p=128), (B_z,), (B_dv,))
            DMA("sp", cw[:], convw_d[l].rearrange("(c p) j -> p c j", p=128), (), (B_layerc,))
            DMA("sp", pscale[:], pscale_d[l], (), (B_layerc,))
            for c in range(2):
                for gl in range(2):
                    DMA("pool", pwblk[gl * 64:(gl + 1) * 64, c, gl * 64:(gl + 1) * 64], poolw_d[l, 2 * c + gl], (), (B_layerc,))
            RED(kmf, dkS.rearrange("p hh (n s) -> p (hh n) s", n=16, s=256), ALU.add, (B_dk,), (B_kmf,))
            CP("dve", kmT.rearrange("p a b -> p (a b)"), kmf, (B_kmf,), (B_km,))

            if stop == "B1a0_ld":
                return
            CH = 256
            W_ = CH + 16
            cp = {}
            for nm in ("ain", "ac", "pv", "hh", "yy", "T1", "T2", "T3", "T4"):
                cp[nm] = AR.get(nm, [128, 2, W_], F32)
            cp["ab"] = AR.get("ab", [128, 2, CH], F32)
            cp["d"] = AR.get("d", [128, 2, CH], BF16)
            cp["ya"] = AR.get("ya", [128, 2, CH], BF16)
            cp["yb"] = AR.get("yb", [128, 2, CH], BF16)
            cp["t16"] = AR.get("t16", [128, 2, 16], F32)
            ain, B_ain = cp["ain"]
            ac, B_ac = cp["ac"]
            pv, B_pv = cp["pv"]
            hh, B_hh = cp["hh"]
            yy, B_yy = cp["yy"]
            T1, B_T1 = cp["T1"]
            T2, B_T2 = cp["T2"]
            T3, B_T3 = cp["T3"]
            T4, B_T4 = cp["T4"]
            ab, B_ab = cp["ab"]
            dd, B_dd = cp["d"]
            ya, B_ya = cp["ya"]
            yb, B_yb = cp["yb"]
            t16, B_t16 = cp["t16"]
            pcv, B_pcv = ps[6], B_ps[6]
            for ci in range(S // CH):
                c0 = ci * CH

                def ld(dst, r0, halo):
                    if halo:
                        if ci == 0:
                            return [("m", dst[:, :, 0:16]), ("d", dst[:, :, 16:W_], aT[r0:r0 + 256, 0:CH])]
                        return [("d", dst[:, :, :], aT[r0:r0 + 256, c0 - 16:c0 + CH])]
                    return [("d", dst[:, :, :], aT[r0:r0 + 256, c0:c0 + CH])]

                for (dst, B_d, r0, halo) in ((ain, B_ain, 0, True), (ac, B_ac, 256, True), (ab, B_ab, 512, False), (pv, B_pv, 768, True)):
                    for op in ld(dst, r0, halo):
                        if op[0] == "m":
                            MSET("pool", op[1], 0.0, (B_d,))
                        else:
                            DMA("sp", op[1], op[2].rearrange("(c p) t -> p c t", p=128), (B_z,), (B_d,))
                TT("pool", hh, ain, ac, ALU.mult, (B_ain, B_ac), (B_hh,))
                for c in range(2):
                    TS("dve", yy[:, c, 0:CH], hh[:, c, 16:W_], cw[:, c, 2:3], None, ALU.mult, None, (B_hh, B_layerc), (B_yy,))
                    STT(yy[:, c, 0:CH], hh[:, c, 15:W_ - 1], cw[:, c, 1:2], yy[:, c, 0:CH], ALU.mult, ALU.add, (B_hh, B_layerc, B_yy), (B_yy,))
                    STT(yy[:, c, 0:CH], hh[:, c, 14:W_ - 2], cw[:, c, 0:1], yy[:, c, 0:CH], ALU.mult, ALU.add, (B_hh, B_layerc, B_yy), (B_yy,))
                TT("pool", ya, yy[:, :, 0:CH], ab, ALU.mult, (B_yy, B_ab), (B_ya,))
                DMA("sp", mixT[0:256, c0:c0 + CH].rearrange("(c p) t -> p c t", p=128), ya, (B_ya,), (B_mix,))
                TT("pool", T1[:, :, 1:W_], pv[:, :, 1:W_], pv[:, :, 0:W_ - 1], ALU.add, (B_pv,), (B_T1,))
                TT("pool", T2[:, :, 3:W_], T1[:, :, 3:W_], T1[:, :, 1:W_ - 2], ALU.add, (B_T1,), (B_T2,))
                TT("pool", T3[:, 1, 7:W_], T2[:, 1, 7:W_], T2[:, 1, 3:W_ - 4], ALU.add, (B_T2,), (B_T3,))
                TT("pool", T4[64:128, 1, 15:W_], T3[64:128, 1, 15:W_], T3[64:128, 1, 7:W_ - 8], ALU.add, (B_T3,), (B_T4,))
                grp = ((T1, B_T1, 0, 64, 0, 0.5), (T2, B_T2, 64, 128, 0, 0.25), (T3, B_T3, 0, 64, 1, 0.125), (T4, B_T4, 64, 128, 1, 0.0625))
                for (Tg, B_Tg, p0, p1, c, iw_) in grp:
                    STT(dd[p0:p1, c, :], Tg[p0:p1, c, 16:W_], iw_, pv[p0:p1, c, 16:W_], ALU.mult, ALU.subtract, (B_Tg, B_pv), (B_dd,))
                    if ci == 0:
                        TT("dve", t16[p0:p1, c, :], Tg[p0:p1, c, 16:32], invc[p0:p1, c, :], ALU.mult, (B_Tg, B_const), (B_t16,))
                        TT("dve", dd[p0:p1, c, 0:16], t16[p0:p1, c, :], pv[p0:p1, c, 16:32], ALU.subtract, (B_t16, B_pv), (B_dd,))
                for c in range(2):
                    MM(pcv[:, c * CH:(c + 1) * CH], pwblk[:, c, :], dd[:, c, :], c == 0, c == 1, (B_layerc, B_dd, B_const), (B_pcv,))
                for c in range(2):
                    ACT(yb[:, c, :], pcv[:, c * CH:(c + 1) * CH], AF.Copy, (B_pcv, B_layerc), (B_yb,), scale=pscale[:, c:c + 1])
                DMA("sp", mixT[256:512, c0:c0 + CH].rearrange("(c p) t -> p c t", p=128), yb, (B_yb,), (B_mix,))

            if stop == "B1a0_cp":
                return
            barrier()
            AR.off = mark
            iqj = [AR.get(f"iqj{i}", [128, 8, 128], BF16) for i in range(2)]
            cqj = [AR.get(f"cqj{i}", [128, 4, 128], BF16) for i in range(2)]
            dqj = [AR.get(f"dqj{i}", [128, 4, 128], BF16) for i in range(2)]
            for lst in (iqj, cqj, dqj):
                for (ap_, b_) in lst:
                    MSET("pool", ap_, 0.0, (b_,))
            iwj = [AR.get(f"iwj{i}", [128, 8], F32) for i in range(2)]
            rsb = [AR.get(f"rsb{i}", [128, 512], F32) for i in range(2)]
            PTs = [AR.get(f"PT{i}", [128, 512], BF16) for i in range(3)]
            rden, B_rden = AR.get("rden", [64, 512], F32)
            outb = [AR.get(f"outb{i}", [64, 4, 128], BF16) for i in range(2)]
            st_, B_st = AR.get("tk", [128, 64], F32)
            Gt, B_G = AR.get("G", [128, 4, 16], F32)
            top8, B_t8 = AR.get("top8", [128, 4, 8], F32)
            mbias, B_mb = AR.get("mbias", [128, 4, 16], BF16)
            ptc = [0]

            def attn_tile(kS, B_k, vS, B_v, qj, B_q, stl, head0, j, O, B_O, Dn, B_Dn, Lrot, first, lastt, extra_ops):
                L, B_L = Lrot.get()
                ops = []
                for h_ in range(4):
                    ops.append((h_ * 128, (h_ + 1) * 128, kS[:, h_ // 2, stl * 128:(stl + 1) * 128], qj[:, h_, :], (B_k, B_q)))
                ops.extend(extra_ops(stl))
                for dlt in (0, 1):
                    if stl == j - dlt:
                        for h_ in range(4):
                            ops.append((h_ * 128, (h_ + 1) * 128, Tb[:, head0 + h_, dlt, :], ident[:], (B_const,)))
                firstw = {}
                lastw = {}
                for oi, op in enumerate(ops):
                    for r_ in range(op[0] // 128, op[1] // 128):
                        firstw.setdefault(r_, oi)
                        lastw[r_] = oi
                for oi, op in enumerate(ops):
                    MM(L[:, op[0]:op[1]], op[2], op[3], oi == 0, oi == len(ops) - 1, op[4], (B_L,))
                PT, B_PT = PTs[ptc[0] % 3]
                ptc[0] += 1
                ACT(PT, L[:, :], AF.Exp, (B_L,), (B_PT,))
                for h_ in range(4):
                    MM(O[0:64, h_ * 128:(h_ + 1) * 128], vS[:, stl, h_ * 64:(h_ + 1) * 64], PT[:, h_ * 128:(h_ + 1) * 128],
                       first and h_ == 0, lastt and h_ == 3, (B_v, B_PT), (B_O,))
                MM(Dn[0:64, :], ones64[:, :], PT, first, lastt, (B_const, B_PT), (B_Dn,))

            def finish(O, B_O, Dn, B_Dn, ob, B_ob, row0, j):
                P.emit("dve", lambda h: h.reciprocal(out=rden, in_=Dn[0:64, :]), (B_Dn,), (B_rden,))
                TT("dve", ob.rearrange("p a b -> p (a b)"), O[0:64, :], rden, ALU.mult, (B_O, B_rden), (B_ob,))
                DMA("sp", mixT[row0:row0 + 256, j * 128:(j + 1) * 128].rearrange("(h d) q -> d h q", d=64), ob, (B_ob,), (B_mix,))

            Lrot = Rot([0, 1])
            Srot = Rot([4, 5])
            def q_loads(j):
                jj = j % 2
                qcols = slice(j * 128, (j + 1) * 128)
                iq_, B_iq = iqj[jj]
                cq_, B_cq = cqj[jj]
                dq_, B_dq = dqj[jj]
                iw_, B_iw = iwj[jj]
                for e_ in range(2):
                    pr_ = slice(e_ * 64, (e_ + 1) * 64)
                    for (dst_, Bd_, src_) in ((iq_, B_iq, iqT), (cq_, B_cq, cqT), (dq_, B_dq, dqT)):
                        DMA("sp", dst_.rearrange("p (hh e) q -> p hh e q", e=2)[pr_, :, e_, :],
                            src_[:, qcols].rearrange("(hh e d) q -> e d hh q", e=2, d=64)[e_], (B_z,), (Bd_,))
                DMA("sp", iw_, iwd[qcols, :], (B_z,), (B_iw,))

            ric = [0]

            def indexer_units(j):
                jj = j % 2
                iq_, B_iq = iqj[jj]
                iw_, B_iw = iwj[jj]
                nv = (j + 1) * 128
                n512 = (j + 4) // 4
                units = []
                for sc_ in range(n512):
                    wdt = min(512, nv - sc_ * 512)
                    for h_ in range(8):
                        def unit(sc_=sc_, h_=h_, wdt=wdt):
                            pS, B_pS = Srot.get()
                            MM(pS[:, 0:wdt], iq_[:, h_, :], ikS[:, sc_ * 512:sc_ * 512 + wdt], True, True, (B_iq, B_ik), (B_pS,))
                            r_, B_r = rsb[ric[0] % 2]
                            ric[0] += 1
                            ACT(r_[:, 0:wdt], pS[:, 0:wdt], AF.Relu, (B_pS,), (B_r,))
                            dst = scores[:, sc_ * 512:sc_ * 512 + wdt]
                            if h_ == 0:
                                TS("dve", dst, r_[:, 0:wdt], iw_[:, 0:1], None, ALU.mult, None, (B_r, B_iw), (B_sc,))
                            else:
                                STT(dst, r_[:, 0:wdt], iw_[:, h_:h_ + 1], dst, ALU.mult, ALU.add, (B_r, B_iw, B_sc), (B_sc,))
                        units.append(unit)
                return units

            q_loads(0)
            for u_ in indexer_units(0):
                u_()
            for j in range(NT):
                jj = j % 2
                cq_, B_cq = cqj[jj]
                dq_, B_dq = dqj[jj]
                ns = j + 1
                nv = ns * 128
                obk = j // 2
                if obk > 0:
                    pG, B_pG = Srot.get()
                    for h_ in range(4):
                        MM(pG[:, h_ * 16:(h_ + 1) * 16], dq_[:, h_, :], kmT[:, h_ // 2, :], h_ == 0, h_ == 3, (B_dq, B_km), (B_pG,))
                    MSET("dve", Gt[:], -1e9, (B_G,))
                    CP("dve", Gt[:, :, 0:obk], pG[:, 0:64].rearrange("p (h n) -> p h n", h=4, n=16)[:, :, 0:obk], (B_pG,), (B_G,))
                    for h_ in range(4):
                        P.emit("dve", (lambda hh_: (lambda h: h.max(out=top8[:, hh_, :], in_=Gt[:, hh_, :])))(h_), (B_G,), (B_t8,))
                        TS("dve", top8[:, h_, 2:3], top8[:, h_, 2:3], -1e8, None, ALU.max, None, (B_t8,), (B_t8,))
                        TS("dve", mbias[:, h_, :], Gt[:, h_, :], top8[:, h_, 2:3], NEG, ALU.is_lt, ALU.mult, (B_G, B_t8), (B_mb,))

                def moba_mask(stl, obk=obk):
                    n = stl // 2
                    r_ = []
                    if n < obk:
                        for h_ in range(4):
                            r_.append((h_ * 128, (h_ + 1) * 128, mbias[:, h_, n:n + 1].to_broadcast([128, 128]), ident[:], (B_mb, B_const)))
                    return r_

                Om, B_Om = ps[6], B_ps[6]
                Dm, B_Dm = ps[7], B_ps[7]
                for stl in range(ns):
                    attn_tile(dkS, B_dk, dvS, B_dv, dq_, B_dq, stl, 4, j, Om, B_Om, Dm, B_Dm, Lrot, stl == 0, stl == ns - 1, moba_mask)

                sv = scores[:, 0:nv]
                RED(st_[:, 0:1], sv, ALU.min, (B_sc,), (B_st,))
                RED(st_[:, 1:2], sv, ALU.max, (B_sc,), (B_st,))
                TT("dve", scores[:, j * 128:(j + 1) * 128], scores[:, j * 128:(j + 1) * 128], trineg, ALU.add, (B_sc, B_const), (B_sc,))
                TS("dve", st_[:, 2:3], st_[:, 1:2], st_[:, 0:1], 0.02, ALU.subtract, ALU.add, (B_st,), (B_st,))
                STT(st_[:, 3:4], st_[:, 2:3], 0.5, st_[:, 0:1], ALU.mult, ALU.add, (B_st,), (B_st,))
                TS("dve", st_[:, 3:4], st_[:, 3:4], -0.01, None, ALU.add, None, (B_st,), (B_st,))
                TS("dve", st_[:, 16:32], c1, st_[:, 2:3], None, ALU.mult, None, (B_st, B_const), (B_st,))
                TS("dve", st_[:, 32:48], c2, st_[:, 2:3], None, ALU.mult, None, (B_st, B_const), (B_st,))
                for it in range(nbis):
                    TS("dve", maskb[:, 0:nv], sv, st_[:, 3:4], 0.0, ALU.is_ge, ALU.add, (B_sc, B_st), (B_mk, B_st), accum=st_[:, 4:5])
                    STT(st_[:, 5:6], st_[:, 4:5], KTOP - 0.5, st_[:, 32 + it:33 + it], ALU.is_ge, ALU.mult, (B_st,), (B_st,))
                    STT(st_[:, 3:4], st_[:, 3:4], st_[:, 16 + it:17 + it], st_[:, 5:6], ALU.subtract, ALU.add, (B_st,), (B_st,))
                TS("dve", maskb[:, 0:nv], sv, st_[:, 3:4], NEG, ALU.is_lt, ALU.mult, (B_sc, B_st), (B_mk,))

                ob, B_ob = outb[1]
                finish(Om, B_Om, Dm, B_Dm, ob, B_ob, 768, j)

                units = []
                if j + 1 < NT:
                    q_loads(j + 1)
                    units = indexer_units(j + 1)

                def dsa_mask(stl):
                    return [(0, 512, maskb[:, stl * 128:(stl + 1) * 128], I4[:, :], (B_mk, B_const))]

                O, B_O = ps[2], B_ps[2]
                Dn, B_Dn = ps[3], B_ps[3]
                ui = 0
                for stl in range(ns):
                    attn_tile(ckS, B_ck, cvS, B_cv, cq_, B_cq, stl, 0, j, O, B_O, Dn, B_Dn, Lrot, stl == 0, stl == ns - 1, dsa_mask)
                    tgt = (len(units) * (stl + 1) + ns - 1) // ns
                    while ui < tgt:
                        units[ui]()
                        ui += 1
                while ui < len(units):
                    units[ui]()
                    ui += 1
                ob, B_ob = outb[0]
                finish(O, B_O, Dn, B_Dn, ob, B_ob, 512, j)


        run_A0()
        barrier()
        if stop != "A0":
            for l in range(nlayers):
                run_B1a(l)
                barrier()
                if stop is not None and stop.startswith("B1a0"):
                    break
                try:
                    run_dense(l)
                except _Stop:
                    pass
                barrier()
                if stop is not None and stop.startswith("dense0"):
                    break
        if stop is not None:
            AR.reset()
            t_, B_t = AR.get("dbg", [128, 64], F32)
            MSET("dve", t_, 0.0, (B_t,))
            DMA("sp", y_out[0:128, 0:64], t_, (B_t,), ())
        P.generate(nc, block, sems)
    return nc, P


def _rel_bucket(n):
    n = np.maximum(n, 0)
    nf = np.maximum(n, 1).astype(np.float32)
    large = 16 + (np.log(nf / np.float32(16)) / np.float32(np.log(128 / 16)) * np.float32(16)).astype(np.int32)
    large = np.minimum(large, 31)
    return np.where(n < 16, n, large)


def host_inputs(inputs, nlayers=DEPTH):
    f = lambda a: np.ascontiguousarray(np.asarray(a, dtype=np.float32))
    rel_bias = f(inputs["rel_bias"])
    q = np.arange(128)[:, None]
    s = np.arange(128)[None, :]
    relT = np.zeros((128, 16, 128), np.float32)
    for h in range(8):
        for d in range(2):
            relT[:, h * 2 + d, :] = rel_bias[h][_rel_bucket(d * 128 + q - s)]
    relb = np.ascontiguousarray(np.broadcast_to(rel_bias.reshape(1, 256), (128, 256)))
    consts = np.zeros((128, 512), np.float32)
    consts[:, 0:128] = np.eye(128)
    consts[:, 128:256] = np.where(s > q, NEG, 0.0)
    consts[:, 256:384] = np.where(s > q, -1e9, 0.0)
    K = NBIS
    c1 = np.array([2.0 ** -(i + 2) for i in range(K - 1)] + [2.0 ** -K])
    c2 = np.array([2.0 ** -(i + 1) for i in range(K - 1)] + [2.0 ** -K])
    consts[:, 384:384 + K] = c1
    consts[:, 400:400 + K] = c2
    wins = {(0, 0): 2, (1, 0): 4, (0, 1): 8, (1, 1): 16}
    for ph in range(2):
        for c in range(2):
            w = wins[(ph, c)]
            for t in range(16):
                consts[ph * 64:(ph + 1) * 64, 416 + c * 16 + t] = 1.0 / min(t + 1, w)
    g = lambda k: f(inputs[k])
    gpack = np.zeros((DEPTH + 1, 5, D), np.float32)
    for l in range(DEPTH):
        gpack[l + 1, 0] = g("g_mix_post")[l]
        gpack[l + 1, 1] = g("g_ffn_pre")[l]
        gpack[l + 1, 2] = g("g_ffn_post")[l]
        gpack[l + 1, 3] = g("g_ple")[l]
        gpack[l, 4] = g("g_mix_pre")[l]
    shared = dict(
        w_in=g("w_in")[:nlayers], w_out=g("w_out")[:nlayers], w_gate_up=g("w_gate_up")[:nlayers], w_down=g("w_down")[:nlayers],
        w_ple_gate=g("w_ple_gate")[:nlayers], w_ple_proj=g("w_ple_proj")[:nlayers], gpack=gpack,
        convw_t=np.ascontiguousarray(g("conv_w").transpose(0, 2, 1)),
        pool_w=g("pool_w"),
        pscale_t=np.ascontiguousarray(g("pool_scale").reshape(DEPTH, 2, 128).transpose(0, 2, 1)),
        relT=relT, relb=relb, consts=consts,
    )
    x = g("x")
    p = g("p")
    maps = []
    for c in range(8):
        b = c % 4
        m = dict(shared)
        m["x"] = np.ascontiguousarray(x[b])
        m["p"] = np.ascontiguousarray(p[:nlayers, b])
        maps.append(m)
    return maps


_CACHE = {}


def kernel(**inputs):
    if "nc" not in _CACHE:
        _CACHE["nc"] = build()[0]
    nc = _CACHE["nc"]
    maps = host_inputs(inputs)
    res = run_bass_kernel_spmd(nc, maps, core_ids=list(range(8)))
    out = np.stack([np.asarray(res.results[b]["y"], dtype=np.float32) for b in range(4)], axis=0)
    return out
```
